# Optimizing a Trainium2 kernel written in Bass

```python
import jax
import jax.numpy as jnp
from jax import lax
import numpy as np

D_MODEL = 1024
BATCH = 8
SEQ = 4096
DEPTH = 2

GRID_W = 64
CTX_LEN = 256
HEAD_DIM = 64
N_Q_HEADS = 8
N_KV_HEADS = 2
Q_PER_KV = N_Q_HEADS // N_KV_HEADS
ATT_Q_W = N_Q_HEADS * HEAD_DIM
ATT_KV_W = N_KV_HEADS * HEAD_DIM
ATT_SCALE = HEAD_DIM ** -0.5
ROPE_THETA = 10000.0
ROPE_AXIS_DIM = HEAD_DIM // 2
QBLK = 128
WINDOW = 128
SPAN = QBLK + 2 * WINDOW
N_FOURIER_GROUPS = 4
FOURIER_GROUP_W = D_MODEL // 8
FOURIER_W = N_FOURIER_GROUPS * FOURIER_GROUP_W
POOL_WINDOWS = (2, 4, 8, 16)
POOL_GROUP_W = D_MODEL // 8
POOL_W = len(POOL_WINDOWS) * POOL_GROUP_W
N_BRANCHES = 4
BRANCH_W = ATT_Q_W
IN_SPLITS = (FOURIER_W, POOL_W, ATT_Q_W, ATT_KV_W, ATT_KV_W, ATT_Q_W, ATT_KV_W, ATT_KV_W)
IN_W = sum(IN_SPLITS)
IN_OFFSETS = tuple(sum(IN_SPLITS[:i + 1]) for i in range(len(IN_SPLITS) - 1))
N_EXPERTS = 16
N_EXPERT_GROUPS = 4
EXPERTS_PER_GROUP = N_EXPERTS // N_EXPERT_GROUPS
TOP_K = 2
EXPERT_FF = D_MODEL // 2
EPS = 1e-6

kernel_name = 'hybrid_parallel_dit_block'


def rms_norm(x, gain):
    xf = x.astype(jnp.float32)
    y = xf * lax.rsqrt(jnp.mean(xf * xf, axis=-1, keepdims=True) + EPS)
    return (y * gain.astype(jnp.float32)).astype(x.dtype)


def modulate(x, shift, scale):
    return x * (1 + scale) + shift


def rope_tables(n_tokens):
    rows = n_tokens // GRID_W
    row = jnp.repeat(jnp.arange(rows, dtype=jnp.float32), GRID_W)
    col = jnp.tile(jnp.arange(GRID_W, dtype=jnp.float32), rows)
    inv_freq = ROPE_THETA ** (-jnp.arange(0, ROPE_AXIS_DIM, 2, dtype=jnp.float32) / ROPE_AXIS_DIM)
    ang = jnp.stack([row[:, None] * inv_freq, col[:, None] * inv_freq], axis=1)
    return jnp.cos(ang), jnp.sin(ang)


def apply_rope_2d(x, cos, sin):
    xs = x.astype(jnp.float32).reshape(*x.shape[:-1], 2, 2, ROPE_AXIS_DIM // 2)
    x1, x2 = xs[..., 0, :], xs[..., 1, :]
    c, s = cos[:, None], sin[:, None]
    out = jnp.stack([x1 * c - x2 * s, x2 * c + x1 * s], axis=-2)
    return out.reshape(x.shape).astype(x.dtype)


def heads(a, n):
    return a.reshape(a.shape[0], a.shape[1], n, HEAD_DIM)


def group_q(q):
    return q.reshape(q.shape[0], q.shape[1], N_KV_HEADS, Q_PER_KV, HEAD_DIM)


def to_blocks(q):
    b, t = q.shape[:2]
    return q.reshape(b, t // QBLK, QBLK, *q.shape[2:]).swapaxes(0, 1)


def from_blocks(o):
    o = o.swapaxes(0, 1)
    return o.reshape(o.shape[0], -1, ATT_Q_W)


def sink_softmax(s, sink):
    col = jnp.broadcast_to(sink.astype(jnp.float32)[None, :, :, None, None], s.shape[:-1] + (1,))
    p = jax.nn.softmax(jnp.concatenate([s, col], axis=-1), axis=-1)
    return p[..., :-1]


def context_attention(qc, kc, vc, sink=None):
    s = jnp.einsum('bqgrd,bkgd->bgrqk', qc, kc).astype(jnp.float32) * ATT_SCALE
    p = jax.nn.softmax(s, axis=-1) if sink is None else sink_softmax(s, sink)
    o = jnp.einsum('bgrqk,bkgd->bqgrd', p.astype(vc.dtype), vc)
    return o.reshape(o.shape[0], o.shape[1], ATT_Q_W)


def global_attention(q, k, v, kc, vc):
    k_all = jnp.concatenate([kc, k], axis=1)
    v_all = jnp.concatenate([vc, v], axis=1)

    def one_block(qb):
        s = jnp.einsum('bqgrd,bkgd->bgrqk', qb, k_all).astype(jnp.float32) * ATT_SCALE
        p = jax.nn.softmax(s, axis=-1).astype(v_all.dtype)
        return jnp.einsum('bgrqk,bkgd->bqgrd', p, v_all)

    return from_blocks(lax.map(one_block, to_blocks(q)))


def window_attention(q, k, v, kc, vc, sink):
    t = q.shape[1]
    n_ctx = kc.shape[1]
    pad = ((0, 0), (WINDOW, WINDOW), (0, 0), (0, 0))
    kp, vp = jnp.pad(k, pad), jnp.pad(v, pad)
    qi = jnp.arange(QBLK)[:, None]
    kj = jnp.arange(SPAN)[None, :]
    in_band = jnp.abs(kj - WINDOW - qi) <= WINDOW

    def one_block(args):
        qb, blk = args
        start = blk * QBLK
        kw = lax.dynamic_slice_in_dim(kp, start, SPAN, axis=1)
        vw = lax.dynamic_slice_in_dim(vp, start, SPAN, axis=1)
        kpos = start - WINDOW + kj
        valid = in_band & (kpos >= 0) & (kpos < t)
        s_ctx = jnp.einsum('bqgrd,bkgd->bgrqk', qb, kc).astype(jnp.float32)
        s_win = jnp.where(valid, jnp.einsum('bqgrd,bkgd->bgrqk', qb, kw).astype(jnp.float32), -jnp.inf)
        p = sink_softmax(jnp.concatenate([s_ctx, s_win], axis=-1) * ATT_SCALE, sink).astype(v.dtype)
        return (jnp.einsum('bgrqk,bkgd->bqgrd', p[..., :n_ctx], vc)
                + jnp.einsum('bgrqk,bkgd->bqgrd', p[..., n_ctx:], vw))

    return from_blocks(lax.map(one_block, (to_blocks(q), jnp.arange(t // QBLK))))


def fourier_mix(u):
    b, t = u.shape[:2]
    g = u.astype(jnp.float32).reshape(b, t, N_FOURIER_GROUPS, FOURIER_GROUP_W)
    f = jnp.fft.fft2(g, axes=(1, 3), norm='ortho').real
    return f.reshape(b, t, FOURIER_W).astype(u.dtype)


def pool_mix(u, pool_w, pool_scale):
    b, t = u.shape[:2]
    g = u.astype(jnp.float32).reshape(b, t, len(POOL_WINDOWS), POOL_GROUP_W)
    cs = jnp.pad(jnp.cumsum(g, axis=1), ((0, 0), (1, 0), (0, 0), (0, 0)))
    pos = jnp.arange(t)
    outs = []
    for gi, w in enumerate(POOL_WINDOWS):
        lo = jnp.clip(pos - w // 2, 0, t)
        hi = jnp.clip(pos + w // 2, 0, t)
        cs_g = cs[:, :, gi]
        mean = (cs_g[:, hi] - cs_g[:, lo]) / (hi - lo).astype(jnp.float32)[:, None]
        outs.append(mean - g[:, :, gi])
    pooled = jnp.stack(outs, axis=2).astype(u.dtype)
    mixed = jnp.einsum('btgc,gce->btge', pooled, pool_w)
    return mixed.reshape(b, t, POOL_W) * pool_scale


def branch_merge(h, branches, w_branch, w_gate, b_gate, w_out):
    merged = jnp.zeros_like(h)
    for i, br in enumerate(branches):
        gate = jax.nn.sigmoid(h @ w_gate[i] + b_gate[i])
        merged = merged + gate * (br @ w_branch[i])
    return merged @ w_out


def token_mixer(h, hc, cos, sin, w_in, q_gain, k_gain, sink, pool_w, pool_scale,
                w_branch, w_gate, b_gate, w_out, need_ctx_out):
    f_in, p_in, qb, kb, vb, qw, kw, vw = jnp.split(h @ w_in, IN_OFFSETS, axis=-1)
    fc_in, pc_in, qbc, kbc, vbc, qwc, kwc, vwc = jnp.split(hc @ w_in, IN_OFFSETS, axis=-1)
    q_b = apply_rope_2d(rms_norm(heads(qb, N_Q_HEADS), q_gain), cos, sin)
    k_b = apply_rope_2d(rms_norm(heads(kb, N_KV_HEADS), k_gain), cos, sin)
    v_b = heads(vb, N_KV_HEADS)
    k_bc = rms_norm(heads(kbc, N_KV_HEADS), k_gain)
    v_bc = heads(vbc, N_KV_HEADS)
    out_b = global_attention(group_q(q_b), k_b, v_b, k_bc, v_bc)
    sink_gr = sink.reshape(N_KV_HEADS, Q_PER_KV)
    q_c = apply_rope_2d(heads(qw, N_Q_HEADS), cos, sin)
    k_c = apply_rope_2d(heads(kw, N_KV_HEADS), cos, sin)
    v_c = heads(vw, N_KV_HEADS)
    k_cc = heads(kwc, N_KV_HEADS)
    v_cc = heads(vwc, N_KV_HEADS)
    out_c = window_attention(group_q(q_c), k_c, v_c, k_cc, v_cc, sink_gr)
    out_a = fourier_mix(f_in)
    out_d = pool_mix(p_in, pool_w, pool_scale)
    y = branch_merge(h, (out_a, out_b, out_c, out_d), w_branch, w_gate, b_gate, w_out)
    yc = None
    if need_ctx_out:
        oc_b = context_attention(group_q(rms_norm(heads(qbc, N_Q_HEADS), q_gain)), k_bc, v_bc)
        oc_c = context_attention(group_q(heads(qwc, N_Q_HEADS)), k_cc, v_cc, sink_gr)
        oc_a = fourier_mix(fc_in)
        oc_d = pool_mix(pc_in, pool_w, pool_scale)
        yc = branch_merge(hc, (oc_a, oc_b, oc_c, oc_d), w_branch, w_gate, b_gate, w_out)
    return y, yc


def moe_ffn(h, router_w, router_bias, w1, w3, w2):
    shape = h.shape
    tok = h.reshape(-1, D_MODEL)
    aff = jax.nn.sigmoid((tok @ router_w).astype(jnp.float32))
    sel = aff + router_bias.astype(jnp.float32)
    grp = sel.reshape(-1, N_EXPERT_GROUPS, EXPERTS_PER_GROUP)
    group_score = lax.top_k(grp, TOP_K)[0].sum(-1)
    best = jnp.argmax(group_score, axis=-1)
    in_group = best[:, None] == (jnp.arange(N_EXPERTS) // EXPERTS_PER_GROUP)[None, :]
    _, top_idx = lax.top_k(jnp.where(in_group, sel, -jnp.inf), TOP_K)
    w_top = jnp.take_along_axis(aff, top_idx, axis=-1)
    w_top = w_top / jnp.sum(w_top, axis=-1, keepdims=True)
    combine = jnp.sum(jax.nn.one_hot(top_idx, N_EXPERTS, dtype=jnp.float32) * w_top[..., None], axis=1)
    combine = combine.astype(h.dtype)
    out = jnp.zeros_like(tok)
    for e in range(N_EXPERTS):
        hid = jax.nn.silu(tok @ w1[e]) * (tok @ w3[e])
        out = out + combine[:, e:e + 1] * (hid @ w2[e])
    return out.reshape(shape)


def trunk_layer(x, xc, mod, mod_c, cos, sin, norm1, norm2, w_in, q_gain, k_gain, sink,
                pool_w, pool_scale, w_branch, w_gate, b_gate, w_out,
                router_w, router_bias, w1, w3, w2, need_ctx_out):
    sh1, sc1, g1, sh2, sc2, g2 = jnp.split(mod[:, None, :], 6, axis=-1)
    sh1c, sc1c, g1c, sh2c, sc2c, g2c = jnp.split(mod_c, 6)
    h = modulate(rms_norm(x, norm1), sh1, sc1)
    hc = modulate(rms_norm(xc, norm1), sh1c, sc1c)
    y, yc = token_mixer(h, hc, cos, sin, w_in, q_gain, k_gain, sink, pool_w, pool_scale,
                        w_branch, w_gate, b_gate, w_out, need_ctx_out)
    x = x + g1 * y
    x = x + g2 * moe_ffn(modulate(rms_norm(x, norm2), sh2, sc2), router_w, router_bias, w1, w3, w2)
    if need_ctx_out:
        xc = xc + g1c * yc
        xc = xc + g2c * moe_ffn(modulate(rms_norm(xc, norm2), sh2c, sc2c), router_w, router_bias, w1, w3, w2)
    return x, xc


def setup_inputs(seed: int = 0) -> dict:
    key = jax.random.key(seed)
    ks = jax.random.split(key, 24)
    f32 = jnp.float32

    def nrm(k, shape, scale):
        return jax.random.normal(k, shape, f32) * scale

    return {
        'x': nrm(ks[0], (BATCH, SEQ, D_MODEL), 1.0),
        'c': nrm(ks[1], (BATCH, D_MODEL), 1.0),
        'ctx': nrm(ks[2], (BATCH, CTX_LEN, D_MODEL), 1.0),
        'c_ctx': nrm(ks[3], (D_MODEL,), 1.0),
        'w_ada': nrm(ks[4], (DEPTH, D_MODEL, 6 * D_MODEL), 0.5 * D_MODEL ** -0.5),
        'b_ada': nrm(ks[5], (DEPTH, 6 * D_MODEL), 0.02),
        'norm1': 1.0 + nrm(ks[6], (DEPTH, D_MODEL), 0.05),
        'norm2': 1.0 + nrm(ks[7], (DEPTH, D_MODEL), 0.05),
        'w_in': nrm(ks[8], (DEPTH, D_MODEL, IN_W), D_MODEL ** -0.5),
        'q_gain': 1.0 + nrm(ks[9], (DEPTH, HEAD_DIM), 0.05),
        'k_gain': 1.0 + nrm(ks[10], (DEPTH, HEAD_DIM), 0.05),
        'sink': nrm(ks[11], (DEPTH, N_Q_HEADS), 0.5),
        'pool_w': nrm(ks[12], (DEPTH, len(POOL_WINDOWS), POOL_GROUP_W, POOL_GROUP_W), POOL_GROUP_W ** -0.5),
        'pool_scale': 1.0 + nrm(ks[13], (DEPTH, POOL_W), 0.1),
        'w_branch': nrm(ks[14], (DEPTH, N_BRANCHES, BRANCH_W, D_MODEL), BRANCH_W ** -0.5),
        'w_gate': nrm(ks[15], (DEPTH, N_BRANCHES, D_MODEL, D_MODEL), D_MODEL ** -0.5),
        'b_gate': nrm(ks[16], (DEPTH, N_BRANCHES, D_MODEL), 0.02),
        'w_out': nrm(ks[17], (DEPTH, D_MODEL, D_MODEL), D_MODEL ** -0.5),
        'router_w': nrm(ks[18], (D_MODEL, N_EXPERTS), D_MODEL ** -0.5),
        'router_bias': nrm(ks[19], (N_EXPERTS,), 0.01),
        'w1': nrm(ks[20], (DEPTH, N_EXPERTS, D_MODEL, EXPERT_FF), D_MODEL ** -0.5),
        'w3': nrm(ks[21], (DEPTH, N_EXPERTS, D_MODEL, EXPERT_FF), D_MODEL ** -0.5),
        'w2': nrm(ks[22], (DEPTH, N_EXPERTS, EXPERT_FF, D_MODEL), EXPERT_FF ** -0.5),
        'norm_f': 1.0 + nrm(ks[23], (D_MODEL,), 0.05),
    }


def reference(x, c, ctx, c_ctx, w_ada, b_ada, norm1, norm2, w_in, q_gain, k_gain, sink,
              pool_w, pool_scale, w_branch, w_gate, b_gate, w_out, router_w, router_bias,
              w1, w3, w2, norm_f):
    cos, sin = rope_tables(x.shape[1])
    xc = ctx
    for l in range(DEPTH):
        mod = jax.nn.silu(c) @ w_ada[l] + b_ada[l]
        mod_c = jax.nn.silu(c_ctx) @ w_ada[l] + b_ada[l]
        x, xc = trunk_layer(x, xc, mod, mod_c, cos, sin, norm1[l], norm2[l], w_in[l],
                            q_gain[l], k_gain[l], sink[l], pool_w[l], pool_scale[l],
                            w_branch[l], w_gate[l], b_gate[l], w_out[l],
                            router_w, router_bias, w1[l], w3[l], w2[l],
                            need_ctx_out=(l < DEPTH - 1))
    return rms_norm(x, norm_f)
```

```python
import contextlib
import numpy as np
import ml_dtypes
import concourse.bass as bass
import concourse.mybir as mybir
from concourse.bass_utils import run_bass_kernel_spmd

F32 = mybir.dt.float32
I32 = mybir.dt.int32
BF16 = mybir.dt.bfloat16
AF = mybir.ActivationFunctionType
ALU = mybir.AluOpType
AX = mybir.AxisListType

D = 1024
T = 4096
CL = 256
NK = T + CL
DEPTH = 2
NE = 16
NSLOT = 12
NCHT = 34
NTT = T + CL
EPS = 1e-6
SAME_SYNC = True
POOL_K = 6
BIG = 1.0e4


class DSem:
    def __init__(self, h):
        self.h = h
        self.total = 0


class Tl:
    def __init__(self, h, name):
        self.h = h
        self.name = name
        self.w = None
        self.rs = {}
        self.sem = None

    def __getitem__(self, k):
        return self.h[k]


class FW:
    CE = ("pe", "act", "dve", "pool")

    def __init__(self, nc):
        self.nc = nc
        self.E = {"pe": nc.tensor, "act": nc.scalar, "dve": nc.vector, "pool": nc.gpsimd, "sp": nc.sync}
        self.ges = contextlib.ExitStack()
        self.sem = {e: self.ges.enter_context(nc.semaphore("s_" + e)) for e in self.CE}
        self.cnt = {e: 0 for e in self.CE}
        self.seen = {e: {} for e in self.E}
        self.free_dsems = []
        self.live_dsems = []
        self.pes = None
        self.uid = 0
        self.npe = 0
        self.marks = []
        self.pool_out = []
        self.glob_dsems = []

    def phase(self, label=""):
        self.marks.append((label, self.npe))
        self.pes = contextlib.ExitStack()
        return self.pes

    def _name(self, n):
        self.uid += 1
        return "%s_%d" % (n, self.uid)

    def tile(self, name, shape, dtype, dma=False, glob=False):
        es = self.ges if glob else self.pes
        h = es.enter_context(self.nc.sbuf_tensor(self._name(name), list(shape), dtype))
        t = Tl(h, name)
        if dma:
            if self.free_dsems:
                ds = self.free_dsems.pop()
            else:
                ds = DSem(self.ges.enter_context(self.nc.semaphore(self._name("d"))))
            t.sem = ds
            if not glob:
                self.live_dsems.append(ds)
            else:
                self.glob_dsems.append(ds)
        return t


    def ptile(self, name, shape, dtype=F32):
        h = self.pes.enter_context(self.nc.psum_tensor(self._name(name), list(shape), dtype))
        return Tl(h, name)

    def ring(self, name, n, shape, dtype, dma=False):
        return [self.tile("%s%d" % (name, i), shape, dtype, dma=dma) for i in range(n)]

    def pring(self, name, n, shape, dtype=F32):
        return [self.ptile("%s%d" % (name, i), shape, dtype) for i in range(n)]

    def _wait(self, eng, deps):
        for (key, sem, val, src) in deps:
            if src == eng:
                if eng == "pe" or not SAME_SYNC:
                    continue
            if self.seen[eng].get(key, 0) >= val:
                continue
            self.E[eng].wait_ge(sem, val)
            self.seen[eng][key] = val

    def op(self, eng, fn, R=(), W=(), sig=True):
        deps = []
        for t in R:
            if t.w is not None:
                deps.append(t.w)
        for t in W:
            if t.w is not None:
                deps.append(t.w)
            deps.extend(t.rs.values())
        self._wait(eng, deps)
        ins = fn()
        if eng == "pe":
            self.npe += 1
        if sig:
            self.cnt[eng] += 1
            ins.then_inc(self.sem[eng], 1)
            tok = (eng, self.sem[eng], self.cnt[eng], eng)
        else:
            tok = (eng, self.sem[eng], self.cnt[eng] + 1, eng)
        for t in R:
            t.rs[eng] = tok
        for t in W:
            t.w = tok
            t.rs = {}
        return ins

    def dma(self, out, in_, tile, store=False, q="sp", deps=()):
        d = list(deps)
        if store:
            if tile.w is not None:
                d.append(tile.w)
        else:
            if tile.w is not None and not (tile.w[3] == "dma" and tile.w[0] == id(tile.sem)):
                d.append(tile.w)
            d.extend(tile.rs.values())
        self._wait(q, d)
        ins = self.E[q].dma_start(out=out, in_=in_)
        ds = tile.sem
        ds.total += 16
        ins.then_inc(ds.h, 16)
        tok = (id(ds), ds.h, ds.total, "dma")
        if store:
            tile.rs["dma"] = tok
        else:
            tile.w = tok
            tile.rs = {}
        return ins

    def idma(self, out, out_off, in_, in_off, bound, tile, idx, store=False, deps=()):
        q = "pool"
        d = list(deps)
        if idx.w is not None:
            d.append(idx.w)
        if store:
            if tile.w is not None:
                d.append(tile.w)
        else:
            if tile.w is not None and not (tile.w[3] == "dma" and tile.w[0] == id(tile.sem)):
                d.append(tile.w)
            d.extend(tile.rs.values())
        self._wait(q, d)
        while len(self.pool_out) >= POOL_K:
            self._wait(q, [self.pool_out.pop(0)])
        ins = self.nc.gpsimd.indirect_dma_start(out=out, out_offset=out_off, in_=in_, in_offset=in_off)
        ds = tile.sem
        ds.total += 16
        ins.then_inc(ds.h, 16)
        tok = (id(ds), ds.h, ds.total, "dma")
        idx.rs["idma"] = tok
        self.pool_out.append(tok)
        if store:
            tile.rs["dma"] = tok
        else:
            tile.w = tok
            tile.rs = {}
        return ins

    def barrier(self, end_phase=True):
        for e in self.E:
            deps = []
            for o in self.CE:
                if o != e and self.cnt[o] > 0:
                    deps.append((o, self.sem[o], self.cnt[o], o))
            for ds in self.live_dsems + self.glob_dsems:
                if ds.total > 0:
                    deps.append((id(ds), ds.h, ds.total, "dma"))
            self._wait(e, deps)
        if end_phase:
            self.free_dsems.extend(self.live_dsems)
            self.live_dsems = []
            self.pes.close()
            self.pes = None


def prefetch_loop(n, load, compute, depth=1):
    q = [load(k) for k in range(min(depth, n))]
    for k in range(n):
        if k + depth < n:
            q.append(load(k + depth))
        compute(k, q.pop(0))


class Seq:
    def __init__(self, name, L, NB, n, koff, rope):
        self.name = name
        self.L = L
        self.NB = NB
        self.nblk = L // NB
        self.nsub = NB // 128
        self.n = n
        self.koff = koff
        self.rope = rope
        self.SB = min(L, 512)
        self.nsb = L // self.SB


LAT = Seq("lat", T, 512, 0, CL, True)
CTX = Seq("ctx", CL, 256, 1, 0, False)

V_N1, V_N2, V_BG, V_PS, V_BA, V_NF = 0, 8, 16, 48, 52, 100
NV = 108
R_GAIN, R_SINK, R_RB = 0, 640, 648
NR = 664
M_SH1, M_SC1, M_G1, M_SH2, M_SC2, M_G2 = 0, 8, 16, 24, 32, 40


class Ctx:
    pass


def build(debug=False, stop=None):
    nc = bass.Bass("TRN2", target_bir_lowering=False)
    fw = FW(nc)
    cx = Ctx()
    skind = "ExternalOutput" if debug else "Internal"

    def din(name, shape, dt=F32):
        return nc.dram_tensor(name, list(shape), dt, kind="ExternalInput").ap()

    def dscr(name, shape, dt):
        return nc.dram_tensor(name, list(shape), dt, kind=skind).ap()

    cx.xT = din("xT", [D, T])
    cx.ctxT = din("ctxT", [D, CL])
    cx.c2 = din("c2", [128, 8, 2])
    cx.vecs = din("vecs", [DEPTH, 128, NV])
    cx.rep = din("rep", [DEPTH, 128, NR])
    cx.w_ada = din("w_ada", [DEPTH, 12, 128, 8, 512])
    cx.w_in = din("w_in", [DEPTH, 128, 8, 2560])
    cx.w_gate = din("w_gate", [DEPTH, 8, 128, 4, 8, 128])
    cx.w_branch = din("w_branch", [DEPTH, 8, 128, 4, 4, 128])
    cx.w_out = din("w_out", [DEPTH, 8, 128, 8, 128])
    cx.pool_w = din("pool_w", [DEPTH, 128, 4, 128])
    cx.router_w = din("router_w", [128, 8, 16])
    cx.w1 = din("w1", [DEPTH, NE, 128, 8, 512])
    cx.w3 = din("w3", [DEPTH, NE, 128, 8, 512])
    cx.w2 = din("w2", [DEPTH, NE, 128, 4, 1024])
    cx.rope_cs = din("rope_cs", [128, 32, 64])
    cx.dftC = din("dftC", [T, T], BF16)
    cx.dftS = din("dftS", [T, T], BF16)
    cx.dftCc = din("dftCc", [CL, CL], BF16)
    cx.dftSc = din("dftSc", [CL, CL], BF16)
    cx.csm = din("csm", [128, 256], BF16)
    cx.ident = din("ident", [128, 128], BF16)
    cx.identf = din("identf", [128, 128], F32)
    cx.wmask = din("wmask", [128, 6, 512], BF16)
    cx.invcnt = din("invcnt", [4, 128, T])
    cx.invcntc = din("invcntc", [4, 128, CL])
    cx.ustrict = din("ustrict", [128, 128], BF16)
    cx.th9 = din("th9", [128, 4, 9])
    cx.siota = din("siota", [128, NSLOT, 3])
    cx.jp = din("jp", [128, 4])
    cx.tokidx = din("tokidx", [128, NCHT], I32)
    cx.sel4 = din("sel4", [4, 4, 128])
    cx.oobfill = din("oobfill", [NSLOT * 512, 1], I32)
    cx.zero4 = din("zero4", [NSLOT * 512, 4])
    cx.yT = nc.dram_tensor("yT", [D, T], F32, kind="ExternalOutput").ap()
    cx.h2tm = dscr("h2tm", [NTT + 128, D], BF16)
    cx.moe_tm = dscr("moe_tm", [NTT + 128, D], F32)
    cx.zrow = din("zrow", [128, D], BF16)
    cx.permD = dscr("permD", [NSLOT * 512, 1], I32)
    cx.c4D = dscr("c4D", [NSLOT * 512, 4], F32)
    cx.debug = debug
    if debug:
        cx.d_hs = dscr("d_hs", [128, 8, 512], BF16)
        cx.d_cb = dscr("d_cb", [4, 128, 512], F32)
        cx.d_acc = dscr("d_acc", [128, 8, 512], F32)
        cx.d_om = dscr("d_om", [128, 4, 1024], F32)
        cx.d_w = dscr("d_w", [128, 12288], BF16)
        cx.d_hg = dscr("d_hg", [128, 4, 1024], BF16)

    cx.b_ada = dscr("b_w_ada", [DEPTH, 12, 128, 8, 512], BF16)
    cx.b_in = dscr("b_w_in", [DEPTH, 128, 8, 2560], BF16)
    cx.b_gate = dscr("b_w_gate", [DEPTH, 8, 128, 4, 8, 128], BF16)
    cx.b_branch = dscr("b_w_branch", [DEPTH, 8, 128, 4, 4, 128], BF16)
    cx.b_out = dscr("b_w_out", [DEPTH, 8, 128, 8, 128], BF16)
    cx.b_pool = dscr("b_pool_w", [DEPTH, 128, 4, 128], BF16)
    cx.b_router = dscr("b_router", [128, 8, 16], BF16)
    cx.b_wcat = dscr("b_wcat", [DEPTH, NE, 128, 12288], BF16)

    for s in (LAT, CTX):
        L = s.L
        sc = Ctx()
        sc.x = [None, dscr(s.name + "_x1", [D, L], F32)]
        sc.xmid = dscr(s.name + "_xmid", [D, L], F32)
        sc.hT = dscr(s.name + "_hT", [D, L], BF16)
        sc.h2T = dscr(s.name + "_h2T", [D, L], BF16)
        sc.pinT = dscr(s.name + "_pinT", [512, L], F32)
        sc.AB = dscr(s.name + "_AB", [L, 1024], BF16)
        sc.qT = {"b": dscr(s.name + "_qbT", [512, L], BF16), "c": dscr(s.name + "_qcT", [512, L], BF16)}
        sc.br = dscr(s.name + "_br", [4, 512, L], BF16)
        sc.combT = dscr(s.name + "_combT", [NE, L], F32)
        cx.__dict__[s.name] = sc
    cx.lat.x[0] = cx.xT
    cx.ctx.x[0] = cx.ctxT
    cx.kT = {"b": dscr("kbT", [2, 128, NK], BF16), "c": dscr("kcT", [2, 128, NK], BF16)}
    cx.v = {"b": dscr("vb", [NK, 130], BF16), "c": dscr("vc", [NK, 130], BF16)}

    psem = {}

    def precast(key, dst, src):
        if key not in psem:
            psem[key] = DSem(fw.ges.enter_context(nc.semaphore(fw._name("pc"))))
        ds = psem[key]
        nc.gpsimd.dma_start(out=dst, in_=src).then_inc(ds.h, 16)
        ds.total += 16

    def pdep(key):
        ds = psem[key]
        return [(id(ds), ds.h, ds.total, "dma")]
    cx.pdep = pdep

    def precast_group(key):
        kind, l = key
        if kind == "a":
            wi = cx.w_in[l].rearrange("p c n -> (p c) n")
            bi = cx.b_in[l].rearrange("p c n -> (p c) n")
            for jb in range(12):
                precast(key, cx.b_ada[l, jb], cx.w_ada[l, jb])
            for i in range(4):
                precast(key, bi[i * 256:(i + 1) * 256, :], wi[i * 256:(i + 1) * 256, :])
            precast(key, cx.b_pool[l], cx.pool_w[l])
            if l == 0:
                precast(key, cx.b_router, cx.router_w)
        elif kind == "e":
            for oc in range(8):
                precast(key, cx.b_gate[l, oc], cx.w_gate[l, oc])
                precast(key, cx.b_branch[l, oc], cx.w_branch[l, oc])
            for oc in range(8):
                precast(key, cx.b_out[l, oc], cx.w_out[l, oc])
        else:
            for e in range(NE):
                precast(key, cx.b_wcat[l, e, :, 0:4096].rearrange("p (c n) -> p c n", c=8), cx.w1[l, e])
                precast(key, cx.b_wcat[l, e, :, 4096:8192].rearrange("p (c n) -> p c n", c=8), cx.w3[l, e])
                precast(key, cx.b_wcat[l, e, :, 8192:12288].rearrange("p (c n) -> p c n", c=4), cx.w2[l, e])

    def precast_later(l):
        if fw.cnt["pe"] > 0:
            nc.gpsimd.wait_ge(fw.sem["pe"], fw.cnt["pe"])
        precast_group(("e", l))
        precast_group(("f", l))
        if l + 1 < DEPTH:
            precast_group(("a", l + 1))
    cx.precast_later = precast_later
    precast_group(("a", 0))

    g = Ctx()
    cx.g = g
    g.vecs = [fw.tile("vecs%d" % l, [128, NV], F32, dma=True, glob=True) for l in range(DEPTH)]
    g.rep = [fw.tile("rep%d" % l, [128, NR], F32, dma=True, glob=True) for l in range(DEPTH)]
    g.ident = fw.tile("ident", [128, 128], BF16, dma=True, glob=True)
    g.identf = fw.tile("identf", [128, 128], F32, dma=True, glob=True)
    g.ones = fw.tile("ones", [128, 128], BF16, glob=True)
    g.onesf = fw.tile("onesf", [128, 64], F32, glob=True)
    g.mod = [fw.tile("mod%d" % l, [128, 48, 2], F32, glob=True) for l in range(DEPTH)]
    g.a1 = [fw.tile("a1_%d" % l, [128, 8, 2], F32, glob=True) for l in range(DEPTH)]
    g.a2 = [fw.tile("a2_%d" % l, [128, 8, 2], F32, glob=True) for l in range(DEPTH)]
    g.esink = [fw.tile("esink%d" % l, [128, 8], F32, glob=True) for l in range(DEPTH)]
    g.Gall = fw.tile("Gall", [128, NCHT, 4], F32, glob=True)
    g.C4all = fw.tile("C4all", [128, NCHT, 4], F32, glob=True)
    g.widx = fw.tile("widx", [128, NSLOT, 4], I32, glob=True)
    g.tokidx = fw.tile("tokidx", [128, NCHT], I32, dma=True, glob=True)
    g.sel4 = fw.tile("sel4", [4, 4, 128], F32, dma=True, glob=True)
    fw.dma(g.tokidx[:, :], cx.tokidx, g.tokidx)
    zs = DSem(fw.ges.enter_context(nc.semaphore(fw._name("zs"))))
    nc.sync.dma_start(out=cx.h2tm[NTT:NTT + 128, :], in_=cx.zrow).then_inc(zs.h, 16)
    zs.total = 16
    fw.glob_dsems.append(zs)
    fw.dma(g.sel4[:, :, :], cx.sel4, g.sel4)
    for l in range(DEPTH):
        fw.dma(g.vecs[l][:, :], cx.vecs[l], g.vecs[l])
        fw.dma(g.rep[l][:, :], cx.rep[l], g.rep[l])
    fw.dma(g.ident[:, :], cx.ident, g.ident)
    fw.dma(g.identf[:, :], cx.identf, g.identf)
    fw.op("dve", lambda: nc.vector.memset(g.ones[:, :], 1.0), W=[g.ones])
    fw.op("dve", lambda: nc.vector.memset(g.onesf[:, :], 1.0), W=[g.onesf])
    g.negone = fw.tile("negone", [128, 512], F32, glob=True)
    fw.op("dve", lambda: nc.vector.memset(g.negone[:, :], -1.0), W=[g.negone])
    for l in range(DEPTH):
        fw.op("act", lambda l=l: nc.scalar.activation(out=g.esink[l][:, :], in_=g.rep[l][:, R_SINK:R_SINK + 8], func=AF.Exp),
              R=[g.rep[l]], W=[g.esink[l]])

    class _Stop(Exception):
        pass

    def run(name, fn, *a, **k):
        fn(*a, **k)
        if stop is not None and name == stop:
            raise _Stop()

    try:
        for l in range(DEPTH):
            last = (l == DEPTH - 1)
            run("mod%d" % l, phase_mod, fw, cx, l)
            run("Actx%d" % l, phase_A, fw, cx, l, CTX, kv_only=last)
            run("A%d" % l, phase_A, fw, cx, l, LAT, kv_only=False)
            run("B%d" % l, phase_B, fw, cx, l, LAT)
            if not last:
                phase_B(fw, cx, l, CTX)
            precast_later(l)
            run("Cb%d" % l, phase_C, fw, cx, l, "b", ctx_too=not last)
            run("Cc%d" % l, phase_C, fw, cx, l, "c", ctx_too=not last)
            run("D%d" % l, phase_D, fw, cx, l, LAT)
            if not last:
                phase_D(fw, cx, l, CTX)
            run("E%d" % l, phase_E, fw, cx, l, LAT)
            if not last:
                phase_E(fw, cx, l, CTX)
            run("R%d" % l, phase_R, fw, cx, l, with_ctx=not last)
            run("F%d" % l, phase_F2, fw, cx, l, with_ctx=not last)
            run("G%d" % l, phase_G, fw, cx, l, LAT, final=last)
            if not last:
                phase_G(fw, cx, l, CTX, final=False)
    except _Stop:
        pass
    fw.phase("end")
    fw.barrier()
    fw.ges.close()
    _NC["marks"] = fw.marks
    return nc


def phase_mod(fw, cx, l0):
    nc = fw.nc
    g = cx.g
    fw.phase("mod%d" % l0)
    c2 = fw.tile("c2", [128, 8, 2], F32, dma=True)
    sc = fw.tile("sc", [128, 8, 2], BF16)
    wa = fw.ring("wa", 2, [128, 8, 512], BF16, dma=True)
    mps = {l0: fw.ptile("modps", [128, 48, 2])}
    fw.dma(c2[:, :, :], cx.c2, c2)
    fw.op("act", lambda: nc.scalar.activation(out=sc[:, :, :], in_=c2[:, :, :], func=AF.Silu), R=[c2], W=[sc])
    for l in (l0,):
        def load(jb, l=l):
            t = wa[(l * 12 + jb) % 2]
            fw.dma(t[:, :, :], cx.b_ada[l, jb], t, deps=cx.pdep(("a", l)))
            return t

        def comp(jb, t, l=l):
            for jc in range(4):
                for k in range(8):
                    fw.op("pe", lambda jc=jc, k=k: nc.tensor.matmul(
                        mps[l][:, jb * 4 + jc, :], t[:, k, jc * 128:(jc + 1) * 128], sc[:, k, :],
                        start=(k == 0), stop=(k == 7)), R=[t, sc], W=[mps[l]], sig=(k == 7))
        prefetch_loop(12, load, comp)
        v = g.vecs[l]
        fw.op("dve", lambda l=l, v=v: nc.vector.tensor_tensor(
            g.mod[l][:, :, :], mps[l][:, :, :], v[:, V_BA:V_BA + 48].unsqueeze(2).to_broadcast([128, 48, 2]), ALU.add),
            R=[mps[l], v], W=[g.mod[l]])
        for (a, nb, msc) in ((g.a1[l], V_N1, M_SC1), (g.a2[l], V_N2, M_SC2)):
            fw.op("dve", lambda a=a, msc=msc, l=l: nc.vector.tensor_scalar(
                a[:, :, :], g.mod[l][:, msc:msc + 8, :], 1.0, None, ALU.add), R=[g.mod[l]], W=[a])
            fw.op("dve", lambda a=a, nb=nb, v=v: nc.vector.tensor_tensor(
                a[:, :, :], a[:, :, :], v[:, nb:nb + 8].unsqueeze(2).to_broadcast([128, 8, 2]), ALU.mult),
                R=[a, v], W=[a])
    fw.barrier()


def norm_mod(fw, cx, xt, hT, W, scr, a, bsh_tile, bsh_col, n, ss_ps, veng="dve"):
    nc = fw.nc
    g = cx.g
    sq, rt, tmp = scr["sq"], scr["rt"], scr["tmp"]
    fw.op("act", lambda: nc.scalar.activation(out=sq[:, :, 0:W], in_=xt[:, :, 0:W], func=AF.Square), R=[xt], W=[sq])
    for ch in range(8):
        fw.op("pe", lambda ch=ch: nc.tensor.matmul(ss_ps[:, 0:W], g.ones[:, :], sq[:, ch, 0:W], start=(ch == 0), stop=(ch == 7)),
              R=[g.ones, sq], W=[ss_ps], sig=(ch == 7))
    fw.op("act", lambda: nc.scalar.activation(out=rt[:, 0:W], in_=ss_ps[:, 0:W], func=AF.Sqrt, bias=EPS, scale=1.0 / D),
          R=[ss_ps], W=[rt])
    if veng == "dve":
        fw.op("dve", lambda: nc.vector.reciprocal(rt[:, 0:W], rt[:, 0:W]), R=[rt], W=[rt])
        fw.op("dve", lambda: nc.vector.tensor_tensor(tmp[:, :, 0:W], xt[:, :, 0:W],
                                                      rt[:, 0:W].unsqueeze(1).to_broadcast([128, 8, W]), ALU.mult),
              R=[xt, rt], W=[tmp])
    else:
        fw.op("pool", lambda: nc.gpsimd.tensor_tensor(rt[:, 0:W], rt[:, 0:W], g.negone[:, 0:W], ALU.pow), R=[rt, g.negone], W=[rt])
        fw.op("pool", lambda: nc.gpsimd.tensor_tensor(tmp[:, :, 0:W], xt[:, :, 0:W],
                                                       rt[:, 0:W].unsqueeze(1).to_broadcast([128, 8, W]), ALU.mult),
              R=[xt, rt], W=[tmp])
    for ch in range(8):
        fw.op("act", lambda ch=ch: nc.scalar.activation(
            out=hT[:, ch, 0:W], in_=tmp[:, ch, 0:W], func=AF.Identity,
            bias=bsh_tile[:, bsh_col + ch, n:n + 1], scale=a[:, ch, n:n + 1]),
            R=[tmp, a, bsh_tile], W=[hT])


def phase_A(fw, cx, l, s, kv_only):
    nc = fw.nc
    g = cx.g
    sc = cx.__dict__[s.name]
    NB, nsub = s.NB, s.nsub
    fw.phase("A%d%s" % (l, s.name))
    win = fw.tile("win", [128, 8, 2560], BF16, dma=True)
    fw.dma(win[:, :, :], cx.b_in[l], win, deps=cx.pdep(("a", l)))
    csm = fw.tile("csm", [128, 256], BF16, dma=True)
    fw.dma(csm[:, :], cx.csm, csm)
    rcs = None
    if s.rope:
        rcs = fw.tile("rcs", [128, 32, 64], F32, dma=True)
        fw.dma(rcs[:, :, :], cx.rope_cs, rcs)
    xts = fw.ring("xt", 2, [128, 8, NB], F32, dma=True)
    hTs = fw.ring("hT", 1, [128, 8, NB], BF16, dma=True)
    scr = {"sq": fw.tile("sq", [128, 8, NB], BF16), "rt": fw.tile("rt", [128, NB], F32),
           "tmp": None}
    fT = fw.tile("fT", [128, 4, NB], BF16)
    pTs = fw.ring("pT", 2, [128, NB], F32, dma=True)
    ABt = fw.ring("ABt", 1, [128, nsub, 1024], BF16, dma=True)
    qTb = {m: fw.ring("qT" + m, 1, [128, 4, NB], BF16, dma=True) for m in "bc"}
    kTb = fw.ring("kTb", 1, [128, 4, NB], BF16, dma=True)
    vt = {m: fw.ring("vt" + m, 2, [128, nsub, 130], BF16, dma=True) for m in "bc"}
    for m in "bc":
        for t in vt[m]:
            fw.op("dve", lambda t=t: nc.vector.memset(t[:, :, :], 1.0), W=[t])
    buf = {m: fw.ring("buf" + m, 2, [128, 640], F32) for m in "bc"}
    sqb = fw.tile("sqb", [128, 640], F32)
    ssbs = fw.ring("ssb", 2, [128, 10], F32)
    rot = {m: fw.ring("rot" + m, 2, [128, 640], BF16) for m in "bc"}
    kdup = {m: fw.ring("kdup" + m, 2, [128, 2, 2, 64], BF16) for m in "bc"}
    rtmp = [fw.tile("rtmp%d" % i, [128, 320], F32) for i in range(4)]
    ss_ps = fw.ptile("ss_ps", [128, 512])
    pj_ps = fw.pring("pj_ps", 2, [128, 512])
    q_ps = {m: fw.ptile("q_ps" + m, [128, 512]) for m in "bc"}
    kv_ps = fw.ptile("kv_ps", [128, 512])
    tp_ps = fw.pring("tp_ps", 2, [128, 4, 128], BF16)
    a1 = g.a1[l]
    n = s.n
    xin = sc.x[l].rearrange("(c p) t -> p c t", p=128)
    hTd = sc.hT.rearrange("(c p) t -> p c t", p=128)
    pjc = [0]

    def load(blk):
        xt = xts[blk % 2]
        fw.dma(xt[:, :, :], xin[:, :, blk * NB:(blk + 1) * NB], xt)
        return xt

    def rope(m, tch, par):
        src, dst = buf[m][par], rot[m][par]
        if not s.rope:
            fw.op("dve", lambda: nc.vector.tensor_copy(dst[:, :], src[:, :]), R=[src], W=[dst])
            return
        sv = src[:, :].rearrange("p (h a f e) -> p h a f e", h=10, a=2, f=2, e=16)
        dv = dst[:, :].rearrange("p (h a f e) -> p h a f e", h=10, a=2, f=2, e=16)
        x1, x2 = sv[:, :, :, 0, :], sv[:, :, :, 1, :]
        cosb = rcs[:, tch, 0:32].rearrange("p (a e) -> p a e", a=2).unsqueeze(1).to_broadcast([128, 10, 2, 16])
        sinb = rcs[:, tch, 32:64].rearrange("p (a e) -> p a e", a=2).unsqueeze(1).to_broadcast([128, 10, 2, 16])
        tv = [t[:, :].rearrange("p (h a e) -> p h a e", h=10, a=2, e=16) for t in rtmp]
        fw.op("dve", lambda: nc.vector.tensor_tensor(tv[0], x1, cosb, ALU.mult), R=[src, rcs], W=[rtmp[0]])
        fw.op("dve", lambda: nc.vector.tensor_tensor(tv[1], x2, sinb, ALU.mult), R=[src, rcs], W=[rtmp[1]])
        fw.op("dve", lambda: nc.vector.tensor_tensor(dv[:, :, :, 0, :], tv[0], tv[1], ALU.subtract),
              R=[rtmp[0], rtmp[1]], W=[dst])
        fw.op("dve", lambda: nc.vector.tensor_tensor(tv[2], x2, cosb, ALU.mult), R=[src, rcs], W=[rtmp[2]])
        fw.op("dve", lambda: nc.vector.tensor_tensor(tv[3], x1, sinb, ALU.mult), R=[src, rcs], W=[rtmp[3]])
        fw.op("dve", lambda: nc.vector.tensor_tensor(dv[:, :, :, 1, :], tv[2], tv[3], ALU.add),
              R=[rtmp[2], rtmp[3]], W=[dst])

    def comp(blk, xt):
        hT = hTs[0]
        scr["tmp"] = xt
        norm_mod(fw, cx, xt, hT, NB, scr, a1, g.mod[l], M_SH1, n, ss_ps)
        c0, c1 = blk * NB, (blk + 1) * NB
        if not kv_only:
            fw.dma(hTd[:, :, c0:c1], hT[:, :, :], hT, store=True)
            for oc in range(8):
                ps = pj_ps[pjc[0] % 2]
                pjc[0] += 1
                for k in range(8):
                    fw.op("pe", lambda k=k, oc=oc, ps=ps: nc.tensor.matmul(
                        ps[:, 0:NB], win[:, k, oc * 128:(oc + 1) * 128], hT[:, k, :], start=(k == 0), stop=(k == 7)),
                        R=[win, hT], W=[ps], sig=(k == 7))
                if oc < 4:
                    fw.op("act", lambda oc=oc, ps=ps: nc.scalar.copy(fT[:, oc, :], ps[:, 0:NB]), R=[ps], W=[fT])
                else:
                    pT = pTs[oc % 2]
                    fw.op("act", lambda ps=ps, pT=pT: nc.scalar.copy(pT[:, :], ps[:, 0:NB]), R=[ps], W=[pT])
                    fw.dma(sc.pinT[(oc - 4) * 128:(oc - 3) * 128, c0:c1], pT[:, :], pT, store=True)
            abt = ABt[0]
            for sub in range(nsub):
                for gp in range(2):
                    ps = pj_ps[pjc[0] % 2]
                    pjc[0] += 1
                    for gg in range(2):
                        gi = gp * 2 + gg
                        fw.op("pe", lambda gg=gg, gi=gi, ps=ps, sub=sub: nc.tensor.matmul(
                            ps[:, gg * 256:(gg + 1) * 256], fT[:, gi, sub * 128:(sub + 1) * 128], csm[:, :],
                            start=True, stop=True), R=[fT, csm], W=[ps], sig=(gg == 1))
                    fw.op("dve", lambda gp=gp, ps=ps, sub=sub: nc.vector.tensor_copy(
                        abt[:, sub, gp * 512:(gp + 1) * 512], ps[:, :]), R=[ps], W=[abt])
            fw.dma(sc.AB.rearrange("(s p) n -> p s n", p=128)[:, blk * nsub:(blk + 1) * nsub, :], abt[:, :, :], abt, store=True)
        kb = kTb[0]

        def stage1(sub):
            t0, t1 = sub * 128, (sub + 1) * 128
            par = (blk * nsub + sub) % 2
            groups = [(kv_ps, 0, 256, 1536), (kv_ps, 256, 256, 2304)]
            if not kv_only:
                groups = [(q_ps["b"], 0, 512, 1024), (q_ps["c"], 0, 512, 1792)] + groups
            for (ps, o0, w, wc) in groups:
                for k in range(8):
                    fw.op("pe", lambda: nc.tensor.matmul(
                        ps[:, o0:o0 + w], hT[:, k, t0:t1], win[:, k, wc:wc + w], start=(k == 0), stop=(k == 7)),
                        R=[hT, win], W=[ps], sig=(k == 7))
            for m, kc0 in (("b", 0), ("c", 256)):
                bf_ = buf[m][par]
                if not kv_only:
                    fw.op("act", lambda: nc.scalar.copy(bf_[:, 0:512], q_ps[m][:, :]), R=[q_ps[m]], W=[bf_])
                fw.op("act", lambda: nc.scalar.copy(bf_[:, 512:640], kv_ps[:, kc0:kc0 + 128]), R=[kv_ps], W=[bf_])
                vtt = vt[m][blk % 2]
                fw.op("act", lambda: nc.scalar.copy(
                    vtt[:, sub, :].rearrange("p (g e) -> p g e", e=65)[:, :, 0:64],
                    kv_ps[:, kc0 + 128:kc0 + 256].rearrange("p (g e) -> p g e", e=64)), R=[kv_ps], W=[vtt])

        def stage2(sub):
            t0, t1 = sub * 128, (sub + 1) * 128
            tch = blk * nsub + sub
            par = tch % 2
            bb = buf["b"][par]
            ssb = ssbs[par]
            fw.op("dve", lambda: nc.vector.tensor_tensor(sqb[:, :], bb[:, :], bb[:, :], ALU.mult), R=[bb], W=[sqb])
            fw.op("dve", lambda: nc.vector.reduce_sum(ssb[:, :], sqb[:, :].rearrange("p (h e) -> p h e", e=64), axis=AX.X),
                  R=[sqb], W=[ssb])
            fw.op("act", lambda: nc.scalar.activation(out=ssb[:, :], in_=ssb[:, :], func=AF.Sqrt, bias=EPS, scale=1.0 / 64),
                  R=[ssb], W=[ssb])
            fw.op("dve", lambda: nc.vector.reciprocal(ssb[:, :], ssb[:, :]), R=[ssb], W=[ssb])

        def stage2b(sub):
            t0, t1 = sub * 128, (sub + 1) * 128
            tch = blk * nsub + sub
            par = tch % 2
            bb = buf["b"][par]
            ssb = ssbs[par]
            fw.op("dve", lambda: nc.vector.tensor_tensor(
                bb[:, :].rearrange("p (h e) -> p h e", e=64), bb[:, :].rearrange("p (h e) -> p h e", e=64),
                ssb[:, :].unsqueeze(2).to_broadcast([128, 10, 64]), ALU.mult), R=[bb, ssb], W=[bb])
            fw.op("dve", lambda: nc.vector.tensor_tensor(bb[:, :], bb[:, :], g.rep[l][:, R_GAIN:R_GAIN + 640], ALU.mult),
                  R=[bb, g.rep[l]], W=[bb])
            for m in "bc":
                rope(m, tch, par)
                kd = kdup[m][par]
                for dd in range(2):
                    fw.op("dve", lambda: nc.vector.tensor_copy(
                        kd[:, :, dd, :], rot[m][par][:, 512:640].rearrange("p (g e) -> p g e", e=64)), R=[rot[m][par]], W=[kd])
            if not kv_only:
                for mi, m in enumerate("bc"):
                    tp = tp_ps[mi]
                    for j in range(4):
                        fw.op("pe", lambda: nc.tensor.transpose(
                            tp[:, j, :], rot[m][par][:, j * 128:(j + 1) * 128], g.ident[:, :]),
                            R=[rot[m][par], g.ident], W=[tp], sig=(j == 3))
                    qq = qTb[m][0]
                    fw.op("act", lambda: nc.scalar.copy(qq[:, :, t0:t1], tp[:, :, :]), R=[tp], W=[qq])
            tp = tp_ps[0]
            for mi, m in enumerate("bc"):
                for gi in range(2):
                    j = mi * 2 + gi
                    fw.op("pe", lambda: nc.tensor.transpose(
                        tp[:, j, :], kdup[m][par][:, gi, :, :].rearrange("p d e -> p (d e)"), g.ident[:, :]),
                        R=[kdup[m][par], g.ident], W=[tp], sig=(j == 3))
            fw.op("act", lambda: nc.scalar.copy(kb[:, :, t0:t1], tp[:, :, :]), R=[tp], W=[kb])

        stage1(0)
        for sub in range(nsub):
            stage2(sub)
            if sub + 1 < nsub:
                stage1(sub + 1)
            stage2b(sub)
        k0 = s.koff + c0
        if not kv_only:
            for m in "bc":
                qq = qTb[m][0]
                fw.dma(sc.qT[m].rearrange("(j p) t -> p j t", p=128)[:, :, c0:c1], qq[:, :, :], qq, store=True)
        for mi, m in enumerate("bc"):
            fw.dma(cx.kT[m].rearrange("g p t -> p g t")[:, :, k0:k0 + NB], kb[:, mi * 2:mi * 2 + 2, :], kb, store=True)
            vtt = vt[m][blk % 2]
            fw.dma(cx.v[m][k0:k0 + NB, :].rearrange("(s p) n -> p s n", p=128), vtt[:, :, :], vtt, store=True)

    prefetch_loop(s.nblk, load, comp)
    fw.barrier()


def phase_B(fw, cx, l, s):
    nc = fw.nc
    sc = cx.__dict__[s.name]
    L = s.L
    KB = min(512, L)
    nkb = L // KB
    ntc = L // 128
    TG = min(4, ntc)
    ntg = ntc // TG
    fw.phase("B%d%s" % (l, s.name))
    AB = fw.tile("AB", [128, ntc, 1024], BF16, dma=True)
    abd = sc.AB.rearrange("(s p) n -> p s n", p=128)
    npc = max(1, ntc // 8)
    for i in range(0, ntc, npc):
        fw.dma(AB[:, i:i + npc, :], abd[:, i:i + npc, :], AB)
    Ct = fw.ring("Ct", 3, [128, TG, KB], BF16, dma=True)
    St = fw.ring("St", 3, [128, TG, KB], BF16, dma=True)
    oa = fw.ring("oa", 2, [128, 4, KB], BF16, dma=True)
    acc = fw.pring("acc", 8, [128, 512])
    Cd = (cx.dftC if s is LAT else cx.dftCc).rearrange("(c p) k -> p c k", p=128)
    Sd = (cx.dftS if s is LAT else cx.dftSc).rearrange("(c p) k -> p c k", p=128)
    items = [(kb, tg) for kb in range(nkb) for tg in range(ntg)]

    def load(i):
        kb, tg = items[i]
        c, st = Ct[i % 3], St[i % 3]
        fw.dma(c[:, :, :], Cd[:, tg * TG:(tg + 1) * TG, kb * KB:(kb + 1) * KB], c)
        fw.dma(st[:, :, :], Sd[:, tg * TG:(tg + 1) * TG, kb * KB:(kb + 1) * KB], st)
        return (c, st)

    def comp(i, cs):
        kb, tg = items[i]
        c, st = cs
        ac = acc[(kb % 2) * 4:(kb % 2) * 4 + 4]
        for tcc in range(TG):
            tc = tg * TG + tcc
            for gi in range(4):
                fw.op("pe", lambda gi=gi, tc=tc, tcc=tcc: nc.tensor.matmul(
                    ac[gi][:, 0:KB], AB[:, tc, gi * 256:gi * 256 + 128], c[:, tcc, :], start=(tc == 0), stop=False),
                    R=[AB, c], W=[ac[gi]], sig=False)
                last = (tc == ntc - 1)
                fw.op("pe", lambda gi=gi, tc=tc, tcc=tcc, last=last: nc.tensor.matmul(
                    ac[gi][:, 0:KB], AB[:, tc, gi * 256 + 128:gi * 256 + 256], st[:, tcc, :], start=False, stop=last),
                    R=[AB, st], W=[ac[gi]], sig=(last or gi == 3))
        if tg == ntg - 1:
            o = oa[kb % 2]
            for gi in range(4):
                eng = "act" if gi % 2 == 0 else "dve"
                if eng == "act":
                    fw.op("act", lambda gi=gi: nc.scalar.copy(o[:, gi, :], ac[gi][:, 0:KB]), R=[ac[gi]], W=[o])
                else:
                    fw.op("dve", lambda gi=gi: nc.vector.tensor_copy(o[:, gi, :], ac[gi][:, 0:KB]), R=[ac[gi]], W=[o])
            fw.dma(sc.br[0].rearrange("(g p) t -> p g t", p=128)[:, :, kb * KB:(kb + 1) * KB], o[:, :, :], o, store=True)

    prefetch_loop(len(items), load, comp)
    fw.barrier()


def phase_C(fw, cx, l, m, ctx_too):
    nc = fw.nc
    g = cx.g
    fw.phase("C%d%s" % (l, m))
    nkc = NK // 128
    KT = fw.tile("KT", [128, 2, NK], BF16, dma=True)
    V = fw.tile("V", [128, nkc, 130], BF16, dma=True)
    ktd = cx.kT[m].rearrange("g p t -> p g t")
    for i in range(2):
        fw.dma(KT[:, i, :], ktd[:, i, :], KT)
    fw.dma(V[:, :, :], cx.v[m].rearrange("(s p) n -> p s n", p=128), V)
    wm = None
    if m == "c":
        wm = fw.tile("wm", [128, 6, 512], BF16, dma=True)
        fw.dma(wm[:, :, :], cx.wmask, wm)
    Qs = fw.ring("Q", 2, [128, 512], BF16, dma=True)
    Ps = fw.ring("P", 8, [128, 512], BF16)
    accs = fw.ring("accs", 4, [128, 512], F32)
    rec = fw.ring("rec", 4, [128, 512], F32)
    ob = fw.ring("ob", 4, [64, 512], BF16, dma=True)
    S_ps = fw.pring("S", 4, [128, 512])
    acc_ps = fw.pring("acc", 2, [128, 512])
    bc_ps = fw.pring("bc", 2, [64, 512])
    cnt = {"s": 0, "p": 0, "o": 0, "a": 0}
    deferred = []

    seqs = [LAT] + ([CTX] if ctx_too else [])
    items = []
    for s in seqs:
        for j in range(4):
            for qb in range(s.nblk):
                items.append((s, j, qb))

    def load(i):
        s, j, qb = items[i]
        sc = cx.__dict__[s.name]
        q = Qs[i % 2]
        NB = s.NB
        fw.dma(q[:, 0:NB], sc.qT[m][j * 128:(j + 1) * 128, qb * NB:(qb + 1) * NB], q)
        return q

    def comp(i, q):
        s, j, qb = items[i]
        sc = cx.__dict__[s.name]
        NB = s.NB
        gi = j // 2
        if s is CTX:
            kcl = [(0, None), (1, None)]
        elif m == "b":
            kcl = [(kc, None) for kc in range(nkc)]
        else:
            kcl = [(0, None), (1, None)]
            for r in range(-1, 5):
                c = qb * 4 + r
                if 0 <= c < T // 128:
                    kcl.append((2 + c, r + 1))
        nk = len(kcl)

        def qk(ki):
            kc, mk = kcl[ki]
            pp = []
            for hh in range(2):
                sp = S_ps[cnt["s"] % 4]
                cnt["s"] += 1
                r0 = hh * 64
                fw.op("pe", lambda: nc.tensor.matmul(
                    sp[:, 0:NB], KT[r0:r0 + 64, gi, kc * 128:(kc + 1) * 128], q[r0:r0 + 64, 0:NB], start=True, stop=True),
                    R=[KT, q], W=[sp])
                p = Ps[cnt["p"] % 8]
                cnt["p"] += 1
                fw.op("act", lambda: nc.scalar.activation(out=p[:, 0:NB], in_=sp[:, 0:NB], func=AF.Exp, scale=0.125),
                      R=[sp], W=[p])
                if mk is not None:
                    fw.op("dve", lambda: nc.vector.tensor_tensor(p[:, 0:NB], p[:, 0:NB], wm[:, mk, 0:NB], ALU.mult),
                          R=[p, wm], W=[p])
                pp.append(p)
            return pp

        pend = qk(0)
        for ki in range(nk):
            kc = kcl[ki][0]
            nxt = qk(ki + 1) if ki + 1 < nk else None
            for hh in range(2):
                fw.op("pe", lambda: nc.tensor.matmul(
                    acc_ps[hh][0:65, 0:NB], V[:, kc, gi * 65:(gi + 1) * 65], pend[hh][:, 0:NB],
                    start=(ki == 0), stop=(ki == nk - 1)), R=[V, pend[hh]], W=[acc_ps[hh]], sig=True)
            pend = nxt
            if ki == 1 or nk == 1:
                while deferred:
                    deferred.pop(0)()
        for hh in range(2):
            h = 2 * j + hh
            a = accs[cnt["a"] % 4]
            rc = rec[cnt["a"] % 4]
            cnt["a"] += 1
            fw.op("dve", lambda: nc.vector.tensor_copy(a[0:65, 0:NB], acc_ps[hh][0:65, 0:NB]), R=[acc_ps[hh]], W=[a])
            if m == "c":
                fw.op("act", lambda: nc.scalar.activation(out=rc[64:65, 0:NB], in_=a[64:65, 0:NB], func=AF.Ln,
                                                          bias=g.esink[l][64:65, h:h + 1], scale=1.0),
                      R=[a, g.esink[l]], W=[rc])
            else:
                fw.op("act", lambda: nc.scalar.activation(out=rc[64:65, 0:NB], in_=a[64:65, 0:NB], func=AF.Ln),
                      R=[a], W=[rc])
            fw.op("act", lambda: nc.scalar.activation(out=rc[64:65, 0:NB], in_=rc[64:65, 0:NB], func=AF.Exp, scale=-1.0),
                  R=[rc], W=[rc])

            def tail(hh=hh, h=h, a=a, rc=rc, NB=NB, sc=sc, qb=qb):
                fw.op("pe", lambda: nc.tensor.matmul(
                    bc_ps[hh][:, 0:NB], g.onesf[64:65, 0:64], rc[64:65, 0:NB], start=True, stop=True),
                    R=[g.onesf, rc], W=[bc_ps[hh]])
                o = ob[cnt["o"] % 4]
                cnt["o"] += 1
                fw.op("dve", lambda: nc.vector.tensor_tensor(o[:, 0:NB], a[0:64, 0:NB], bc_ps[hh][:, 0:NB], ALU.mult),
                      R=[a, bc_ps[hh]], W=[o])
                bi = 1 if m == "b" else 2
                fw.dma(sc.br[bi][h * 64:(h + 1) * 64, qb * NB:(qb + 1) * NB], o[:, 0:NB], o, store=True)
            deferred.append(tail)

    prefetch_loop(len(items), load, comp)
    while deferred:
        deferred.pop(0)()
    fw.barrier()


def phase_D(fw, cx, l, s):
    nc = fw.nc
    g = cx.g
    sc = cx.__dict__[s.name]
    L, NB = s.L, s.NB
    H = 16
    LL = L + 2 * H
    fw.phase("D%d%s" % (l, s.name))
    pw = fw.tile("pw", [128, 4, 128], BF16, dma=True)
    fw.dma(pw[:, :, :], cx.b_pool[l], pw, deps=cx.pdep(("a", l)))
    P = fw.ring("P", 2, [128, LL], F32, dma=True)
    S1 = fw.tile("S1", [128, LL], F32)
    S2 = fw.tile("S2", [128, LL], F32)
    ic = fw.ring("ic", 2, [128, L], F32, dma=True)
    pl = fw.tile("pl", [128, L], BF16)
    od = fw.ring("od", 2, [128, NB], BF16, dma=True)
    ps = fw.pring("ps", 2, [128, 512])
    icd = cx.invcnt if s is LAT else cx.invcntc
    for t in P:
        fw.op("dve", lambda t=t: nc.vector.memset(t[:, :], 0.0), W=[t])
    cnt = [0]

    def load(gi):
        p = P[gi % 2]
        fw.dma(p[:, H:H + L], sc.pinT[gi * 128:(gi + 1) * 128, :], p)
        fw.dma(ic[gi % 2][:, :], icd[gi], ic[gi % 2])
        return (p, ic[gi % 2])

    def comp(gi, pi):
        p, icn = pi
        fw.op("dve", lambda: nc.vector.tensor_tensor(S1[:, 1:LL], p[:, 0:LL - 1], p[:, 1:LL], ALU.add), R=[p], W=[S1])
        cur, oth = S1, S2
        lo, hi = 1, LL
        w = 2
        for _ in range(gi):
            sh = w // 2
            lo2, hi2 = lo + sh, hi - sh
            fw.op("dve", lambda cur=cur, oth=oth, sh=sh, lo2=lo2, hi2=hi2: nc.vector.tensor_tensor(
                oth[:, lo2:hi2], cur[:, lo2 - sh:hi2 - sh], cur[:, lo2 + sh:hi2 + sh], ALU.add), R=[cur], W=[oth])
            cur, oth = oth, cur
            lo, hi = lo2, hi2
            w *= 2
        assert lo <= H and hi >= H + L
        fw.op("dve", lambda cur=cur, oth=oth: nc.vector.tensor_tensor(oth[:, H:H + L], cur[:, H:H + L], icn[:, :], ALU.mult),
              R=[cur, icn], W=[oth])
        fw.op("dve", lambda oth=oth: nc.vector.tensor_tensor(pl[:, :], oth[:, H:H + L], p[:, H:H + L], ALU.subtract),
              R=[oth, p], W=[pl])
        for blk in range(s.nblk):
            pp = ps[cnt[0] % 2]
            o = od[cnt[0] % 2]
            cnt[0] += 1
            fw.op("pe", lambda blk=blk, pp=pp: nc.tensor.matmul(pp[:, 0:NB], pw[:, gi, :], pl[:, blk * NB:(blk + 1) * NB],
                                                             start=True, stop=True), R=[pw, pl], W=[pp])
            fw.op("act", lambda pp=pp, o=o: nc.scalar.activation(out=o[:, :], in_=pp[:, 0:NB], func=AF.Identity,
                                                              scale=g.vecs[l][:, V_PS + gi:V_PS + gi + 1]),
                  R=[pp, g.vecs[l]], W=[o])
            fw.dma(sc.br[3][gi * 128:(gi + 1) * 128, blk * NB:(blk + 1) * NB], o[:, :], o, store=True)

    prefetch_loop(4, load, comp)
    fw.barrier()


def phase_E(fw, cx, l, s):
    nc = fw.nc
    g = cx.g
    sc = cx.__dict__[s.name]
    SB, NB = s.SB, s.NB
    nb = SB // NB
    n = s.n
    fw.phase("E%d%s" % (l, s.name))
    hTs = fw.ring("hT", 2, [128, 8, SB], BF16, dma=True)
    brs = fw.ring("br", 2, [128, 4, 4, SB], BF16, dma=True)
    xts = fw.ring("xt", 2, [128, 8, SB], F32, dma=True)
    mg = fw.tile("mg", [128, 8, SB], BF16)
    wg = fw.ring("wg", 3, [128, 4, 8, 128], BF16, dma=True)
    wb = fw.ring("wb", 3, [128, 4, 4, 128], BF16, dma=True)
    wo = fw.ring("wo", 3, [128, 8, 128], BF16, dma=True)
    rw = fw.tile("rw", [128, 8, 16], BF16, dma=True)
    fw.dma(rw[:, :, :], cx.b_router, rw, deps=cx.pdep(("a", 0)))
    sig = fw.ring("sig", 2, [128, NB], F32)
    macc = fw.tile("macc", [128, NB], F32)
    mtmp = fw.ring("mtmp", 2, [128, NB], F32)
    scr = {"sq": fw.tile("sq", [128, 8, NB], BF16), "rt": fw.tile("rt", [128, NB], F32),
           "tmp": fw.tile("tmp", [128, 8, NB], F32)}
    h2tm_t = fw.tile("h2tm_t", [128, SB // 128, 1024], BF16, dma=True)
    rt_ = {k: fw.tile("r_" + k, [128, 16], F32) for k in ("aff", "sel", "eq", "s2", "selm", "e1", "e2", "ae", "comb")}
    rs_ = {k: fw.tile("rs_" + k, [128, 4], F32) for k in ("t1", "t2", "gs", "ing")}
    r1_ = {k: fw.tile("r1_" + k, [128, 1], F32) for k in ("gm", "m1", "m2", "sum")}
    gate_ps = fw.pring("gate", 2, [128, 512])
    proj_ps = fw.pring("proj", 2, [128, 512])
    y_ps = fw.pring("y", 1, [128, 512])
    ss_ps = fw.ptile("ss", [128, 512])
    lg_ps = fw.ptile("lg", [128, 512])
    tpE = fw.ptile("tpE", [128, 8, 128], BF16)
    hTd = sc.hT.rearrange("(c p) t -> p c t", p=128)
    h2d = sc.h2T.rearrange("(c p) t -> p c t", p=128)
    xd = sc.x[l].rearrange("(c p) t -> p c t", p=128)
    xmd = sc.xmid.rearrange("(c p) t -> p c t", p=128)
    brd = sc.br.rearrange("i (c p) t -> p i c t", p=128)
    v = g.vecs[l]
    rep = g.rep[l]
    cnt = {"g": 0, "y": 0}
    nsub = SB // 128

    def sb_load(sb, part=None):
        s0, s1 = sb * SB, (sb + 1) * SB
        hT, br, xt = hTs[sb % 2], brs[sb % 2], xts[sb % 2]
        if part is None or part == 4:
            fw.dma(hT[:, :, :], hTd[:, :, s0:s1], hT, q="pool")
        for i in range(4):
            if part is None or part == i:
                fw.dma(br[:, i, :, :], brd[:, i, :, s0:s1], br, q="pool")
        if part is None or part == 5:
            fw.dma(xt[:, :, :], xd[:, :, s0:s1], xt, q="pool")
        return (hT, br, xt)

    def make_tail(sb, hT, xt):
        s0 = sb * SB

        def p_norm():
            for blk in range(nb):
                b0, b1 = blk * NB, (blk + 1) * NB
                norm_mod(fw, cx, _Sub(xt, b0, b1), _Sub(hT, b0, b1), NB, scr, g.a2[l], g.mod[l], M_SH2, n, ss_ps, veng="dve")

        def p_router(sub):
            t0, t1 = sub * 128, (sub + 1) * 128
            while dveq:
                dveq.pop(0)()
            for k in range(8):
                fw.op("pe", lambda: nc.tensor.matmul(lg_ps[:, 0:16], hT[:, k, t0:t1], rw[:, k, :], start=(k == 0), stop=(k == 7)),
                      R=[hT, rw], W=[lg_ps], sig=(k == 7))
            chunk = (sb * nsub + sub) if s is LAT else (T // 128 + sub)
            dveq.extend(router(fw, cx, l, lg_ps, rt_, rs_, r1_, chunk))
            for k in range(8):
                fw.op("pe", lambda: nc.tensor.transpose(tpE[:, k, :], hT[:, k, t0:t1], g.ident[:, :]),
                      R=[hT, g.ident], W=[tpE], sig=(k == 7))
            fw.op("act", lambda: nc.scalar.copy(h2tm_t[:, sub, :], tpE[:, :, :].rearrange("p k n -> p (k n)")), R=[tpE], W=[h2tm_t])

        def p_store():
            r0 = (0 if s is LAT else T) + s0
            fw.dma(cx.h2tm[r0:r0 + SB, :].rearrange("(u p) n -> p u n", p=128), h2tm_t[:, :, :], h2tm_t, store=True)

        pieces = {1: [p_norm]}
        for sub in range(nsub):
            pieces[2 + sub] = [lambda sub=sub: p_router(sub)]
        pieces[2 + nsub - 1].append(p_store)
        return pieces

    cur = [sb_load(0)]
    tail = {}
    dveq = []
    nxt = [None]
    wcnt = {"g": 0, "o": 0}
    items = [(sb, kind, oc) for sb in range(s.nsb) for kind in ("gate", "out") for oc in range(8)]

    def load(i):
        sb, kind, oc = items[i]
        if kind == "gate":
            a, b = wg[wcnt["g"] % 3], wb[wcnt["g"] % 3]
            wcnt["g"] += 1
            fw.dma(a[:, :, :, :], cx.b_gate[l, oc], a, deps=cx.pdep(("e", l)))
            fw.dma(b[:, :, :, :], cx.b_branch[l, oc], b, deps=cx.pdep(("e", l)))
            return (a, b)
        w = wo[wcnt["o"] % 3]
        wcnt["o"] += 1
        fw.dma(w[:, :, :], cx.b_out[l, oc], w, deps=cx.pdep(("e", l)))
        return w

    def comp_gate(sb, oc, ab):
        hT, br, xt = cur[0]
        a, b = ab
        for blk in range(nb):
            b0, b1 = blk * NB, (blk + 1) * NB
            for i in range(4):
                gp = gate_ps[cnt["g"] % 2]
                pp = proj_ps[cnt["g"] % 2]
                sg = sig[cnt["g"] % 2]
                mt = mtmp[cnt["g"] % 2]
                cnt["g"] += 1
                for k in range(8):
                    fw.op("pe", lambda: nc.tensor.matmul(
                        gp[:, 0:NB], a[:, i, k, :], hT[:, k, b0:b1], start=(k == 0), stop=(k == 7)),
                        R=[a, hT], W=[gp], sig=(k == 7))
                for c in range(4):
                    fw.op("pe", lambda: nc.tensor.matmul(
                        pp[:, 0:NB], b[:, i, c, :], br[:, i, c, b0:b1], start=(c == 0), stop=(c == 3)),
                        R=[b, br], W=[pp], sig=(c == 3))
                fw.op("act", lambda: nc.scalar.activation(
                    out=sg[:, :], in_=gp[:, 0:NB], func=AF.Sigmoid, bias=v[:, V_BG + i * 8 + oc:V_BG + i * 8 + oc + 1], scale=1.0),
                    R=[gp, v], W=[sg])
                if i == 0:
                    fw.op("dve", lambda: nc.vector.tensor_tensor(macc[:, :], sg[:, :], pp[:, 0:NB], ALU.mult),
                          R=[sg, pp], W=[macc])
                else:
                    fw.op("dve", lambda: nc.vector.tensor_tensor(mt[:, :], sg[:, :], pp[:, 0:NB], ALU.mult),
                          R=[sg, pp], W=[mt])
                    if i < 3:
                        fw.op("dve", lambda: nc.vector.tensor_tensor(macc[:, :], macc[:, :], mt[:, :], ALU.add),
                              R=[macc, mt], W=[macc])
                    else:
                        fw.op("dve", lambda: nc.vector.tensor_tensor(mg[:, oc, b0:b1], macc[:, :], mt[:, :], ALU.add),
                              R=[macc, mt], W=[mg])
                for _ in range(6):
                    if dveq:
                        dveq.pop(0)()
        for f in tail.pop(oc, []):
            f()
        if sb + 1 < s.nsb:
            part = {1: 0, 2: 1, 3: 2, 4: 3, 6: 4, 7: 5}.get(oc)
            if part is not None:
                nxt[0] = sb_load(sb + 1, part)

    def comp_out(sb, oc, w):
        hT, br, xt = cur[0]
        for blk in range(nb):
            b0, b1 = blk * NB, (blk + 1) * NB
            yring = [y_ps[0], gate_ps[0], proj_ps[0], gate_ps[1], proj_ps[1]]
            yp = yring[cnt["y"] % 5]
            cnt["y"] += 1
            for k in range(8):
                fw.op("pe", lambda: nc.tensor.matmul(
                    yp[:, 0:NB], w[:, k, :], mg[:, k, b0:b1], start=(k == 0), stop=(k == 7)),
                    R=[w, mg], W=[yp], sig=(k == 7))
            fw.op("dve", lambda: nc.vector.scalar_tensor_tensor(
                xt[:, oc, b0:b1], yp[:, 0:NB], g.mod[l][:, M_G1 + oc, n:n + 1], xt[:, oc, b0:b1], ALU.mult, ALU.add),
                R=[yp, g.mod[l], xt], W=[xt])
        if oc == 7:
            s0, s1 = sb * SB, (sb + 1) * SB
            assert not tail
            fw.dma(xmd[:, :, s0:s1], xt[:, :, :], xt, store=True, q="pool")
            tail.update(make_tail(sb, hT, xt))
            cur[0] = nxt[0]

    def comp(i, ws):
        sb, kind, oc = items[i]
        if kind == "gate":
            comp_gate(sb, oc, ws)
        else:
            comp_out(sb, oc, ws)

    prefetch_loop(len(items), load, comp, depth=2)
    for oc in sorted(tail):
        for f in tail[oc]:
            f()
    while dveq:
        dveq.pop(0)()
    fw.barrier()


class _Sub:
    def __init__(self, t, b0, b1):
        self.__dict__["t"] = t
        self.__dict__["b0"] = b0
        self.__dict__["b1"] = b1

    def __getitem__(self, k):
        k = list(k)
        sl = k[-1]
        start = 0 if sl.start is None else sl.start
        stop = (self.b1 - self.b0) if sl.stop is None else sl.stop
        k[-1] = slice(self.b0 + start, self.b0 + stop)
        return self.t.h[tuple(k)]

    def __getattr__(self, a):
        return getattr(self.t, a)

    def __setattr__(self, a, v):
        setattr(self.t, a, v)


def router(fw, cx, l, lg_ps, rt_, rs_, r1_, chunk):
    nc = fw.nc
    rep = cx.g.rep[l]
    aff, sel, eq, s2, selm, e1, e2, ae, comb = (rt_[k] for k in ("aff", "sel", "eq", "s2", "selm", "e1", "e2", "ae", "comb"))
    t1, t2, gs, ing = (rs_[k] for k in ("t1", "t2", "gs", "ing"))
    gm, m1, m2, sm = (r1_[k] for k in ("gm", "m1", "m2", "sum"))
    V = nc.vector
    ops = []

    def g4(t):
        return t[:, :].rearrange("p (g e) -> p g e", e=4)

    def b16(t):
        return t[:, :].unsqueeze(2).to_broadcast([128, 4, 4])

    def c16(t):
        return t[:, 0:1].to_broadcast([128, 16])

    fw.op("act", lambda: nc.scalar.activation(out=aff[:, :], in_=lg_ps[:, 0:16], func=AF.Sigmoid), R=[lg_ps], W=[aff])
    ops.append(lambda: fw.op("dve", lambda: V.tensor_tensor(sel[:, :], aff[:, :], rep[:, R_RB:R_RB + 16], ALU.add), R=[aff, rep], W=[sel]))
    ops.append(lambda: fw.op("dve", lambda: V.tensor_reduce(t1[:, :], g4(sel), axis=AX.X, op=ALU.max), R=[sel], W=[t1]))
    ops.append(lambda: fw.op("dve", lambda: V.tensor_tensor(g4(eq), g4(sel), b16(t1), ALU.is_equal), R=[sel, t1], W=[eq]))
    ops.append(lambda: fw.op("dve", lambda: V.scalar_tensor_tensor(s2[:, :], eq[:, :], -BIG, sel[:, :], ALU.mult, ALU.add), R=[eq, sel], W=[s2]))
    ops.append(lambda: fw.op("dve", lambda: V.tensor_reduce(t2[:, :], g4(s2), axis=AX.X, op=ALU.max), R=[s2], W=[t2]))
    ops.append(lambda: fw.op("dve", lambda: V.tensor_tensor(gs[:, :], t1[:, :], t2[:, :], ALU.add), R=[t1, t2], W=[gs]))
    ops.append(lambda: fw.op("dve", lambda: V.tensor_reduce(gm[:, :], gs[:, :], axis=AX.X, op=ALU.max), R=[gs], W=[gm]))
    ops.append(lambda: fw.op("dve", lambda: V.tensor_tensor(ing[:, :], gs[:, :], gm[:, 0:1].to_broadcast([128, 4]), ALU.is_equal), R=[gs, gm], W=[ing]))
    Gall, C4all = cx.g.Gall, cx.g.C4all
    ops.append(lambda: fw.op("dve", lambda: V.tensor_copy(Gall[:, chunk, :], ing[:, :]), R=[ing], W=[Gall]))
    ops.append(lambda: fw.op("dve", lambda: V.tensor_scalar(ing[:, :], ing[:, :], 1.0, BIG, ALU.subtract, ALU.mult), R=[ing], W=[ing]))
    ops.append(lambda: fw.op("dve", lambda: V.tensor_tensor(g4(selm), g4(sel), b16(ing), ALU.add), R=[sel, ing], W=[selm]))
    ops.append(lambda: fw.op("dve", lambda: V.tensor_reduce(m1[:, :], selm[:, :], axis=AX.X, op=ALU.max), R=[selm], W=[m1]))
    ops.append(lambda: fw.op("dve", lambda: V.tensor_tensor(e1[:, :], selm[:, :], c16(m1), ALU.is_equal), R=[selm, m1], W=[e1]))
    ops.append(lambda: fw.op("dve", lambda: V.scalar_tensor_tensor(s2[:, :], e1[:, :], -BIG, selm[:, :], ALU.mult, ALU.add), R=[e1, selm], W=[s2]))
    ops.append(lambda: fw.op("dve", lambda: V.tensor_reduce(m2[:, :], s2[:, :], axis=AX.X, op=ALU.max), R=[s2], W=[m2]))
    ops.append(lambda: fw.op("dve", lambda: V.tensor_tensor(e2[:, :], s2[:, :], c16(m2), ALU.is_equal), R=[s2, m2], W=[e2]))
    ops.append(lambda: fw.op("dve", lambda: V.tensor_tensor(e1[:, :], e1[:, :], e2[:, :], ALU.add), R=[e1, e2], W=[e1]))
    ops.append(lambda: fw.op("dve", lambda: V.tensor_tensor(ae[:, :], aff[:, :], e1[:, :], ALU.mult), R=[aff, e1], W=[ae]))
    ops.append(lambda: fw.op("dve", lambda: V.tensor_reduce(sm[:, :], ae[:, :], axis=AX.X, op=ALU.add), R=[ae], W=[sm]))
    ops.append(lambda: fw.op("dve", lambda: V.reciprocal(sm[:, :], sm[:, :]), R=[sm], W=[sm]))
    ops.append(lambda: fw.op("dve", lambda: V.tensor_scalar(comb[:, :], ae[:, :], sm[:, 0:1], None, ALU.mult), R=[ae, sm], W=[comb]))
    ops.append(lambda: fw.op("dve", lambda: V.tensor_reduce(C4all[:, chunk, :], comb[:, :].rearrange("p (g j) -> p j g", j=4), axis=AX.X, op=ALU.add),
          R=[comb], W=[C4all]))
    return ops


def phase_R(fw, cx, l, with_ctx):
    nc = fw.nc
    g = cx.g
    V = nc.vector
    NCH = NCHT if with_ctx else T // 128
    NS = NSLOT if with_ctx else NSLOT - 1
    W = NCH * 4
    fw.phase("R%d" % l)
    pf = DSem(fw.ges.enter_context(nc.semaphore(fw._name("pf"))))
    nc.sync.dma_start(out=cx.permD, in_=cx.oobfill).then_inc(pf.h, 16)
    nc.sync.dma_start(out=cx.c4D, in_=cx.zero4).then_inc(pf.h, 16)
    pf.total = 32
    pfdep = [(id(pf), pf.h, pf.total, "dma")]
    us = fw.tile("us", [128, 128], BF16, dma=True)
    th9 = fw.tile("th9", [128, 4, 9], F32, dma=True)
    sio = fw.tile("sio", [128, NSLOT, 3], F32, dma=True)
    jp = fw.tile("jp", [128, 4], F32, dma=True)
    fw.dma(us[:, :], cx.ustrict, us)
    fw.dma(th9[:, :, :], cx.th9, th9)
    fw.dma(sio[:, :, :], cx.siota, sio)
    fw.dma(jp[:, :], cx.jp, jp)
    Gb = fw.tile("Gb", [128, W], BF16)
    ca = fw.tile("ca", [128, NCH, 4], F32)
    cb = fw.tile("cb", [128, NCH, 4], F32)
    cn = fw.tile("cn", [128, NCH, 4], F32)
    posg = fw.tile("posg", [128, NCH, 4], F32)
    pos = fw.tile("pos", [128, NCH], F32)
    posi = fw.tile("posi", [128, NCH], I32)
    ntot = fw.tile("ntot", [128, 4], F32)
    cmp9 = fw.tile("cmp9", [128, 4, 9], F32)
    nblk = fw.tile("nblk", [128, 4], F32)
    bb = fw.tile("bb", [128, 4], F32)
    cmp3 = fw.tile("cmp3", [128, NSLOT, 3], F32)
    gsl = fw.tile("gsl", [128, NSLOT], F32)
    wxf = fw.tile("wxf", [128, NSLOT, 4], F32)
    cnt_ps = fw.ptile("cnt_ps", [128, 512])
    rank_ps = fw.ptile("rank_ps", [128, 512])
    Gall = g.Gall
    Gv = Gall[:, 0:NCH, :]
    fw.op("dve", lambda: V.tensor_copy(Gb[:, :], Gv.rearrange("p c g -> p (c g)")), R=[Gall], W=[Gb])
    fw.op("pe", lambda: nc.tensor.matmul(cnt_ps[:, 0:W], g.ones[:, :], Gb[:, :], start=True, stop=True), R=[g.ones, Gb], W=[cnt_ps])
    fw.op("pe", lambda: nc.tensor.matmul(rank_ps[:, 0:W], us[:, :], Gb[:, :], start=True, stop=True), R=[us, Gb], W=[rank_ps])
    fw.op("dve", lambda: V.tensor_copy(cn[:, :, :].rearrange("p c g -> p (c g)"), cnt_ps[:, 0:W]), R=[cnt_ps], W=[cn])
    fw.op("dve", lambda: V.tensor_copy(ca[:, :, :], cn[:, :, :]), R=[cn], W=[ca])
    a, b = ca, cb
    sh = 1
    while sh < NCH:
        fw.op("dve", lambda a=a, b=b: V.tensor_copy(b[:, 0:sh, :], a[:, 0:sh, :]), R=[a], W=[b])
        fw.op("dve", lambda a=a, b=b: V.tensor_tensor(b[:, sh:NCH, :], a[:, sh:NCH, :], a[:, 0:NCH - sh, :], ALU.add), R=[a], W=[b])
        a, b = b, a
        sh *= 2
    incl = a
    fw.op("dve", lambda: V.tensor_copy(ntot[:, :], incl[:, NCH - 1, :]), R=[incl], W=[ntot])
    fw.op("dve", lambda: V.tensor_tensor(cmp9[:, :, :], ntot[:, :].unsqueeze(2).to_broadcast([128, 4, 9]), th9[:, :, :], ALU.is_gt),
          R=[ntot, th9], W=[cmp9])
    fw.op("dve", lambda: V.tensor_reduce(nblk[:, :], cmp9[:, :, :], axis=AX.X, op=ALU.add), R=[cmp9], W=[nblk])
    fw.op("dve", lambda: V.memset(bb[:, :], 0.0), W=[bb])
    for gi in range(1, 4):
        fw.op("dve", lambda gi=gi: V.tensor_tensor(bb[:, gi:gi + 1], bb[:, gi - 1:gi], nblk[:, gi - 1:gi], ALU.add), R=[bb, nblk], W=[bb])
    fw.op("dve", lambda: V.tensor_tensor(posg[:, :, :], incl[:, :, :], cn[:, :, :], ALU.subtract), R=[incl, cn], W=[posg])
    fw.op("dve", lambda: V.tensor_tensor(posg[:, :, :].rearrange("p c g -> p (c g)"), posg[:, :, :].rearrange("p c g -> p (c g)"),
                                          rank_ps[:, 0:W], ALU.add), R=[posg, rank_ps], W=[posg])
    fw.op("dve", lambda: V.scalar_tensor_tensor(posg[:, :, :], bb[:, :].unsqueeze(1).to_broadcast([128, NCH, 4]), 512.0, posg[:, :, :],
                                                 ALU.mult, ALU.add), R=[bb, posg], W=[posg])
    fw.op("dve", lambda: V.tensor_tensor(posg[:, :, :], posg[:, :, :], Gv, ALU.mult), R=[posg, Gall], W=[posg])
    fw.op("dve", lambda: V.tensor_reduce(pos[:, :], posg[:, :, :], axis=AX.X, op=ALU.add), R=[posg], W=[pos])
    fw.op("dve", lambda: V.tensor_copy(posi[:, :], pos[:, :]), R=[pos], W=[posi])
    fw.op("dve", lambda: V.tensor_tensor(cmp3[:, :, :], sio[:, :, :], bb[:, 1:4].unsqueeze(1).to_broadcast([128, NSLOT, 3]), ALU.is_ge),
          R=[sio, bb], W=[cmp3])
    fw.op("dve", lambda: V.tensor_reduce(gsl[:, :], cmp3[:, :, :], axis=AX.X, op=ALU.add), R=[cmp3], W=[gsl])
    fw.op("dve", lambda: V.scalar_tensor_tensor(wxf[:, :, :], gsl[:, :].unsqueeze(2).to_broadcast([128, NSLOT, 4]), 512.0,
                                                 jp[:, :].unsqueeze(1).to_broadcast([128, NSLOT, 4]), ALU.mult, ALU.add),
          R=[gsl, jp], W=[wxf])
    if l > 0:
        fw.op("dve", lambda: V.tensor_scalar(wxf[:, :, :], wxf[:, :, :], float(l * NE * 128), None, ALU.add), R=[wxf], W=[wxf])
    fw.op("dve", lambda: V.tensor_copy(g.widx[:, :, :], wxf[:, :, :]), R=[wxf], W=[g.widx])
    for c in range(NCH):
        fw.idma(cx.permD[:, :], bass.IndirectOffsetOnAxis(ap=posi[:, c:c + 1], axis=0), g.tokidx[:, c:c + 1], None,
                NS * 512 - 1, g.tokidx, posi, store=True, deps=pfdep)
    c4s = fw.tile("c4s", [128, NCH, 4], F32, dma=True)
    fw.op("dve", lambda: V.tensor_copy(c4s[:, :, :], g.C4all[:, 0:NCH, :]), R=[g.C4all], W=[c4s])
    for c in range(NCH):
        fw.idma(cx.c4D[:, :], bass.IndirectOffsetOnAxis(ap=posi[:, c:c + 1], axis=0), c4s[:, c, :], None,
                NS * 512 - 1, c4s, posi, store=True, deps=pfdep)
    fw.barrier()


def phase_F2(fw, cx, l, with_ctx):
    nc = fw.nc
    g = cx.g
    NS = NSLOT if with_ctx else NSLOT - 1
    NT = NTT if with_ctx else T
    NB = 512
    fw.phase("F%d" % l)
    idxs = fw.ring("idxs", 4, [128, 4], I32, dma=True)
    h2g = fw.ring("h2g", 2, [128, 4, 1024], BF16, dma=True)
    h2s = fw.ring("h2s", 2, [128, 8, NB], BF16, dma=True)
    c4 = fw.ring("c4", 2, [128, 4, 4], F32, dma=True)
    c4T = fw.tile("c4T", [4, NB], F32)
    cbs = fw.ring("cbs", 8, [128, NB], F32, dma=True)
    acc = fw.tile("acc", [128, 8, NB], F32, dma=True)
    om = fw.tile("om", [128, 4, 1024], F32, dma=True)
    wc = fw.ring("wc", 3, [128, 12288], BF16, dma=True)
    sg = fw.ring("sg", 2, [128, NB], F32)
    st = fw.ring("st", 2, [128, NB], F32)
    hid = fw.ring("hid", 2, [128, 4, NB], BF16)
    g_ps = fw.pring("g", 2, [128, 512])
    u_ps = fw.pring("u", 2, [128, 512])
    o_ps = fw.pring("o", 2, [128, 512])
    tp_ps = fw.ptile("tp", [128, 8, 128], BF16)
    m_ps = fw.ptile("m", [128, 512])
    for t in h2g:
        fw.op("dve", lambda t=t: nc.vector.memset(t[:, :, :], 0.0), W=[t])
    wcd = cx.b_wcat.rearrange("l e p n -> (l e p) n")
    permv = cx.permD.rearrange("(s p u) o -> s p (u o)", u=4, p=128)
    c4v = cx.c4D.rearrange("(s p u) j -> s p u j", u=4, p=128)
    cnt = {"g": 0, "o": 0, "h": 0, "w": 0, "cb": 0}
    items = [(sl, j) for sl in range(NS) for j in range(4)]
    slot_state = {}

    def slot_prep(sl):
        ix = idxs[sl % 4]
        fw.dma(ix[:, :], permv[sl], ix)
        hg = h2g[sl % 2]
        for u in range(4):
            fw.idma(hg[:, u, :], None, cx.h2tm[:, :], bass.IndirectOffsetOnAxis(ap=ix[:, u:u + 1], axis=0), NT - 1, hg, ix)
        cc = c4[sl % 2]
        fw.dma(cc[:, :, :], c4v[sl], cc)
        slot_state[sl] = (ix, hg, cc)

    def load(i):
        sl, j = items[i]
        if j == 0 and sl + 1 < NS:
            slot_prep(sl + 1)
        k = cnt["w"] % 3
        cnt["w"] += 1
        off = bass.IndirectOffsetOnAxis(ap=g.widx[:, sl, j:j + 1], axis=0)
        dep = cx.pdep(("f", l))
        fw.idma(wc[k][:, :], None, wcd, off, NE * 128 - 1, wc[k], g.widx, deps=dep)
        return wc[k]

    pend = []

    def w2part(j, a2_, hd):
        for oc in range(8):
            op_ = o_ps[cnt["o"] % 2]
            cnt["o"] += 1
            for ff in range(4):
                c0 = 8192 + ff * 1024 + oc * 128
                fw.op("pe", lambda: nc.tensor.matmul(
                    op_[:, 0:NB], a2_[:, c0:c0 + 128], hd[:, ff, :], start=(ff == 0), stop=(ff == 3)),
                    R=[a2_, hd], W=[op_], sig=(ff == 3))
            if j == 0:
                fw.op("act", lambda: nc.scalar.copy(acc[:, oc, :], op_[:, 0:NB]), R=[op_], W=[acc])
            else:
                fw.op("dve", lambda: nc.vector.tensor_tensor(acc[:, oc, :], acc[:, oc, :], op_[:, 0:NB], ALU.add),
                      R=[op_, acc], W=[acc])

    def slot_finish(sl):
        ix = slot_state[sl][0]
        if cx.debug and sl == 0 and l == 0:
            fw.dma(cx.d_acc, acc[:, :, :], acc, store=True)
        for u in range(4):
            for half in range(2):
                op_ = o_ps[cnt["o"] % 2]
                cnt["o"] += 1
                for q in range(4):
                    oc = half * 4 + q
                    fw.op("pe", lambda: nc.tensor.transpose(op_[:, q * 128:(q + 1) * 128], acc[:, oc, u * 128:(u + 1) * 128], g.identf[:, :]),
                          R=[acc, g.identf], W=[op_], sig=(q == 3))
                fw.op("act", lambda: nc.scalar.copy(om[:, u, half * 512:(half + 1) * 512], op_[:, :]), R=[op_], W=[om])
        if cx.debug and sl == 0 and l == 0:
            fw.dma(cx.d_om, om[:, :, :], om, store=True)
        for u in range(4):
            fw.idma(cx.moe_tm[:, :], bass.IndirectOffsetOnAxis(ap=ix[:, u:u + 1], axis=0), om[:, u, :], None, NT - 1, om, ix, store=True)

    def comp(i, ws):
        sl, j = items[i]
        a1_ = a3_ = a2_ = ws
        ix, hg, cc = slot_state[sl]
        hs = h2s[sl % 2]
        if j == 0:
            for u in range(4):
                for k in range(8):
                    fw.op("pe", lambda: nc.tensor.transpose(tp_ps[:, k, :], hg[:, u, k * 128:(k + 1) * 128], g.ident[:, :]),
                          R=[hg, g.ident], W=[tp_ps], sig=(k == 7))
                fw.op("act", lambda: nc.scalar.copy(hs[:, :, u * 128:(u + 1) * 128], tp_ps[:, :, :]), R=[tp_ps], W=[hs])
            for u in range(4):
                fw.op("pe", lambda: nc.tensor.matmul(m_ps[0:4, u * 128:(u + 1) * 128], cc[:, u, :], g.identf[:, :], start=True, stop=True),
                      R=[cc, g.identf], W=[m_ps], sig=(u == 3))
            fw.op("act", lambda: nc.scalar.copy(c4T[:, :], m_ps[0:4, :]), R=[m_ps], W=[c4T])
            if cx.debug and sl == 0 and l == 0:
                fw.dma(cx.d_hs, hs[:, :, :], hs, store=True)
                fw.dma(cx.d_hg, hg[:, :, :], hg, store=True)
        cbe = cbs[cnt["cb"] % 8]
        cnt["cb"] += 1
        fw.op("pe", lambda: nc.tensor.matmul(m_ps[:, :], g.sel4[:, j, :], c4T[:, :], start=True, stop=True), R=[g.sel4, c4T], W=[m_ps])
        fw.op("act", lambda: nc.scalar.copy(cbe[:, :], m_ps[:, :]), R=[m_ps], W=[cbe])
        if cx.debug and sl == 0 and l == 0:
            fw.dma(cx.d_cb[j], cbe[:, :], cbe, store=True)
            if j == 1:
                fw.dma(cx.d_w, a1_[:, :], a1_, store=True)
        hd = hid[cnt["h"] % 2]
        cnt["h"] += 1
        for ff in range(4):
            gp, up = g_ps[cnt["g"] % 2], u_ps[cnt["g"] % 2]
            sgt, stt = sg[cnt["g"] % 2], st[cnt["g"] % 2]
            cnt["g"] += 1
            for k in range(8):
                c0 = k * 512 + ff * 128
                fw.op("pe", lambda: nc.tensor.matmul(
                    gp[:, 0:NB], a1_[:, c0:c0 + 128], hs[:, k, :], start=(k == 0), stop=(k == 7)),
                    R=[a1_, hs], W=[gp], sig=(k == 7))
            for k in range(8):
                c0 = 4096 + k * 512 + ff * 128
                fw.op("pe", lambda: nc.tensor.matmul(
                    up[:, 0:NB], a3_[:, c0:c0 + 128], hs[:, k, :], start=(k == 0), stop=(k == 7)),
                    R=[a3_, hs], W=[up], sig=(k == 7))
            fw.op("act", lambda: nc.scalar.activation(out=sgt[:, :], in_=gp[:, 0:NB], func=AF.Silu), R=[gp], W=[sgt])
            fw.op("dve", lambda: nc.vector.tensor_tensor(stt[:, :], sgt[:, :], cbe[:, :], ALU.mult), R=[sgt, cbe], W=[stt])
            fw.op("dve", lambda: nc.vector.tensor_tensor(hd[:, ff, :], stt[:, :], up[:, 0:NB], ALU.mult), R=[stt, up], W=[hd])
            if ff == 1:
                while pend:
                    pj, pa2, phd, psl, plast = pend.pop(0)
                    w2part(pj, pa2, phd)
                    if plast:
                        slot_finish(psl)
        pend.append((j, a2_, hd, sl, j == 3))

    slot_prep(0)
    prefetch_loop(len(items), load, comp)
    while pend:
        pj, pa2, phd, psl, plast = pend.pop(0)
        w2part(pj, pa2, phd)
        if plast:
            slot_finish(psl)
    fw.barrier()


def phase_G(fw, cx, l, s, final):
    nc = fw.nc
    g = cx.g
    sc = cx.__dict__[s.name]
    NB, nsub, n = s.NB, s.nsub, s.n
    fw.phase("G%d%s" % (l, s.name))
    xts = fw.ring("xt", 2, [128, 8, NB], F32, dma=True)
    mts = fw.ring("mt", 2, [128, nsub, 1024], F32, dma=True)
    t_ps = fw.pring("t", 4, [128, 512])
    ss_ps = fw.ptile("ss", [128, 512])
    if final:
        scr = {"sq": fw.tile("sq", [128, 8, NB], BF16), "rt": fw.tile("rt", [128, NB], F32),
               "tmp": fw.tile("tmp", [128, 8, NB], F32, dma=True)}
    xmd = sc.xmid.rearrange("(c p) t -> p c t", p=128)
    r0 = 0 if s is LAT else T
    cnt = [0]

    def load(blk):
        xt, mt = xts[blk % 2], mts[blk % 2]
        fw.dma(xt[:, :, :], xmd[:, :, blk * NB:(blk + 1) * NB], xt)
        fw.dma(mt[:, :, :], cx.moe_tm[r0 + blk * NB:r0 + (blk + 1) * NB, :].rearrange("(u p) n -> p u n", p=128), mt)
        return (xt, mt)

    def comp(blk, xm):
        xt, mt = xm
        for oc in range(8):
            tp = t_ps[cnt[0] % 4]
            cnt[0] += 1
            for u in range(nsub):
                fw.op("pe", lambda: nc.tensor.transpose(tp[:, u * 128:(u + 1) * 128], mt[:, u, oc * 128:(oc + 1) * 128], g.identf[:, :]),
                      R=[mt, g.identf], W=[tp], sig=(u == nsub - 1))
            fw.op("dve", lambda: nc.vector.scalar_tensor_tensor(
                xt[:, oc, :], tp[:, 0:NB], g.mod[l][:, M_G2 + oc, n:n + 1], xt[:, oc, :], ALU.mult, ALU.add),
                R=[tp, g.mod[l], xt], W=[xt])
        c0, c1 = blk * NB, (blk + 1) * NB
        if not final:
            fw.dma(sc.x[l + 1].rearrange("(c p) t -> p c t", p=128)[:, :, c0:c1], xt[:, :, :], xt, store=True)
        else:
            y = scr["tmp"]
            final_norm(fw, cx, xt, y, NB, scr, g.vecs[l], ss_ps)
            fw.dma(cx.yT.rearrange("(c p) t -> p c t", p=128)[:, :, c0:c1], y[:, :, :], y, store=True)

    prefetch_loop(s.nblk, load, comp)
    fw.barrier()


def phase_F(fw, cx, l, s, final):
    nc = fw.nc
    g = cx.g
    sc = cx.__dict__[s.name]
    SB, NB = s.SB, s.NB
    nb = SB // NB
    n = s.n
    fw.phase("F%d%s" % (l, s.name))
    h2 = fw.tile("h2", [128, 8, SB], BF16, dma=True)
    xt = fw.tile("xt", [128, 8, SB], F32, dma=True)
    acc = fw.tile("acc", [128, 8, SB], F32)
    w1 = fw.ring("w1", 3, [128, 8, 512], BF16, dma=True)
    w3 = fw.ring("w3", 3, [128, 8, 512], BF16, dma=True)
    w2 = fw.ring("w2", 3, [128, 4, 1024], BF16, dma=True)
    cb = fw.ring("cb", 3, [128, SB], F32, dma=True)
    sg = fw.ring("sg", 2, [128, NB], F32)
    st = fw.ring("st", 2, [128, NB], F32)
    hid = fw.ring("hid", 2, [128, 4, NB], BF16)
    g_ps = fw.pring("g", 2, [128, 512])
    u_ps = fw.pring("u", 2, [128, 512])
    o_ps = fw.pring("o", 3, [128, 512])
    ss_ps = fw.ptile("ss", [128, 512])
    if final:
        scr = {"sq": fw.tile("sq", [128, 8, NB], BF16), "rt": fw.tile("rt", [128, NB], F32),
               "tmp": fw.tile("tmp", [128, 8, NB], F32, dma=True)}
    h2d = sc.h2T.rearrange("(c p) t -> p c t", p=128)
    xmd = sc.xmid.rearrange("(c p) t -> p c t", p=128)
    cnt = {"g": 0, "o": 0, "h": 0}
    for sb in range(s.nsb):
        s0, s1 = sb * SB, (sb + 1) * SB
        fw.dma(h2[:, :, :], h2d[:, :, s0:s1], h2)
        fw.dma(xt[:, :, :], xmd[:, :, s0:s1], xt)

        def load(e):
            i = (sb * NE + e) % 3
            fw.dma(w1[i][:, :, :], cx.b_w1[l, e], w1[i], deps=cx.pdep(("f", l)))
            fw.dma(w3[i][:, :, :], cx.b_w3[l, e], w3[i], deps=cx.pdep(("f", l)))
            fw.dma(w2[i][:, :, :], cx.b_w2[l, e], w2[i], deps=cx.pdep(("f", l)))
            fw.dma(cb[i][:, :], sc.combT[e:e + 1, s0:s1].partition_broadcast(128), cb[i])
            return (w1[i], w3[i], w2[i], cb[i])

        pend = []

        def w2part(e, a2_, hd, b0, b1):
            for oc in range(8):
                op_ = o_ps[cnt["o"] % 3]
                cnt["o"] += 1
                for ff in range(4):
                    fw.op("pe", lambda: nc.tensor.matmul(
                        op_[:, 0:NB], a2_[:, ff, oc * 128:(oc + 1) * 128], hd[:, ff, :], start=(ff == 0), stop=(ff == 3)),
                        R=[a2_, hd], W=[op_], sig=(ff == 3))
                if e == 0:
                    fw.op("act", lambda: nc.scalar.copy(acc[:, oc, b0:b1], op_[:, 0:NB]), R=[op_], W=[acc])
                else:
                    fw.op("dve", lambda: nc.vector.tensor_tensor(acc[:, oc, b0:b1], acc[:, oc, b0:b1], op_[:, 0:NB], ALU.add),
                          R=[op_, acc], W=[acc])

        def comp(e, ws):
            a1_, a3_, a2_, cbe = ws
            for blk in range(nb):
                b0, b1 = blk * NB, (blk + 1) * NB
                hd = hid[cnt["h"] % 2]
                cnt["h"] += 1
                for ff in range(4):
                    gp, up = g_ps[cnt["g"] % 2], u_ps[cnt["g"] % 2]
                    sgt, stt = sg[cnt["g"] % 2], st[cnt["g"] % 2]
                    cnt["g"] += 1
                    for k in range(8):
                        fw.op("pe", lambda: nc.tensor.matmul(
                            gp[:, 0:NB], a1_[:, k, ff * 128:(ff + 1) * 128], h2[:, k, b0:b1], start=(k == 0), stop=(k == 7)),
                            R=[a1_, h2], W=[gp], sig=(k == 7))
                    for k in range(8):
                        fw.op("pe", lambda: nc.tensor.matmul(
                            up[:, 0:NB], a3_[:, k, ff * 128:(ff + 1) * 128], h2[:, k, b0:b1], start=(k == 0), stop=(k == 7)),
                            R=[a3_, h2], W=[up], sig=(k == 7))
                    fw.op("act", lambda: nc.scalar.activation(out=sgt[:, :], in_=gp[:, 0:NB], func=AF.Silu),
                          R=[gp], W=[sgt])
                    fw.op("dve", lambda: nc.vector.tensor_tensor(stt[:, :], sgt[:, :], cbe[:, b0:b1], ALU.mult),
                          R=[sgt, cbe], W=[stt])
                    fw.op("dve", lambda: nc.vector.tensor_tensor(hd[:, ff, :], stt[:, :], up[:, 0:NB], ALU.mult),
                          R=[stt, up], W=[hd])
                    if ff == 1:
                        while pend:
                            w2part(*pend.pop(0))
                pend.append((e, a2_, hd, b0, b1))
        prefetch_loop(NE, load, comp)
        while pend:
            w2part(*pend.pop(0))
        for oc in range(8):
            fw.op("dve", lambda oc=oc: nc.vector.scalar_tensor_tensor(
                xt[:, oc, :], acc[:, oc, :], g.mod[l][:, M_G2 + oc, n:n + 1], xt[:, oc, :], ALU.mult, ALU.add),
                R=[acc, g.mod[l], xt], W=[xt])
        if not final:
            fw.dma(sc.x[l + 1].rearrange("(c p) t -> p c t", p=128)[:, :, s0:s1], xt[:, :, :], xt, store=True)
        else:
            v = g.vecs[l]
            for blk in range(nb):
                b0, b1 = blk * NB, (blk + 1) * NB
                xv = _Sub(xt, b0, b1)
                y = scr["tmp"]
                final_norm(fw, cx, xv, y, NB, scr, v, ss_ps)
                fw.dma(cx.yT.rearrange("(c p) t -> p c t", p=128)[:, :, s0 + b0:s0 + b1], y[:, :, :], y, store=True)
    fw.barrier()


def final_norm(fw, cx, xt, y, W, scr, v, ss_ps):
    nc = fw.nc
    g = cx.g
    sq, rt, tmp = scr["sq"], scr["rt"], scr["tmp"]
    fw.op("act", lambda: nc.scalar.activation(out=sq[:, :, 0:W], in_=xt[:, :, 0:W], func=AF.Square), R=[xt], W=[sq])
    for ch in range(8):
        fw.op("pe", lambda ch=ch: nc.tensor.matmul(ss_ps[:, 0:W], g.ones[:, :], sq[:, ch, 0:W], start=(ch == 0), stop=(ch == 7)),
              R=[g.ones, sq], W=[ss_ps], sig=(ch == 7))
    fw.op("act", lambda: nc.scalar.activation(out=rt[:, 0:W], in_=ss_ps[:, 0:W], func=AF.Sqrt, bias=EPS, scale=1.0 / D),
          R=[ss_ps], W=[rt])
    fw.op("dve", lambda: nc.vector.reciprocal(rt[:, 0:W], rt[:, 0:W]), R=[rt], W=[rt])
    fw.op("dve", lambda: nc.vector.tensor_tensor(tmp[:, :, 0:W], xt[:, :, 0:W],
                                                  rt[:, 0:W].unsqueeze(1).to_broadcast([128, 8, W]), ALU.mult),
          R=[xt, rt], W=[tmp])
    fw.op("dve", lambda: nc.vector.tensor_tensor(y[:, :, 0:W], tmp[:, :, 0:W],
                                                  v[:, V_NF:V_NF + 8].unsqueeze(2).to_broadcast([128, 8, W]), ALU.mult),
          R=[tmp, v], W=[y])


def _fm(vv):
    return np.ascontiguousarray(np.asarray(vv, np.float32).reshape(-1, 128).T)


def _kmaj(w):
    K, N = w.shape
    return np.ascontiguousarray(w.reshape(K // 128, 128, N).transpose(1, 0, 2))


_CONST = {}


def constants():
    if _CONST:
        return _CONST
    bf = ml_dtypes.bfloat16
    t = np.arange(T)
    row = (t // 64).astype(np.float64)
    col = (t % 64).astype(np.float64)
    inv = 10000.0 ** (-np.arange(0, 32, 2, dtype=np.float64) / 32)
    ang = np.stack([row[:, None] * inv, col[:, None] * inv], axis=1).astype(np.float32)
    cs = np.concatenate([np.cos(ang).reshape(T, 32), np.sin(ang).reshape(T, 32)], axis=1).astype(np.float32)
    _CONST["rope_cs"] = np.ascontiguousarray(cs.reshape(32, 128, 64).transpose(1, 0, 2))

    def dft(L):
        i = np.arange(L, dtype=np.int64)
        ph = (np.outer(i, i) % L).astype(np.float64) * (2 * np.pi / L)
        sc = 1.0 / np.sqrt(L * 128.0)
        return (np.cos(ph) * sc).astype(bf), (-np.sin(ph) * sc).astype(bf)
    _CONST["dftC"], _CONST["dftS"] = dft(T)
    _CONST["dftCc"], _CONST["dftSc"] = dft(CL)
    i = np.arange(128, dtype=np.int64)
    ph = (np.outer(i, i) % 128).astype(np.float64) * (2 * np.pi / 128)
    _CONST["csm"] = np.concatenate([np.cos(ph), np.sin(ph)], axis=1).astype(bf)
    _CONST["ident"] = np.eye(128, dtype=np.float32).astype(bf)
    _CONST["identf"] = np.eye(128, dtype=np.float32)
    kk = np.arange(128)[:, None]
    qq = np.arange(512)[None, :]
    wm = np.zeros((128, 6, 512), np.float32)
    for r in range(-1, 5):
        wm[:, r + 1, :] = (np.abs(qq - (128 * r + kk)) <= 128)
    _CONST["wmask"] = wm.astype(bf)

    def invc(L):
        pos = np.arange(L)
        out = np.zeros((4, 128, L), np.float32)
        for gi, w in enumerate((2, 4, 8, 16)):
            lo = np.clip(pos - w // 2, 0, L)
            hi = np.clip(pos + w // 2, 0, L)
            out[gi] = (1.0 / (hi - lo).astype(np.float32))[None, :]
        return out
    pp = np.arange(128)
    _CONST["ustrict"] = (pp[:, None] < pp[None, :]).astype(np.float32).astype(bf)
    _CONST["th9"] = np.ascontiguousarray(np.broadcast_to((512.0 * np.arange(9, dtype=np.float32))[None, None, :], (128, 4, 9)))
    _CONST["siota"] = np.ascontiguousarray(np.broadcast_to(np.arange(NSLOT, dtype=np.float32)[None, :, None], (128, NSLOT, 3)))
    _CONST["jp"] = (np.arange(4, dtype=np.float32)[None, :] * 128 + pp[:, None]).astype(np.float32)
    _CONST["tokidx"] = (np.arange(NCHT, dtype=np.int32)[None, :] * 128 + pp[:, None]).astype(np.int32)
    sel = np.zeros((4, 4, 128), np.float32)
    for jj in range(4):
        sel[jj, jj, :] = 1.0
    _CONST["sel4"] = sel
    _CONST["oobfill"] = np.full((NSLOT * 512, 1), NTT, np.int32)
    _CONST["zrow"] = np.zeros((128, D), np.float32).astype(bf)
    _CONST["zero4"] = np.zeros((NSLOT * 512, 4), np.float32)
    _CONST["invcnt"] = invc(T)
    _CONST["invcntc"] = invc(CL)
    return _CONST


def prep_shared(inp):
    f = lambda a: np.asarray(a, np.float32)
    sh = {}
    w_ada = f(inp["w_ada"])
    sh["w_ada"] = np.ascontiguousarray(w_ada.reshape(DEPTH, 8, 128, 12, 512).transpose(0, 3, 2, 1, 4))
    w_in = f(inp["w_in"])
    sh["w_in"] = np.ascontiguousarray(w_in.reshape(DEPTH, 8, 128, 2560).transpose(0, 2, 1, 3))
    wg = f(inp["w_gate"])
    sh["w_gate"] = np.ascontiguousarray(wg.reshape(DEPTH, 4, 8, 128, 8, 128).transpose(0, 4, 3, 1, 2, 5))
    wb = f(inp["w_branch"])
    sh["w_branch"] = np.ascontiguousarray(wb.reshape(DEPTH, 4, 4, 128, 8, 128).transpose(0, 4, 3, 1, 2, 5))
    wo = f(inp["w_out"])
    sh["w_out"] = np.ascontiguousarray(wo.reshape(DEPTH, 8, 128, 8, 128).transpose(0, 3, 2, 1, 4))
    pw = f(inp["pool_w"])
    sh["pool_w"] = np.ascontiguousarray(pw.transpose(0, 2, 1, 3))
    sh["router_w"] = _kmaj(f(inp["router_w"]))
    sh["w1"] = np.ascontiguousarray(f(inp["w1"]).reshape(DEPTH, NE, 8, 128, 512).transpose(0, 1, 3, 2, 4))
    sh["w3"] = np.ascontiguousarray(f(inp["w3"]).reshape(DEPTH, NE, 8, 128, 512).transpose(0, 1, 3, 2, 4))
    sh["w2"] = np.ascontiguousarray(f(inp["w2"]).reshape(DEPTH, NE, 4, 128, 1024).transpose(0, 1, 3, 2, 4))
    vecs = np.zeros((DEPTH, 128, NV), np.float32)
    rep = np.zeros((DEPTH, 128, NR), np.float32)
    for l in range(DEPTH):
        vecs[l, :, V_N1:V_N1 + 8] = _fm(inp["norm1"][l])
        vecs[l, :, V_N2:V_N2 + 8] = _fm(inp["norm2"][l])
        vecs[l, :, V_BG:V_BG + 32] = _fm(f(inp["b_gate"][l]).reshape(-1))
        vecs[l, :, V_PS:V_PS + 4] = _fm(inp["pool_scale"][l])
        vecs[l, :, V_BA:V_BA + 48] = _fm(inp["b_ada"][l])
        vecs[l, :, V_NF:V_NF + 8] = _fm(inp["norm_f"])
        gain = np.concatenate([np.tile(f(inp["q_gain"][l]), 8), np.tile(f(inp["k_gain"][l]), 2)])
        rep[l, :, R_GAIN:R_GAIN + 640] = gain[None, :]
        rep[l, :, R_SINK:R_SINK + 8] = f(inp["sink"][l])[None, :]
        rep[l, :, R_RB:R_RB + 16] = f(inp["router_bias"])[None, :]
    sh["vecs"] = vecs
    sh["rep"] = rep
    sh.update(constants())
    return sh


_NC = {}


def kernel(**inputs):
    inp = {k: np.asarray(v) for k, v in inputs.items()}
    if "nc" not in _NC:
        _NC["nc"] = build()
    nc = _NC["nc"]
    sh = prep_shared(inp)
    x = np.asarray(inp["x"], np.float32)
    ctx = np.asarray(inp["ctx"], np.float32)
    c = np.asarray(inp["c"], np.float32)
    c_ctx = np.asarray(inp["c_ctx"], np.float32)
    in_maps = []
    for b in range(8):
        m = dict(sh)
        m["xT"] = np.ascontiguousarray(x[b].T)
        m["ctxT"] = np.ascontiguousarray(ctx[b].T)
        c2 = np.stack([c[b], c_ctx], axis=1)
        m["c2"] = np.ascontiguousarray(c2.reshape(8, 128, 2).transpose(1, 0, 2))
        in_maps.append(m)
    res = run_bass_kernel_spmd(nc, in_maps, core_ids=list(range(8)))
    out = np.stack([np.ascontiguousarray(res.results[b]["yT"].T) for b in range(8)], axis=0)
    return out.astype(np.float32)
```

```python
import contextlib
import numpy as np
import ml_dtypes
import concourse.bass as bass
import concourse.mybir as mybir
from concourse.bass_utils import run_bass_kernel_spmd

F32 = mybir.dt.float32
I32 = mybir.dt.int32
BF16 = mybir.dt.bfloat16
AF = mybir.ActivationFunctionType
ALU = mybir.AluOpType
AX = mybir.AxisListType

D = 1024
T = 4096
CL = 256
NK = T + CL
DEPTH = 2
NE = 16
NSLOT = 12
NCHT = 34
NTT = T + CL
EPS = 1e-6
SAME_SYNC = True
POOL_K = 6
BIG = 1.0e4


class DSem:
    def __init__(self, h):
        self.h = h
        self.total = 0


class Tl:
    def __init__(self, h, name):
        self.h = h
        self.name = name
        self.w = None
        self.rs = {}
        self.sem = None

    def __getitem__(self, k):
        return self.h[k]


class FW:
    CE = ("pe", "act", "dve", "pool")

    def __init__(self, nc):
        self.nc = nc
        self.E = {"pe": nc.tensor, "act": nc.scalar, "dve": nc.vector, "pool": nc.gpsimd, "sp": nc.sync}
        self.ges = contextlib.ExitStack()
        self.sem = {e: self.ges.enter_context(nc.semaphore("s_" + e)) for e in self.CE}
        self.cnt = {e: 0 for e in self.CE}
        self.seen = {e: {} for e in self.E}
        self.free_dsems = []
        self.live_dsems = []
        self.pes = None
        self.uid = 0
        self.npe = 0
        self.marks = []
        self.pool_out = []
        self.glob_dsems = []

    def phase(self, label=""):
        self.marks.append((label, self.npe))
        self.pes = contextlib.ExitStack()
        return self.pes

    def _name(self, n):
        self.uid += 1
        return "%s_%d" % (n, self.uid)

    def tile(self, name, shape, dtype, dma=False, glob=False):
        es = self.ges if glob else self.pes
        h = es.enter_context(self.nc.sbuf_tensor(self._name(name), list(shape), dtype))
        t = Tl(h, name)
        if dma:
            if self.free_dsems:
                ds = self.free_dsems.pop()
            else:
                ds = DSem(self.ges.enter_context(self.nc.semaphore(self._name("d"))))
            t.sem = ds
            if not glob:
                self.live_dsems.append(ds)
            else:
                self.glob_dsems.append(ds)
        return t


    def ptile(self, name, shape, dtype=F32):
        h = self.pes.enter_context(self.nc.psum_tensor(self._name(name), list(shape), dtype))
        return Tl(h, name)

    def ring(self, name, n, shape, dtype, dma=False):
        return [self.tile("%s%d" % (name, i), shape, dtype, dma=dma) for i in range(n)]

    def pring(self, name, n, shape, dtype=F32):
        return [self.ptile("%s%d" % (name, i), shape, dtype) for i in range(n)]

    def _wait(self, eng, deps):
        for (key, sem, val, src) in deps:
            if src == eng:
                if eng == "pe" or not SAME_SYNC:
                    continue
            if self.seen[eng].get(key, 0) >= val:
                continue
            self.E[eng].wait_ge(sem, val)
            self.seen[eng][key] = val

    def op(self, eng, fn, R=(), W=(), sig=True):
        deps = []
        for t in R:
            if t.w is not None:
                deps.append(t.w)
        for t in W:
            if t.w is not None:
                deps.append(t.w)
            deps.extend(t.rs.values())
        self._wait(eng, deps)
        ins = fn()
        if eng == "pe":
            self.npe += 1
        if sig:
            self.cnt[eng] += 1
            ins.then_inc(self.sem[eng], 1)
            tok = (eng, self.sem[eng], self.cnt[eng], eng)
        else:
            tok = (eng, self.sem[eng], self.cnt[eng] + 1, eng)
        for t in R:
            t.rs[eng] = tok
        for t in W:
            t.w = tok
            t.rs = {}
        return ins

    def dma(self, out, in_, tile, store=False, q="sp", deps=()):
        d = list(deps)
        if store:
            if tile.w is not None:
                d.append(tile.w)
        else:
            if tile.w is not None and not (tile.w[3] == "dma" and tile.w[0] == id(tile.sem)):
                d.append(tile.w)
            d.extend(tile.rs.values())
        self._wait(q, d)
        ins = self.E[q].dma_start(out=out, in_=in_)
        ds = tile.sem
        ds.total += 16
        ins.then_inc(ds.h, 16)
        tok = (id(ds), ds.h, ds.total, "dma")
        if store:
            tile.rs["dma"] = tok
        else:
            tile.w = tok
            tile.rs = {}
        return ins

    def idma(self, out, out_off, in_, in_off, bound, tile, idx, store=False, deps=()):
        q = "pool"
        d = list(deps)
        if idx.w is not None:
            d.append(idx.w)
        if store:
            if tile.w is not None:
                d.append(tile.w)
        else:
            if tile.w is not None and not (tile.w[3] == "dma" and tile.w[0] == id(tile.sem)):
                d.append(tile.w)
            d.extend(tile.rs.values())
        self._wait(q, d)
        while len(self.pool_out) >= POOL_K:
            self._wait(q, [self.pool_out.pop(0)])
        ins = self.nc.gpsimd.indirect_dma_start(out=out, out_offset=out_off, in_=in_, in_offset=in_off)
        ds = tile.sem
        ds.total += 16
        ins.then_inc(ds.h, 16)
        tok = (id(ds), ds.h, ds.total, "dma")
        idx.rs["idma"] = tok
        self.pool_out.append(tok)
        if store:
            tile.rs["dma"] = tok
        else:
            tile.w = tok
            tile.rs = {}
        return ins

    def barrier(self, end_phase=True):
        for e in self.E:
            deps = []
            for o in self.CE:
                if o != e and self.cnt[o] > 0:
                    deps.append((o, self.sem[o], self.cnt[o], o))
            for ds in self.live_dsems + self.glob_dsems:
                if ds.total > 0:
                    deps.append((id(ds), ds.h, ds.total, "dma"))
            self._wait(e, deps)
        if end_phase:
            self.free_dsems.extend(self.live_dsems)
            self.live_dsems = []
            self.pes.close()
            self.pes = None


def prefetch_loop(n, load, compute, depth=1):
    q = [load(k) for k in range(min(depth, n))]
    for k in range(n):
        if k + depth < n:
            q.append(load(k + depth))
        compute(k, q.pop(0))


class Seq:
    def __init__(self, name, L, NB, n, koff, rope):
        self.name = name
        self.L = L
        self.NB = NB
        self.nblk = L // NB
        self.nsub = NB // 128
        self.n = n
        self.koff = koff
        self.rope = rope
        self.SB = min(L, 512)
        self.nsb = L // self.SB


LAT = Seq("lat", T, 512, 0, CL, True)
CTX = Seq("ctx", CL, 256, 1, 0, False)

V_N1, V_N2, V_BG, V_PS, V_BA, V_NF = 0, 8, 16, 48, 52, 100
NV = 108
R_GAIN, R_SINK, R_RB = 0, 640, 648
NR = 664
M_SH1, M_SC1, M_G1, M_SH2, M_SC2, M_G2 = 0, 8, 16, 24, 32, 40


class Ctx:
    pass


def build(debug=False, stop=None):
    nc = bass.Bass("TRN2", target_bir_lowering=False)
    fw = FW(nc)
    cx = Ctx()
    skind = "ExternalOutput" if debug else "Internal"

    def din(name, shape, dt=F32):
        return nc.dram_tensor(name, list(shape), dt, kind="ExternalInput").ap()

    def dscr(name, shape, dt):
        return nc.dram_tensor(name, list(shape), dt, kind=skind).ap()

    cx.xT = din("xT", [D, T])
    cx.ctxT = din("ctxT", [D, CL])
    cx.c2 = din("c2", [128, 8, 2])
    cx.vecs = din("vecs", [DEPTH, 128, NV])
    cx.rep = din("rep", [DEPTH, 128, NR])
    cx.w_ada = din("w_ada", [DEPTH, 12, 128, 8, 512])
    cx.w_in = din("w_in", [DEPTH, 128, 8, 2560])
    cx.w_gate = din("w_gate", [DEPTH, 8, 128, 4, 8, 128])
    cx.w_branch = din("w_branch", [DEPTH, 8, 128, 4, 4, 128])
    cx.w_out = din("w_out", [DEPTH, 8, 128, 8, 128])
    cx.pool_w = din("pool_w", [DEPTH, 128, 4, 128])
    cx.router_w = din("router_w", [128, 8, 16])
    cx.w1 = din("w1", [DEPTH, NE, 128, 8, 512])
    cx.w3 = din("w3", [DEPTH, NE, 128, 8, 512])
    cx.w2 = din("w2", [DEPTH, NE, 128, 4, 1024])
    cx.rope_cs = din("rope_cs", [128, 32, 64])
    cx.dftC = din("dftC", [T, T], BF16)
    cx.dftS = din("dftS", [T, T], BF16)
    cx.dftCc = din("dftCc", [CL, CL], BF16)
    cx.dftSc = din("dftSc", [CL, CL], BF16)
    cx.csm = din("csm", [128, 256], BF16)
    cx.ident = din("ident", [128, 128], BF16)
    cx.identf = din("identf", [128, 128], F32)
    cx.wmask = din("wmask", [128, 6, 512], BF16)
    cx.invcnt = din("invcnt", [4, 128, T])
    cx.invcntc = din("invcntc", [4, 128, CL])
    cx.ustrict = din("ustrict", [128, 128], BF16)
    cx.th9 = din("th9", [128, 4, 9])
    cx.siota = din("siota", [128, NSLOT, 3])
    cx.jp = din("jp", [128, 4])
    cx.tokidx = din("tokidx", [128, NCHT], I32)
    cx.sel4 = din("sel4", [4, 4, 128])
    cx.oobfill = din("oobfill", [NSLOT * 512, 1], I32)
    cx.zero4 = din("zero4", [NSLOT * 512, 4])
    cx.yT = nc.dram_tensor("yT", [D, T], F32, kind="ExternalOutput").ap()
    cx.h2tm = dscr("h2tm", [NTT + 128, D], BF16)
    cx.moe_tm = dscr("moe_tm", [NTT + 128, D], F32)
    cx.zrow = din("zrow", [128, D], BF16)
    cx.permD = dscr("permD", [NSLOT * 512, 1], I32)
    cx.c4D = dscr("c4D", [NSLOT * 512, 4], F32)
    cx.debug = debug
    if debug:
        cx.d_hs = dscr("d_hs", [128, 8, 512], BF16)
        cx.d_cb = dscr("d_cb", [4, 128, 512], F32)
        cx.d_acc = dscr("d_acc", [128, 8, 512], F32)
        cx.d_om = dscr("d_om", [128, 4, 1024], F32)
        cx.d_w = dscr("d_w", [128, 12288], BF16)
        cx.d_hg = dscr("d_hg", [128, 4, 1024], BF16)

    cx.b_ada = dscr("b_w_ada", [DEPTH, 12, 128, 8, 512], BF16)
    cx.b_in = dscr("b_w_in", [DEPTH, 128, 8, 2560], BF16)
    cx.b_gate = dscr("b_w_gate", [DEPTH, 8, 128, 4, 8, 128], BF16)
    cx.b_branch = dscr("b_w_branch", [DEPTH, 8, 128, 4, 4, 128], BF16)
    cx.b_out = dscr("b_w_out", [DEPTH, 8, 128, 8, 128], BF16)
    cx.b_pool = dscr("b_pool_w", [DEPTH, 128, 4, 128], BF16)
    cx.b_router = dscr("b_router", [128, 8, 16], BF16)
    cx.b_wcat = dscr("b_wcat", [DEPTH, NE, 128, 12288], BF16)

    for s in (LAT, CTX):
        L = s.L
        sc = Ctx()
        sc.x = [None, dscr(s.name + "_x1", [D, L], F32)]
        sc.xmid = dscr(s.name + "_xmid", [D, L], F32)
        sc.hT = dscr(s.name + "_hT", [D, L], BF16)
        sc.h2T = dscr(s.name + "_h2T", [D, L], BF16)
        sc.pinT = dscr(s.name + "_pinT", [512, L], F32)
        sc.AB = dscr(s.name + "_AB", [L, 1024], BF16)
        sc.qT = {"b": dscr(s.name + "_qbT", [512, L], BF16), "c": dscr(s.name + "_qcT", [512, L], BF16)}
        sc.br = dscr(s.name + "_br", [4, 512, L], BF16)
        sc.combT = dscr(s.name + "_combT", [NE, L], F32)
        cx.__dict__[s.name] = sc
    cx.lat.x[0] = cx.xT
    cx.ctx.x[0] = cx.ctxT
    cx.kT = {"b": dscr("kbT", [2, 128, NK], BF16), "c": dscr("kcT", [2, 128, NK], BF16)}
    cx.v = {"b": dscr("vb", [NK, 130], BF16), "c": dscr("vc", [NK, 130], BF16)}

    psem = {}

    def precast(key, dst, src):
        if key not in psem:
            psem[key] = DSem(fw.ges.enter_context(nc.semaphore(fw._name("pc"))))
        ds = psem[key]
        nc.gpsimd.dma_start(out=dst, in_=src).then_inc(ds.h, 16)
        ds.total += 16

    def pdep(key):
        ds = psem[key]
        return [(id(ds), ds.h, ds.total, "dma")]
    cx.pdep = pdep

    def precast_group(key):
        kind, l = key
        if kind == "a":
            wi = cx.w_in[l].rearrange("p c n -> (p c) n")
            bi = cx.b_in[l].rearrange("p c n -> (p c) n")
            for jb in range(12):
                precast(key, cx.b_ada[l, jb], cx.w_ada[l, jb])
            for i in range(4):
                precast(key, bi[i * 256:(i + 1) * 256, :], wi[i * 256:(i + 1) * 256, :])
            precast(key, cx.b_pool[l], cx.pool_w[l])
            if l == 0:
                precast(key, cx.b_router, cx.router_w)
        elif kind == "e":
            for oc in range(8):
                precast(key, cx.b_gate[l, oc], cx.w_gate[l, oc])
                precast(key, cx.b_branch[l, oc], cx.w_branch[l, oc])
            for oc in range(8):
                precast(key, cx.b_out[l, oc], cx.w_out[l, oc])
        else:
            for e in range(NE):
                precast(key, cx.b_wcat[l, e, :, 0:4096].rearrange("p (c n) -> p c n", c=8), cx.w1[l, e])
                precast(key, cx.b_wcat[l, e, :, 4096:8192].rearrange("p (c n) -> p c n", c=8), cx.w3[l, e])
                precast(key, cx.b_wcat[l, e, :, 8192:12288].rearrange("p (c n) -> p c n", c=4), cx.w2[l, e])

    def precast_later(l):
        if fw.cnt["pe"] > 0:
            nc.gpsimd.wait_ge(fw.sem["pe"], fw.cnt["pe"])
        precast_group(("e", l))
        precast_group(("f", l))
        if l + 1 < DEPTH:
            precast_group(("a", l + 1))
    cx.precast_later = precast_later
    precast_group(("a", 0))

    g = Ctx()
    cx.g = g
    g.vecs = [fw.tile("vecs%d" % l, [128, NV], F32, dma=True, glob=True) for l in range(DEPTH)]
    g.rep = [fw.tile("rep%d" % l, [128, NR], F32, dma=True, glob=True) for l in range(DEPTH)]
    g.ident = fw.tile("ident", [128, 128], BF16, dma=True, glob=True)
    g.identf = fw.tile("identf", [128, 128], F32, dma=True, glob=True)
    g.ones = fw.tile("ones", [128, 128], BF16, glob=True)
    g.onesf = fw.tile("onesf", [128, 64], F32, glob=True)
    g.mod = [fw.tile("mod%d" % l, [128, 48, 2], F32, glob=True) for l in range(DEPTH)]
    g.a1 = [fw.tile("a1_%d" % l, [128, 8, 2], F32, glob=True) for l in range(DEPTH)]
    g.a2 = [fw.tile("a2_%d" % l, [128, 8, 2], F32, glob=True) for l in range(DEPTH)]
    g.esink = [fw.tile("esink%d" % l, [128, 8], F32, glob=True) for l in range(DEPTH)]
    g.Gall = fw.tile("Gall", [128, NCHT, 4], F32, glob=True)
    g.C4all = fw.tile("C4all", [128, NCHT, 4], F32, glob=True)
    g.widx = fw.tile("widx", [128, NSLOT, 4], I32, glob=True)
    g.tokidx = fw.tile("tokidx", [128, NCHT], I32, dma=True, glob=True)
    g.sel4 = fw.tile("sel4", [4, 4, 128], F32, dma=True, glob=True)
    fw.dma(g.tokidx[:, :], cx.tokidx, g.tokidx)
    zs = DSem(fw.ges.enter_context(nc.semaphore(fw._name("zs"))))
    nc.sync.dma_start(out=cx.h2tm[NTT:NTT + 128, :], in_=cx.zrow).then_inc(zs.h, 16)
    zs.total = 16
    fw.glob_dsems.append(zs)
    fw.dma(g.sel4[:, :, :], cx.sel4, g.sel4)
    for l in range(DEPTH):
        fw.dma(g.vecs[l][:, :], cx.vecs[l], g.vecs[l])
        fw.dma(g.rep[l][:, :], cx.rep[l], g.rep[l])
    fw.dma(g.ident[:, :], cx.ident, g.ident)
    fw.dma(g.identf[:, :], cx.identf, g.identf)
    fw.op("dve", lambda: nc.vector.memset(g.ones[:, :], 1.0), W=[g.ones])
    fw.op("dve", lambda: nc.vector.memset(g.onesf[:, :], 1.0), W=[g.onesf])
    g.negone = fw.tile("negone", [128, 512], F32, glob=True)
    fw.op("dve", lambda: nc.vector.memset(g.negone[:, :], -1.0), W=[g.negone])
    for l in range(DEPTH):
        fw.op("act", lambda l=l: nc.scalar.activation(out=g.esink[l][:, :], in_=g.rep[l][:, R_SINK:R_SINK + 8], func=AF.Exp),
              R=[g.rep[l]], W=[g.esink[l]])

    class _Stop(Exception):
        pass

    def run(name, fn, *a, **k):
        fn(*a, **k)
        if stop is not None and name == stop:
            raise _Stop()

    try:
        for l in range(DEPTH):
            last = (l == DEPTH - 1)
            run("mod%d" % l, phase_mod, fw, cx, l)
            run("Actx%d" % l, phase_A, fw, cx, l, CTX, kv_only=last)
            run("A%d" % l, phase_A, fw, cx, l, LAT, kv_only=False)
            run("B%d" % l, phase_B, fw, cx, l, LAT)
            if not last:
                phase_B(fw, cx, l, CTX)
            precast_later(l)
            run("Cb%d" % l, phase_C, fw, cx, l, "b", ctx_too=not last)
            run("Cc%d" % l, phase_C, fw, cx, l, "c", ctx_too=not last)
            run("D%d" % l, phase_D, fw, cx, l, LAT)
            if not last:
                phase_D(fw, cx, l, CTX)
            run("E%d" % l, phase_E, fw, cx, l, LAT)
            if not last:
                phase_E(fw, cx, l, CTX)
            run("R%d" % l, phase_R, fw, cx, l, with_ctx=not last)
            run("F%d" % l, phase_F2, fw, cx, l, with_ctx=not last)
            run("G%d" % l, phase_G, fw, cx, l, LAT, final=last)
            if not last:
                phase_G(fw, cx, l, CTX, final=False)
    except _Stop:
        pass
    fw.phase("end")
    fw.barrier()
    fw.ges.close()
    _NC["marks"] = fw.marks
    return nc


def phase_mod(fw, cx, l0):
    nc = fw.nc
    g = cx.g
    fw.phase("mod%d" % l0)
    c2 = fw.tile("c2", [128, 8, 2], F32, dma=True)
    sc = fw.tile("sc", [128, 8, 2], BF16)
    wa = fw.ring("wa", 2, [128, 8, 512], BF16, dma=True)
    mps = {l0: fw.ptile("modps", [128, 48, 2])}
    fw.dma(c2[:, :, :], cx.c2, c2)
    fw.op("act", lambda: nc.scalar.activation(out=sc[:, :, :], in_=c2[:, :, :], func=AF.Silu), R=[c2], W=[sc])
    for l in (l0,):
        def load(jb, l=l):
            t = wa[(l * 12 + jb) % 2]
            fw.dma(t[:, :, :], cx.b_ada[l, jb], t, deps=cx.pdep(("a", l)))
            return t

        def comp(jb, t, l=l):
            for jc in range(4):
                for k in range(8):
                    fw.op("pe", lambda jc=jc, k=k: nc.tensor.matmul(
                        mps[l][:, jb * 4 + jc, :], t[:, k, jc * 128:(jc + 1) * 128], sc[:, k, :],
                        start=(k == 0), stop=(k == 7)), R=[t, sc], W=[mps[l]], sig=(k == 7))
        prefetch_loop(12, load, comp)
        v = g.vecs[l]
        fw.op("dve", lambda l=l, v=v: nc.vector.tensor_tensor(
            g.mod[l][:, :, :], mps[l][:, :, :], v[:, V_BA:V_BA + 48].unsqueeze(2).to_broadcast([128, 48, 2]), ALU.add),
            R=[mps[l], v], W=[g.mod[l]])
        for (a, nb, msc) in ((g.a1[l], V_N1, M_SC1), (g.a2[l], V_N2, M_SC2)):
            fw.op("dve", lambda a=a, msc=msc, l=l: nc.vector.tensor_scalar(
                a[:, :, :], g.mod[l][:, msc:msc + 8, :], 1.0, None, ALU.add), R=[g.mod[l]], W=[a])
            fw.op("dve", lambda a=a, nb=nb, v=v: nc.vector.tensor_tensor(
                a[:, :, :], a[:, :, :], v[:, nb:nb + 8].unsqueeze(2).to_broadcast([128, 8, 2]), ALU.mult),
                R=[a, v], W=[a])
    fw.barrier()


def norm_mod(fw, cx, xt, hT, W, scr, a, bsh_tile, bsh_col, n, ss_ps, veng="dve"):
    nc = fw.nc
    g = cx.g
    sq, rt, tmp = scr["sq"], scr["rt"], scr["tmp"]
    fw.op("act", lambda: nc.scalar.activation(out=sq[:, :, 0:W], in_=xt[:, :, 0:W], func=AF.Square), R=[xt], W=[sq])
    for ch in range(8):
        fw.op("pe", lambda ch=ch: nc.tensor.matmul(ss_ps[:, 0:W], g.ones[:, :], sq[:, ch, 0:W], start=(ch == 0), stop=(ch == 7)),
              R=[g.ones, sq], W=[ss_ps], sig=(ch == 7))
    fw.op("act", lambda: nc.scalar.activation(out=rt[:, 0:W], in_=ss_ps[:, 0:W], func=AF.Sqrt, bias=EPS, scale=1.0 / D),
          R=[ss_ps], W=[rt])
    if veng == "dve":
        fw.op("dve", lambda: nc.vector.reciprocal(rt[:, 0:W], rt[:, 0:W]), R=[rt], W=[rt])
        fw.op("dve", lambda: nc.vector.tensor_tensor(tmp[:, :, 0:W], xt[:, :, 0:W],
                                                      rt[:, 0:W].unsqueeze(1).to_broadcast([128, 8, W]), ALU.mult),
              R=[xt, rt], W=[tmp])
    else:
        fw.op("pool", lambda: nc.gpsimd.tensor_tensor(rt[:, 0:W], rt[:, 0:W], g.negone[:, 0:W], ALU.pow), R=[rt, g.negone], W=[rt])
        fw.op("pool", lambda: nc.gpsimd.tensor_tensor(tmp[:, :, 0:W], xt[:, :, 0:W],
                                                       rt[:, 0:W].unsqueeze(1).to_broadcast([128, 8, W]), ALU.mult),
              R=[xt, rt], W=[tmp])
    for ch in range(8):
        fw.op("act", lambda ch=ch: nc.scalar.activation(
            out=hT[:, ch, 0:W], in_=tmp[:, ch, 0:W], func=AF.Identity,
            bias=bsh_tile[:, bsh_col + ch, n:n + 1], scale=a[:, ch, n:n + 1]),
            R=[tmp, a, bsh_tile], W=[hT])


def phase_A(fw, cx, l, s, kv_only):
    nc = fw.nc
    g = cx.g
    sc = cx.__dict__[s.name]
    NB, nsub = s.NB, s.nsub
    fw.phase("A%d%s" % (l, s.name))
    win = fw.tile("win", [128, 8, 2560], BF16, dma=True)
    fw.dma(win[:, :, :], cx.b_in[l], win, deps=cx.pdep(("a", l)))
    csm = fw.tile("csm", [128, 256], BF16, dma=True)
    fw.dma(csm[:, :], cx.csm, csm)
    rcs = None
    if s.rope:
        rcs = fw.tile("rcs", [128, 32, 64], F32, dma=True)
        fw.dma(rcs[:, :, :], cx.rope_cs, rcs)
    xts = fw.ring("xt", 3, [128, 8, NB], F32, dma=True)
    hTs = fw.ring("hT", 2, [128, 8, NB], BF16, dma=True)
    scr = {"sq": fw.tile("sq", [128, 8, NB], BF16), "rt": fw.tile("rt", [128, NB], F32),
           "tmp": None}
    fT = fw.tile("fT", [128, 4, NB], BF16)
    pTs = fw.ring("pT", 2, [128, NB], F32, dma=True)
    ABt = fw.ring("ABt", 1, [128, nsub, 1024], BF16, dma=True)
    qTb = {m: fw.ring("qT" + m, 1, [128, 4, NB], BF16, dma=True) for m in "bc"}
    kTb = fw.ring("kTb", 1, [128, 4, NB], BF16, dma=True)
    vt = {m: fw.ring("vt" + m, 2, [128, nsub, 130], BF16, dma=True) for m in "bc"}
    for m in "bc":
        for t in vt[m]:
            fw.op("dve", lambda t=t: nc.vector.memset(t[:, :, :], 1.0), W=[t])
    buf = {m: fw.ring("buf" + m, 2, [128, 640], F32) for m in "bc"}
    sqb = fw.tile("sqb", [128, 640], F32)
    ssbs = fw.ring("ssb", 2, [128, 10], F32)
    rot = {m: fw.ring("rot" + m, 2, [128, 640], BF16) for m in "bc"}
    kdup = {m: fw.ring("kdup" + m, 2, [128, 2, 2, 64], BF16) for m in "bc"}
    rtmp = [fw.tile("rtmp%d" % i, [128, 320], F32) for i in range(4)]
    ss_ps = fw.ptile("ss_ps", [128, 512])
    pj_ps = fw.pring("pj_ps", 2, [128, 512])
    q_ps = {m: fw.ptile("q_ps" + m, [128, 512]) for m in "bc"}
    kv_ps = fw.ptile("kv_ps", [128, 512])
    tp_ps = fw.pring("tp_ps", 2, [128, 4, 128], BF16)
    a1 = g.a1[l]
    n = s.n
    xin = sc.x[l].rearrange("(c p) t -> p c t", p=128)
    hTd = sc.hT.rearrange("(c p) t -> p c t", p=128)
    pjc = [0]

    def load(blk):
        xt = xts[blk % 3]
        fw.dma(xt[:, :, :], xin[:, :, blk * NB:(blk + 1) * NB], xt)
        return xt

    def do_norm(blk):
        xt = xts[blk % 3]
        scr["tmp"] = xt
        norm_mod(fw, cx, xt, hTs[blk % 2], NB, scr, a1, g.mod[l], M_SH1, n, ss_ps)

    def rope(m, tch, par):
        src, dst = buf[m][par], rot[m][par]
        if not s.rope:
            fw.op("dve", lambda: nc.vector.tensor_copy(dst[:, :], src[:, :]), R=[src], W=[dst])
            return
        sv = src[:, :].rearrange("p (h a f e) -> p h a f e", h=10, a=2, f=2, e=16)
        dv = dst[:, :].rearrange("p (h a f e) -> p h a f e", h=10, a=2, f=2, e=16)
        x1, x2 = sv[:, :, :, 0, :], sv[:, :, :, 1, :]
        cosb = rcs[:, tch, 0:32].rearrange("p (a e) -> p a e", a=2).unsqueeze(1).to_broadcast([128, 10, 2, 16])
        sinb = rcs[:, tch, 32:64].rearrange("p (a e) -> p a e", a=2).unsqueeze(1).to_broadcast([128, 10, 2, 16])
        tv = [t[:, :].rearrange("p (h a e) -> p h a e", h=10, a=2, e=16) for t in rtmp]
        fw.op("dve", lambda: nc.vector.tensor_tensor(tv[0], x1, cosb, ALU.mult), R=[src, rcs], W=[rtmp[0]])
        fw.op("dve", lambda: nc.vector.tensor_tensor(tv[1], x2, sinb, ALU.mult), R=[src, rcs], W=[rtmp[1]])
        fw.op("dve", lambda: nc.vector.tensor_tensor(dv[:, :, :, 0, :], tv[0], tv[1], ALU.subtract),
              R=[rtmp[0], rtmp[1]], W=[dst])
        fw.op("dve", lambda: nc.vector.tensor_tensor(tv[2], x2, cosb, ALU.mult), R=[src, rcs], W=[rtmp[2]])
        fw.op("dve", lambda: nc.vector.tensor_tensor(tv[3], x1, sinb, ALU.mult), R=[src, rcs], W=[rtmp[3]])
        fw.op("dve", lambda: nc.vector.tensor_tensor(dv[:, :, :, 1, :], tv[2], tv[3], ALU.add),
              R=[rtmp[2], rtmp[3]], W=[dst])

    def comp(blk, xt):
        hT = hTs[blk % 2]
        c0, c1 = blk * NB, (blk + 1) * NB
        if not kv_only:
            fw.dma(hTd[:, :, c0:c1], hT[:, :, :], hT, store=True)
            for oc in range(8):
                ps = pj_ps[pjc[0] % 2]
                pjc[0] += 1
                for k in range(8):
                    fw.op("pe", lambda k=k, oc=oc, ps=ps: nc.tensor.matmul(
                        ps[:, 0:NB], win[:, k, oc * 128:(oc + 1) * 128], hT[:, k, :], start=(k == 0), stop=(k == 7)),
                        R=[win, hT], W=[ps], sig=(k == 7))
                if oc < 4:
                    fw.op("act", lambda oc=oc, ps=ps: nc.scalar.copy(fT[:, oc, :], ps[:, 0:NB]), R=[ps], W=[fT])
                else:
                    pT = pTs[oc % 2]
                    fw.op("act", lambda ps=ps, pT=pT: nc.scalar.copy(pT[:, :], ps[:, 0:NB]), R=[ps], W=[pT])
                    fw.dma(sc.pinT[(oc - 4) * 128:(oc - 3) * 128, c0:c1], pT[:, :], pT, store=True)
            abt = ABt[0]
            for sub in range(nsub):
                for gp in range(2):
                    ps = pj_ps[pjc[0] % 2]
                    pjc[0] += 1
                    for gg in range(2):
                        gi = gp * 2 + gg
                        fw.op("pe", lambda gg=gg, gi=gi, ps=ps, sub=sub: nc.tensor.matmul(
                            ps[:, gg * 256:(gg + 1) * 256], fT[:, gi, sub * 128:(sub + 1) * 128], csm[:, :],
                            start=True, stop=True), R=[fT, csm], W=[ps], sig=(gg == 1))
                    fw.op("dve", lambda gp=gp, ps=ps, sub=sub: nc.vector.tensor_copy(
                        abt[:, sub, gp * 512:(gp + 1) * 512], ps[:, :]), R=[ps], W=[abt])
            fw.dma(sc.AB.rearrange("(s p) n -> p s n", p=128)[:, blk * nsub:(blk + 1) * nsub, :], abt[:, :, :], abt, store=True)
        if blk + 1 < s.nblk:
            do_norm(blk + 1)
        kb = kTb[0]

        def stage1(sub):
            t0, t1 = sub * 128, (sub + 1) * 128
            par = (blk * nsub + sub) % 2
            groups = [(kv_ps, 0, 256, 1536), (kv_ps, 256, 256, 2304)]
            if not kv_only:
                groups = [(q_ps["b"], 0, 512, 1024), (q_ps["c"], 0, 512, 1792)] + groups
            for (ps, o0, w, wc) in groups:
                for k in range(8):
                    fw.op("pe", lambda: nc.tensor.matmul(
                        ps[:, o0:o0 + w], hT[:, k, t0:t1], win[:, k, wc:wc + w], start=(k == 0), stop=(k == 7)),
                        R=[hT, win], W=[ps], sig=(k == 7))
            for m, kc0 in (("b", 0), ("c", 256)):
                bf_ = buf[m][par]
                if not kv_only:
                    fw.op("act", lambda: nc.scalar.copy(bf_[:, 0:512], q_ps[m][:, :]), R=[q_ps[m]], W=[bf_])
                fw.op("act", lambda: nc.scalar.copy(bf_[:, 512:640], kv_ps[:, kc0:kc0 + 128]), R=[kv_ps], W=[bf_])
                vtt = vt[m][blk % 2]
                fw.op("act", lambda: nc.scalar.copy(
                    vtt[:, sub, :].rearrange("p (g e) -> p g e", e=65)[:, :, 0:64],
                    kv_ps[:, kc0 + 128:kc0 + 256].rearrange("p (g e) -> p g e", e=64)), R=[kv_ps], W=[vtt])

        def stage2(sub):
            t0, t1 = sub * 128, (sub + 1) * 128
            tch = blk * nsub + sub
            par = tch % 2
            bb = buf["b"][par]
            ssb = ssbs[par]
            fw.op("dve", lambda: nc.vector.tensor_tensor(sqb[:, :], bb[:, :], bb[:, :], ALU.mult), R=[bb], W=[sqb])
            fw.op("dve", lambda: nc.vector.reduce_sum(ssb[:, :], sqb[:, :].rearrange("p (h e) -> p h e", e=64), axis=AX.X),
                  R=[sqb], W=[ssb])
            fw.op("act", lambda: nc.scalar.activation(out=ssb[:, :], in_=ssb[:, :], func=AF.Sqrt, bias=EPS, scale=1.0 / 64),
                  R=[ssb], W=[ssb])
            fw.op("dve", lambda: nc.vector.reciprocal(ssb[:, :], ssb[:, :]), R=[ssb], W=[ssb])

        def stage2b(sub):
            t0, t1 = sub * 128, (sub + 1) * 128
            tch = blk * nsub + sub
            par = tch % 2
            bb = buf["b"][par]
            ssb = ssbs[par]
            fw.op("dve", lambda: nc.vector.tensor_tensor(
                bb[:, :].rearrange("p (h e) -> p h e", e=64), bb[:, :].rearrange("p (h e) -> p h e", e=64),
                ssb[:, :].unsqueeze(2).to_broadcast([128, 10, 64]), ALU.mult), R=[bb, ssb], W=[bb])
            fw.op("dve", lambda: nc.vector.tensor_tensor(bb[:, :], bb[:, :], g.rep[l][:, R_GAIN:R_GAIN + 640], ALU.mult),
                  R=[bb, g.rep[l]], W=[bb])
            for m in "bc":
                rope(m, tch, par)
                kd = kdup[m][par]
                for dd in range(2):
                    fw.op("dve", lambda: nc.vector.tensor_copy(
                        kd[:, :, dd, :], rot[m][par][:, 512:640].rearrange("p (g e) -> p g e", e=64)), R=[rot[m][par]], W=[kd])
            if not kv_only:
                for mi, m in enumerate("bc"):
                    tp = tp_ps[mi]
                    for j in range(4):
                        fw.op("pe", lambda: nc.tensor.transpose(
                            tp[:, j, :], rot[m][par][:, j * 128:(j + 1) * 128], g.ident[:, :]),
                            R=[rot[m][par], g.ident], W=[tp], sig=(j == 3))
                    qq = qTb[m][0]
                    fw.op("act", lambda: nc.scalar.copy(qq[:, :, t0:t1], tp[:, :, :]), R=[tp], W=[qq])
            tp = tp_ps[0]
            for mi, m in enumerate("bc"):
                for gi in range(2):
                    j = mi * 2 + gi
                    fw.op("pe", lambda: nc.tensor.transpose(
                        tp[:, j, :], kdup[m][par][:, gi, :, :].rearrange("p d e -> p (d e)"), g.ident[:, :]),
                        R=[kdup[m][par], g.ident], W=[tp], sig=(j == 3))
            fw.op("act", lambda: nc.scalar.copy(kb[:, :, t0:t1], tp[:, :, :]), R=[tp], W=[kb])

        stage1(0)
        for sub in range(nsub):
            stage2(sub)
            if sub + 1 < nsub:
                stage1(sub + 1)
            stage2b(sub)
        k0 = s.koff + c0
        if not kv_only:
            for m in "bc":
                qq = qTb[m][0]
                fw.dma(sc.qT[m].rearrange("(j p) t -> p j t", p=128)[:, :, c0:c1], qq[:, :, :], qq, store=True)
        for mi, m in enumerate("bc"):
            fw.dma(cx.kT[m].rearrange("g p t -> p g t")[:, :, k0:k0 + NB], kb[:, mi * 2:mi * 2 + 2, :], kb, store=True)
            vtt = vt[m][blk % 2]
            fw.dma(cx.v[m][k0:k0 + NB, :].rearrange("(s p) n -> p s n", p=128), vtt[:, :, :], vtt, store=True)

    do_norm_first = [load(0)]
    if s.nblk > 1:
        do_norm_first.append(load(1))
    do_norm(0)
    for blk in range(s.nblk):
        if blk + 2 < s.nblk:
            load(blk + 2)
        comp(blk, xts[blk % 3])
    fw.barrier()


def phase_B(fw, cx, l, s):
    nc = fw.nc
    sc = cx.__dict__[s.name]
    L = s.L
    KB = min(512, L)
    nkb = L // KB
    ntc = L // 128
    TG = min(4, ntc)
    ntg = ntc // TG
    fw.phase("B%d%s" % (l, s.name))
    AB = fw.tile("AB", [128, ntc, 1024], BF16, dma=True)
    abd = sc.AB.rearrange("(s p) n -> p s n", p=128)
    npc = max(1, ntc // 8)
    for i in range(0, ntc, npc):
        fw.dma(AB[:, i:i + npc, :], abd[:, i:i + npc, :], AB)
    Ct = fw.ring("Ct", 3, [128, TG, KB], BF16, dma=True)
    St = fw.ring("St", 3, [128, TG, KB], BF16, dma=True)
    oa = fw.ring("oa", 2, [128, 4, KB], BF16, dma=True)
    acc = fw.pring("acc", 8, [128, 512])
    Cd = (cx.dftC if s is LAT else cx.dftCc).rearrange("(c p) k -> p c k", p=128)
    Sd = (cx.dftS if s is LAT else cx.dftSc).rearrange("(c p) k -> p c k", p=128)
    items = [(kb, tg) for kb in range(nkb) for tg in range(ntg)]

    def load(i):
        kb, tg = items[i]
        c, st = Ct[i % 3], St[i % 3]
        fw.dma(c[:, :, :], Cd[:, tg * TG:(tg + 1) * TG, kb * KB:(kb + 1) * KB], c)
        fw.dma(st[:, :, :], Sd[:, tg * TG:(tg + 1) * TG, kb * KB:(kb + 1) * KB], st)
        return (c, st)

    def comp(i, cs):
        kb, tg = items[i]
        c, st = cs
        ac = acc[(kb % 2) * 4:(kb % 2) * 4 + 4]
        for tcc in range(TG):
            tc = tg * TG + tcc
            for gi in range(4):
                fw.op("pe", lambda gi=gi, tc=tc, tcc=tcc: nc.tensor.matmul(
                    ac[gi][:, 0:KB], AB[:, tc, gi * 256:gi * 256 + 128], c[:, tcc, :], start=(tc == 0), stop=False),
                    R=[AB, c], W=[ac[gi]], sig=False)
                last = (tc == ntc - 1)
                fw.op("pe", lambda gi=gi, tc=tc, tcc=tcc, last=last: nc.tensor.matmul(
                    ac[gi][:, 0:KB], AB[:, tc, gi * 256 + 128:gi * 256 + 256], st[:, tcc, :], start=False, stop=last),
                    R=[AB, st], W=[ac[gi]], sig=(last or gi == 3))
        if tg == ntg - 1:
            o = oa[kb % 2]
            for gi in range(4):
                eng = "act" if gi % 2 == 0 else "dve"
                if eng == "act":
                    fw.op("act", lambda gi=gi: nc.scalar.copy(o[:, gi, :], ac[gi][:, 0:KB]), R=[ac[gi]], W=[o])
                else:
                    fw.op("dve", lambda gi=gi: nc.vector.tensor_copy(o[:, gi, :], ac[gi][:, 0:KB]), R=[ac[gi]], W=[o])
            fw.dma(sc.br[0].rearrange("(g p) t -> p g t", p=128)[:, :, kb * KB:(kb + 1) * KB], o[:, :, :], o, store=True)

    prefetch_loop(len(items), load, comp)
    fw.barrier()


def phase_C(fw, cx, l, m, ctx_too):
    nc = fw.nc
    g = cx.g
    fw.phase("C%d%s" % (l, m))
    nkc = NK // 128
    KT = fw.tile("KT", [128, 2, NK], BF16, dma=True)
    V = fw.tile("V", [128, nkc, 130], BF16, dma=True)
    ktd = cx.kT[m].rearrange("g p t -> p g t")
    for i in range(2):
        fw.dma(KT[:, i, :], ktd[:, i, :], KT)
    fw.dma(V[:, :, :], cx.v[m].rearrange("(s p) n -> p s n", p=128), V)
    wm = None
    if m == "c":
        wm = fw.tile("wm", [128, 6, 512], BF16, dma=True)
        fw.dma(wm[:, :, :], cx.wmask, wm)
    Qs = fw.ring("Q", 2, [128, 512], BF16, dma=True)
    Ps = fw.ring("P", 8, [128, 512], BF16)
    accs = fw.ring("accs", 4, [128, 512], F32)
    rec = fw.ring("rec", 4, [128, 512], F32)
    ob = fw.ring("ob", 4, [64, 512], BF16, dma=True)
    S_ps = fw.pring("S", 4, [128, 512])
    acc_ps = fw.pring("acc", 2, [128, 512])
    bc_ps = fw.pring("bc", 2, [64, 512])
    cnt = {"s": 0, "p": 0, "o": 0, "a": 0}
    deferred = []

    seqs = [LAT] + ([CTX] if ctx_too else [])
    items = []
    for s in seqs:
        for j in range(4):
            for qb in range(s.nblk):
                items.append((s, j, qb))

    def load(i):
        s, j, qb = items[i]
        sc = cx.__dict__[s.name]
        q = Qs[i % 2]
        NB = s.NB
        fw.dma(q[:, 0:NB], sc.qT[m][j * 128:(j + 1) * 128, qb * NB:(qb + 1) * NB], q)
        return q

    def comp(i, q):
        s, j, qb = items[i]
        sc = cx.__dict__[s.name]
        NB = s.NB
        gi = j // 2
        if s is CTX:
            kcl = [(0, None), (1, None)]
        elif m == "b":
            kcl = [(kc, None) for kc in range(nkc)]
        else:
            kcl = [(0, None), (1, None)]
            for r in range(-1, 5):
                c = qb * 4 + r
                if 0 <= c < T // 128:
                    kcl.append((2 + c, r + 1))
        nk = len(kcl)

        def qk(ki):
            kc, mk = kcl[ki]
            pp = []
            for hh in range(2):
                sp = S_ps[cnt["s"] % 4]
                cnt["s"] += 1
                r0 = hh * 64
                fw.op("pe", lambda: nc.tensor.matmul(
                    sp[:, 0:NB], KT[r0:r0 + 64, gi, kc * 128:(kc + 1) * 128], q[r0:r0 + 64, 0:NB], start=True, stop=True),
                    R=[KT, q], W=[sp])
                p = Ps[cnt["p"] % 8]
                cnt["p"] += 1
                fw.op("act", lambda: nc.scalar.activation(out=p[:, 0:NB], in_=sp[:, 0:NB], func=AF.Exp, scale=0.125),
                      R=[sp], W=[p])
                if mk is not None:
                    fw.op("dve", lambda: nc.vector.tensor_tensor(p[:, 0:NB], p[:, 0:NB], wm[:, mk, 0:NB], ALU.mult),
                          R=[p, wm], W=[p])
                pp.append(p)
            return pp

        pend = qk(0)
        for ki in range(nk):
            kc = kcl[ki][0]
            nxt = qk(ki + 1) if ki + 1 < nk else None
            for hh in range(2):
                fw.op("pe", lambda: nc.tensor.matmul(
                    acc_ps[hh][0:65, 0:NB], V[:, kc, gi * 65:(gi + 1) * 65], pend[hh][:, 0:NB],
                    start=(ki == 0), stop=(ki == nk - 1)), R=[V, pend[hh]], W=[acc_ps[hh]], sig=True)
            pend = nxt
            if ki == 1 or nk == 1:
                while deferred:
                    deferred.pop(0)()
        for hh in range(2):
            h = 2 * j + hh
            a = accs[cnt["a"] % 4]
            rc = rec[cnt["a"] % 4]
            cnt["a"] += 1
            fw.op("dve", lambda: nc.vector.tensor_copy(a[0:65, 0:NB], acc_ps[hh][0:65, 0:NB]), R=[acc_ps[hh]], W=[a])
            if m == "c":
                fw.op("act", lambda: nc.scalar.activation(out=rc[64:65, 0:NB], in_=a[64:65, 0:NB], func=AF.Ln,
                                                          bias=g.esink[l][64:65, h:h + 1], scale=1.0),
                      R=[a, g.esink[l]], W=[rc])
            else:
                fw.op("act", lambda: nc.scalar.activation(out=rc[64:65, 0:NB], in_=a[64:65, 0:NB], func=AF.Ln),
                      R=[a], W=[rc])
            fw.op("act", lambda: nc.scalar.activation(out=rc[64:65, 0:NB], in_=rc[64:65, 0:NB], func=AF.Exp, scale=-1.0),
                  R=[rc], W=[rc])

            def tail(hh=hh, h=h, a=a, rc=rc, NB=NB, sc=sc, qb=qb):
                fw.op("pe", lambda: nc.tensor.matmul(
                    bc_ps[hh][:, 0:NB], g.onesf[64:65, 0:64], rc[64:65, 0:NB], start=True, stop=True),
                    R=[g.onesf, rc], W=[bc_ps[hh]])
                o = ob[cnt["o"] % 4]
                cnt["o"] += 1
                fw.op("dve", lambda: nc.vector.tensor_tensor(o[:, 0:NB], a[0:64, 0:NB], bc_ps[hh][:, 0:NB], ALU.mult),
                      R=[a, bc_ps[hh]], W=[o])
                bi = 1 if m == "b" else 2
                fw.dma(sc.br[bi][h * 64:(h + 1) * 64, qb * NB:(qb + 1) * NB], o[:, 0:NB], o, store=True)
            deferred.append(tail)

    prefetch_loop(len(items), load, comp)
    while deferred:
        deferred.pop(0)()
    fw.barrier()


def phase_D(fw, cx, l, s):
    nc = fw.nc
    g = cx.g
    sc = cx.__dict__[s.name]
    L, NB = s.L, s.NB
    H = 16
    LL = L + 2 * H
    fw.phase("D%d%s" % (l, s.name))
    pw = fw.tile("pw", [128, 4, 128], BF16, dma=True)
    fw.dma(pw[:, :, :], cx.b_pool[l], pw, deps=cx.pdep(("a", l)))
    P = fw.ring("P", 2, [128, LL], F32, dma=True)
    S1 = fw.tile("S1", [128, LL], F32)
    S2 = fw.tile("S2", [128, LL], F32)
    ic = fw.ring("ic", 2, [128, L], F32, dma=True)
    pl = fw.tile("pl", [128, L], BF16)
    od = fw.ring("od", 2, [128, NB], BF16, dma=True)
    ps = fw.pring("ps", 2, [128, 512])
    icd = cx.invcnt if s is LAT else cx.invcntc
    for t in P:
        fw.op("dve", lambda t=t: nc.vector.memset(t[:, :], 0.0), W=[t])
    cnt = [0]

    def load(gi):
        p = P[gi % 2]
        fw.dma(p[:, H:H + L], sc.pinT[gi * 128:(gi + 1) * 128, :], p)
        fw.dma(ic[gi % 2][:, :], icd[gi], ic[gi % 2])
        return (p, ic[gi % 2])

    def comp(gi, pi):
        p, icn = pi
        fw.op("dve", lambda: nc.vector.tensor_tensor(S1[:, 1:LL], p[:, 0:LL - 1], p[:, 1:LL], ALU.add), R=[p], W=[S1])
        cur, oth = S1, S2
        lo, hi = 1, LL
        w = 2
        for _ in range(gi):
            sh = w // 2
            lo2, hi2 = lo + sh, hi - sh
            fw.op("dve", lambda cur=cur, oth=oth, sh=sh, lo2=lo2, hi2=hi2: nc.vector.tensor_tensor(
                oth[:, lo2:hi2], cur[:, lo2 - sh:hi2 - sh], cur[:, lo2 + sh:hi2 + sh], ALU.add), R=[cur], W=[oth])
            cur, oth = oth, cur
            lo, hi = lo2, hi2
            w *= 2
        assert lo <= H and hi >= H + L
        fw.op("dve", lambda cur=cur, oth=oth: nc.vector.tensor_tensor(oth[:, H:H + L], cur[:, H:H + L], icn[:, :], ALU.mult),
              R=[cur, icn], W=[oth])
        fw.op("dve", lambda oth=oth: nc.vector.tensor_tensor(pl[:, :], oth[:, H:H + L], p[:, H:H + L], ALU.subtract),
              R=[oth, p], W=[pl])
        for blk in range(s.nblk):
            pp = ps[cnt[0] % 2]
            o = od[cnt[0] % 2]
            cnt[0] += 1
            fw.op("pe", lambda blk=blk, pp=pp: nc.tensor.matmul(pp[:, 0:NB], pw[:, gi, :], pl[:, blk * NB:(blk + 1) * NB],
                                                             start=True, stop=True), R=[pw, pl], W=[pp])
            fw.op("act", lambda pp=pp, o=o: nc.scalar.activation(out=o[:, :], in_=pp[:, 0:NB], func=AF.Identity,
                                                              scale=g.vecs[l][:, V_PS + gi:V_PS + gi + 1]),
                  R=[pp, g.vecs[l]], W=[o])
            fw.dma(sc.br[3][gi * 128:(gi + 1) * 128, blk * NB:(blk + 1) * NB], o[:, :], o, store=True)

    prefetch_loop(4, load, comp)
    fw.barrier()


def phase_E(fw, cx, l, s):
    nc = fw.nc
    g = cx.g
    sc = cx.__dict__[s.name]
    SB, NB = s.SB, s.NB
    nb = SB // NB
    n = s.n
    fw.phase("E%d%s" % (l, s.name))
    hTs = fw.ring("hT", 2, [128, 8, SB], BF16, dma=True)
    brs = fw.ring("br", 2, [128, 4, 4, SB], BF16, dma=True)
    xts = fw.ring("xt", 2, [128, 8, SB], F32, dma=True)
    mg = fw.tile("mg", [128, 8, SB], BF16)
    wg = fw.ring("wg", 3, [128, 4, 8, 128], BF16, dma=True)
    wb = fw.ring("wb", 3, [128, 4, 4, 128], BF16, dma=True)
    wo = fw.ring("wo", 3, [128, 8, 128], BF16, dma=True)
    rw = fw.tile("rw", [128, 8, 16], BF16, dma=True)
    fw.dma(rw[:, :, :], cx.b_router, rw, deps=cx.pdep(("a", 0)))
    sig = fw.ring("sig", 2, [128, NB], F32)
    macc = fw.tile("macc", [128, NB], F32)
    mtmp = fw.ring("mtmp", 2, [128, NB], F32)
    scr = {"sq": fw.tile("sq", [128, 8, NB], BF16), "rt": fw.tile("rt", [128, NB], F32),
           "tmp": fw.tile("tmp", [128, 8, NB], F32)}
    h2tm_t = fw.tile("h2tm_t", [128, SB // 128, 1024], BF16, dma=True)
    rt_ = {k: fw.tile("r_" + k, [128, 16], F32) for k in ("aff", "sel", "eq", "s2", "selm", "e1", "e2", "ae", "comb")}
    rs_ = {k: fw.tile("rs_" + k, [128, 4], F32) for k in ("t1", "t2", "gs", "ing")}
    r1_ = {k: fw.tile("r1_" + k, [128, 1], F32) for k in ("gm", "m1", "m2", "sum")}
    gate_ps = fw.pring("gate", 2, [128, 512])
    proj_ps = fw.pring("proj", 2, [128, 512])
    y_ps = fw.pring("y", 1, [128, 512])
    ss_ps = fw.ptile("ss", [128, 512])
    lg_ps = fw.ptile("lg", [128, 512])
    tpE = fw.ptile("tpE", [128, 8, 128], BF16)
    hTd = sc.hT.rearrange("(c p) t -> p c t", p=128)
    h2d = sc.h2T.rearrange("(c p) t -> p c t", p=128)
    xd = sc.x[l].rearrange("(c p) t -> p c t", p=128)
    xmd = sc.xmid.rearrange("(c p) t -> p c t", p=128)
    brd = sc.br.rearrange("i (c p) t -> p i c t", p=128)
    v = g.vecs[l]
    rep = g.rep[l]
    cnt = {"g": 0, "y": 0}
    nsub = SB // 128

    def sb_load(sb, part=None):
        s0, s1 = sb * SB, (sb + 1) * SB
        hT, br, xt = hTs[sb % 2], brs[sb % 2], xts[sb % 2]
        if part is None or part == 4:
            fw.dma(hT[:, :, :], hTd[:, :, s0:s1], hT, q="pool")
        for i in range(4):
            if part is None or part == i:
                fw.dma(br[:, i, :, :], brd[:, i, :, s0:s1], br, q="pool")
        if part is None or part == 5:
            fw.dma(xt[:, :, :], xd[:, :, s0:s1], xt, q="pool")
        return (hT, br, xt)

    def make_tail(sb, hT, xt):
        s0 = sb * SB

        def p_norm():
            for blk in range(nb):
                b0, b1 = blk * NB, (blk + 1) * NB
                norm_mod(fw, cx, _Sub(xt, b0, b1), _Sub(hT, b0, b1), NB, scr, g.a2[l], g.mod[l], M_SH2, n, ss_ps, veng="dve")

        def p_router(sub):
            t0, t1 = sub * 128, (sub + 1) * 128
            while dveq:
                dveq.pop(0)()
            for k in range(8):
                fw.op("pe", lambda: nc.tensor.matmul(lg_ps[:, 0:16], hT[:, k, t0:t1], rw[:, k, :], start=(k == 0), stop=(k == 7)),
                      R=[hT, rw], W=[lg_ps], sig=(k == 7))
            chunk = (sb * nsub + sub) if s is LAT else (T // 128 + sub)
            dveq.extend(router(fw, cx, l, lg_ps, rt_, rs_, r1_, chunk))
            for k in range(8):
                fw.op("pe", lambda: nc.tensor.transpose(tpE[:, k, :], hT[:, k, t0:t1], g.ident[:, :]),
                      R=[hT, g.ident], W=[tpE], sig=(k == 7))
            fw.op("act", lambda: nc.scalar.copy(h2tm_t[:, sub, :], tpE[:, :, :].rearrange("p k n -> p (k n)")), R=[tpE], W=[h2tm_t])

        def p_store():
            r0 = (0 if s is LAT else T) + s0
            fw.dma(cx.h2tm[r0:r0 + SB, :].rearrange("(u p) n -> p u n", p=128), h2tm_t[:, :, :], h2tm_t, store=True)

        pieces = {1: [p_norm]}
        for sub in range(nsub):
            pieces[2 + sub] = [lambda sub=sub: p_router(sub)]
        pieces[2 + nsub - 1].append(p_store)
        return pieces

    cur = [sb_load(0)]
    tail = {}
    dveq = []
    nxt = [None]
    wcnt = {"g": 0, "o": 0}
    items = [(sb, kind, oc) for sb in range(s.nsb) for kind in ("gate", "out") for oc in range(8)]

    def load(i):
        sb, kind, oc = items[i]
        if kind == "gate":
            a, b = wg[wcnt["g"] % 3], wb[wcnt["g"] % 3]
            wcnt["g"] += 1
            fw.dma(a[:, :, :, :], cx.b_gate[l, oc], a, deps=cx.pdep(("e", l)))
            fw.dma(b[:, :, :, :], cx.b_branch[l, oc], b, deps=cx.pdep(("e", l)))
            return (a, b)
        w = wo[wcnt["o"] % 3]
        wcnt["o"] += 1
        fw.dma(w[:, :, :], cx.b_out[l, oc], w, deps=cx.pdep(("e", l)))
        return w

    def comp_gate(sb, oc, ab):
        hT, br, xt = cur[0]
        a, b = ab
        for blk in range(nb):
            b0, b1 = blk * NB, (blk + 1) * NB
            for i in range(4):
                gp = gate_ps[cnt["g"] % 2]
                pp = proj_ps[cnt["g"] % 2]
                sg = sig[cnt["g"] % 2]
                mt = mtmp[cnt["g"] % 2]
                cnt["g"] += 1
                for k in range(8):
                    fw.op("pe", lambda: nc.tensor.matmul(
                        gp[:, 0:NB], a[:, i, k, :], hT[:, k, b0:b1], start=(k == 0), stop=(k == 7)),
                        R=[a, hT], W=[gp], sig=(k == 7))
                for c in range(4):
                    fw.op("pe", lambda: nc.tensor.matmul(
                        pp[:, 0:NB], b[:, i, c, :], br[:, i, c, b0:b1], start=(c == 0), stop=(c == 3)),
                        R=[b, br], W=[pp], sig=(c == 3))
                fw.op("act", lambda: nc.scalar.activation(
                    out=sg[:, :], in_=gp[:, 0:NB], func=AF.Sigmoid, bias=v[:, V_BG + i * 8 + oc:V_BG + i * 8 + oc + 1], scale=1.0),
                    R=[gp, v], W=[sg])
                if i == 0:
                    fw.op("dve", lambda: nc.vector.tensor_tensor(macc[:, :], sg[:, :], pp[:, 0:NB], ALU.mult),
                          R=[sg, pp], W=[macc])
                else:
                    fw.op("dve", lambda: nc.vector.tensor_tensor(mt[:, :], sg[:, :], pp[:, 0:NB], ALU.mult),
                          R=[sg, pp], W=[mt])
                    if i < 3:
                        fw.op("dve", lambda: nc.vector.tensor_tensor(macc[:, :], macc[:, :], mt[:, :], ALU.add),
                              R=[macc, mt], W=[macc])
                    else:
                        fw.op("dve", lambda: nc.vector.tensor_tensor(mg[:, oc, b0:b1], macc[:, :], mt[:, :], ALU.add),
                              R=[macc, mt], W=[mg])
                for _ in range(6):
                    if dveq:
                        dveq.pop(0)()
        for f in tail.pop(oc, []):
            f()
        if sb + 1 < s.nsb:
            part = {1: 0, 2: 1, 3: 2, 4: 3, 6: 4, 7: 5}.get(oc)
            if part is not None:
                nxt[0] = sb_load(sb + 1, part)

    def comp_out(sb, oc, w):
        hT, br, xt = cur[0]
        for blk in range(nb):
            b0, b1 = blk * NB, (blk + 1) * NB
            yring = [y_ps[0], gate_ps[0], proj_ps[0], gate_ps[1], proj_ps[1]]
            yp = yring[cnt["y"] % 5]
            cnt["y"] += 1
            for k in range(8):
                fw.op("pe", lambda: nc.tensor.matmul(
                    yp[:, 0:NB], w[:, k, :], mg[:, k, b0:b1], start=(k == 0), stop=(k == 7)),
                    R=[w, mg], W=[yp], sig=(k == 7))
            fw.op("dve", lambda: nc.vector.scalar_tensor_tensor(
                xt[:, oc, b0:b1], yp[:, 0:NB], g.mod[l][:, M_G1 + oc, n:n + 1], xt[:, oc, b0:b1], ALU.mult, ALU.add),
                R=[yp, g.mod[l], xt], W=[xt])
        if oc == 7:
            s0, s1 = sb * SB, (sb + 1) * SB
            assert not tail
            fw.dma(xmd[:, :, s0:s1], xt[:, :, :], xt, store=True, q="pool")
            tail.update(make_tail(sb, hT, xt))
            cur[0] = nxt[0]

    def comp(i, ws):
        sb, kind, oc = items[i]
        if kind == "gate":
            comp_gate(sb, oc, ws)
        else:
            comp_out(sb, oc, ws)

    prefetch_loop(len(items), load, comp, depth=2)
    for oc in sorted(tail):
        for f in tail[oc]:
            f()
    while dveq:
        dveq.pop(0)()
    fw.barrier()


class _Sub:
    def __init__(self, t, b0, b1):
        self.__dict__["t"] = t
        self.__dict__["b0"] = b0
        self.__dict__["b1"] = b1

    def __getitem__(self, k):
        k = list(k)
        sl = k[-1]
        start = 0 if sl.start is None else sl.start
        stop = (self.b1 - self.b0) if sl.stop is None else sl.stop
        k[-1] = slice(self.b0 + start, self.b0 + stop)
        return self.t.h[tuple(k)]

    def __getattr__(self, a):
        return getattr(self.t, a)

    def __setattr__(self, a, v):
        setattr(self.t, a, v)


def router(fw, cx, l, lg_ps, rt_, rs_, r1_, chunk):
    nc = fw.nc
    rep = cx.g.rep[l]
    aff, sel, eq, s2, selm, e1, e2, ae, comb = (rt_[k] for k in ("aff", "sel", "eq", "s2", "selm", "e1", "e2", "ae", "comb"))
    t1, t2, gs, ing = (rs_[k] for k in ("t1", "t2", "gs", "ing"))
    gm, m1, m2, sm = (r1_[k] for k in ("gm", "m1", "m2", "sum"))
    V = nc.vector
    ops = []

    def g4(t):
        return t[:, :].rearrange("p (g e) -> p g e", e=4)

    def b16(t):
        return t[:, :].unsqueeze(2).to_broadcast([128, 4, 4])

    def c16(t):
        return t[:, 0:1].to_broadcast([128, 16])

    fw.op("act", lambda: nc.scalar.activation(out=aff[:, :], in_=lg_ps[:, 0:16], func=AF.Sigmoid), R=[lg_ps], W=[aff])
    ops.append(lambda: fw.op("dve", lambda: V.tensor_tensor(sel[:, :], aff[:, :], rep[:, R_RB:R_RB + 16], ALU.add), R=[aff, rep], W=[sel]))
    ops.append(lambda: fw.op("dve", lambda: V.tensor_reduce(t1[:, :], g4(sel), axis=AX.X, op=ALU.max), R=[sel], W=[t1]))
    ops.append(lambda: fw.op("dve", lambda: V.tensor_tensor(g4(eq), g4(sel), b16(t1), ALU.is_equal), R=[sel, t1], W=[eq]))
    ops.append(lambda: fw.op("dve", lambda: V.scalar_tensor_tensor(s2[:, :], eq[:, :], -BIG, sel[:, :], ALU.mult, ALU.add), R=[eq, sel], W=[s2]))
    ops.append(lambda: fw.op("dve", lambda: V.tensor_reduce(t2[:, :], g4(s2), axis=AX.X, op=ALU.max), R=[s2], W=[t2]))
    ops.append(lambda: fw.op("dve", lambda: V.tensor_tensor(gs[:, :], t1[:, :], t2[:, :], ALU.add), R=[t1, t2], W=[gs]))
    ops.append(lambda: fw.op("dve", lambda: V.tensor_reduce(gm[:, :], gs[:, :], axis=AX.X, op=ALU.max), R=[gs], W=[gm]))
    ops.append(lambda: fw.op("dve", lambda: V.tensor_tensor(ing[:, :], gs[:, :], gm[:, 0:1].to_broadcast([128, 4]), ALU.is_equal), R=[gs, gm], W=[ing]))
    Gall, C4all = cx.g.Gall, cx.g.C4all
    ops.append(lambda: fw.op("dve", lambda: V.tensor_copy(Gall[:, chunk, :], ing[:, :]), R=[ing], W=[Gall]))
    ops.append(lambda: fw.op("dve", lambda: V.tensor_scalar(ing[:, :], ing[:, :], 1.0, BIG, ALU.subtract, ALU.mult), R=[ing], W=[ing]))
    ops.append(lambda: fw.op("dve", lambda: V.tensor_tensor(g4(selm), g4(sel), b16(ing), ALU.add), R=[sel, ing], W=[selm]))
    ops.append(lambda: fw.op("dve", lambda: V.tensor_reduce(m1[:, :], selm[:, :], axis=AX.X, op=ALU.max), R=[selm], W=[m1]))
    ops.append(lambda: fw.op("dve", lambda: V.tensor_tensor(e1[:, :], selm[:, :], c16(m1), ALU.is_equal), R=[selm, m1], W=[e1]))
    ops.append(lambda: fw.op("dve", lambda: V.scalar_tensor_tensor(s2[:, :], e1[:, :], -BIG, selm[:, :], ALU.mult, ALU.add), R=[e1, selm], W=[s2]))
    ops.append(lambda: fw.op("dve", lambda: V.tensor_reduce(m2[:, :], s2[:, :], axis=AX.X, op=ALU.max), R=[s2], W=[m2]))
    ops.append(lambda: fw.op("dve", lambda: V.tensor_tensor(e2[:, :], s2[:, :], c16(m2), ALU.is_equal), R=[s2, m2], W=[e2]))
    ops.append(lambda: fw.op("dve", lambda: V.tensor_tensor(e1[:, :], e1[:, :], e2[:, :], ALU.add), R=[e1, e2], W=[e1]))
    ops.append(lambda: fw.op("dve", lambda: V.tensor_tensor(ae[:, :], aff[:, :], e1[:, :], ALU.mult), R=[aff, e1], W=[ae]))
    ops.append(lambda: fw.op("dve", lambda: V.tensor_reduce(sm[:, :], ae[:, :], axis=AX.X, op=ALU.add), R=[ae], W=[sm]))
    ops.append(lambda: fw.op("dve", lambda: V.reciprocal(sm[:, :], sm[:, :]), R=[sm], W=[sm]))
    ops.append(lambda: fw.op("dve", lambda: V.tensor_scalar(comb[:, :], ae[:, :], sm[:, 0:1], None, ALU.mult), R=[ae, sm], W=[comb]))
    ops.append(lambda: fw.op("dve", lambda: V.tensor_reduce(C4all[:, chunk, :], comb[:, :].rearrange("p (g j) -> p j g", j=4), axis=AX.X, op=ALU.add),
          R=[comb], W=[C4all]))
    return ops


def phase_R(fw, cx, l, with_ctx):
    nc = fw.nc
    g = cx.g
    V = nc.vector
    NCH = NCHT if with_ctx else T // 128
    NS = NSLOT if with_ctx else NSLOT - 1
    W = NCH * 4
    fw.phase("R%d" % l)
    pf = DSem(fw.ges.enter_context(nc.semaphore(fw._name("pf"))))
    nc.sync.dma_start(out=cx.permD, in_=cx.oobfill).then_inc(pf.h, 16)
    nc.sync.dma_start(out=cx.c4D, in_=cx.zero4).then_inc(pf.h, 16)
    pf.total = 32
    pfdep = [(id(pf), pf.h, pf.total, "dma")]
    us = fw.tile("us", [128, 128], BF16, dma=True)
    th9 = fw.tile("th9", [128, 4, 9], F32, dma=True)
    sio = fw.tile("sio", [128, NSLOT, 3], F32, dma=True)
    jp = fw.tile("jp", [128, 4], F32, dma=True)
    fw.dma(us[:, :], cx.ustrict, us)
    fw.dma(th9[:, :, :], cx.th9, th9)
    fw.dma(sio[:, :, :], cx.siota, sio)
    fw.dma(jp[:, :], cx.jp, jp)
    Gb = fw.tile("Gb", [128, W], BF16)
    ca = fw.tile("ca", [128, NCH, 4], F32)
    cb = fw.tile("cb", [128, NCH, 4], F32)
    cn = fw.tile("cn", [128, NCH, 4], F32)
    posg = fw.tile("posg", [128, NCH, 4], F32)
    pos = fw.tile("pos", [128, NCH], F32)
    posi = fw.tile("posi", [128, NCH], I32)
    ntot = fw.tile("ntot", [128, 4], F32)
    cmp9 = fw.tile("cmp9", [128, 4, 9], F32)
    nblk = fw.tile("nblk", [128, 4], F32)
    bb = fw.tile("bb", [128, 4], F32)
    cmp3 = fw.tile("cmp3", [128, NSLOT, 3], F32)
    gsl = fw.tile("gsl", [128, NSLOT], F32)
    wxf = fw.tile("wxf", [128, NSLOT, 4], F32)
    cnt_ps = fw.ptile("cnt_ps", [128, 512])
    rank_ps = fw.ptile("rank_ps", [128, 512])
    Gall = g.Gall
    Gv = Gall[:, 0:NCH, :]
    fw.op("dve", lambda: V.tensor_copy(Gb[:, :], Gv.rearrange("p c g -> p (c g)")), R=[Gall], W=[Gb])
    fw.op("pe", lambda: nc.tensor.matmul(cnt_ps[:, 0:W], g.ones[:, :], Gb[:, :], start=True, stop=True), R=[g.ones, Gb], W=[cnt_ps])
    fw.op("pe", lambda: nc.tensor.matmul(rank_ps[:, 0:W], us[:, :], Gb[:, :], start=True, stop=True), R=[us, Gb], W=[rank_ps])
    fw.op("dve", lambda: V.tensor_copy(cn[:, :, :].rearrange("p c g -> p (c g)"), cnt_ps[:, 0:W]), R=[cnt_ps], W=[cn])
    fw.op("dve", lambda: V.tensor_copy(ca[:, :, :], cn[:, :, :]), R=[cn], W=[ca])
    a, b = ca, cb
    sh = 1
    while sh < NCH:
        fw.op("dve", lambda a=a, b=b: V.tensor_copy(b[:, 0:sh, :], a[:, 0:sh, :]), R=[a], W=[b])
        fw.op("dve", lambda a=a, b=b: V.tensor_tensor(b[:, sh:NCH, :], a[:, sh:NCH, :], a[:, 0:NCH - sh, :], ALU.add), R=[a], W=[b])
        a, b = b, a
        sh *= 2
    incl = a
    fw.op("dve", lambda: V.tensor_copy(ntot[:, :], incl[:, NCH - 1, :]), R=[incl], W=[ntot])
    fw.op("dve", lambda: V.tensor_tensor(cmp9[:, :, :], ntot[:, :].unsqueeze(2).to_broadcast([128, 4, 9]), th9[:, :, :], ALU.is_gt),
          R=[ntot, th9], W=[cmp9])
    fw.op("dve", lambda: V.tensor_reduce(nblk[:, :], cmp9[:, :, :], axis=AX.X, op=ALU.add), R=[cmp9], W=[nblk])
    fw.op("dve", lambda: V.memset(bb[:, :], 0.0), W=[bb])
    for gi in range(1, 4):
        fw.op("dve", lambda gi=gi: V.tensor_tensor(bb[:, gi:gi + 1], bb[:, gi - 1:gi], nblk[:, gi - 1:gi], ALU.add), R=[bb, nblk], W=[bb])
    fw.op("dve", lambda: V.tensor_tensor(posg[:, :, :], incl[:, :, :], cn[:, :, :], ALU.subtract), R=[incl, cn], W=[posg])
    fw.op("dve", lambda: V.tensor_tensor(posg[:, :, :].rearrange("p c g -> p (c g)"), posg[:, :, :].rearrange("p c g -> p (c g)"),
                                          rank_ps[:, 0:W], ALU.add), R=[posg, rank_ps], W=[posg])
    fw.op("dve", lambda: V.scalar_tensor_tensor(posg[:, :, :], bb[:, :].unsqueeze(1).to_broadcast([128, NCH, 4]), 512.0, posg[:, :, :],
                                                 ALU.mult, ALU.add), R=[bb, posg], W=[posg])
    fw.op("dve", lambda: V.tensor_tensor(posg[:, :, :], posg[:, :, :], Gv, ALU.mult), R=[posg, Gall], W=[posg])
    fw.op("dve", lambda: V.tensor_reduce(pos[:, :], posg[:, :, :], axis=AX.X, op=ALU.add), R=[posg], W=[pos])
    fw.op("dve", lambda: V.tensor_copy(posi[:, :], pos[:, :]), R=[pos], W=[posi])
    fw.op("dve", lambda: V.tensor_tensor(cmp3[:, :, :], sio[:, :, :], bb[:, 1:4].unsqueeze(1).to_broadcast([128, NSLOT, 3]), ALU.is_ge),
          R=[sio, bb], W=[cmp3])
    fw.op("dve", lambda: V.tensor_reduce(gsl[:, :], cmp3[:, :, :], axis=AX.X, op=ALU.add), R=[cmp3], W=[gsl])
    fw.op("dve", lambda: V.scalar_tensor_tensor(wxf[:, :, :], gsl[:, :].unsqueeze(2).to_broadcast([128, NSLOT, 4]), 512.0,
                                                 jp[:, :].unsqueeze(1).to_broadcast([128, NSLOT, 4]), ALU.mult, ALU.add),
          R=[gsl, jp], W=[wxf])
    if l > 0:
        fw.op("dve", lambda: V.tensor_scalar(wxf[:, :, :], wxf[:, :, :], float(l * NE * 128), None, ALU.add), R=[wxf], W=[wxf])
    fw.op("dve", lambda: V.tensor_copy(g.widx[:, :, :], wxf[:, :, :]), R=[wxf], W=[g.widx])
    for c in range(NCH):
        fw.idma(cx.permD[:, :], bass.IndirectOffsetOnAxis(ap=posi[:, c:c + 1], axis=0), g.tokidx[:, c:c + 1], None,
                NS * 512 - 1, g.tokidx, posi, store=True, deps=pfdep)
    c4s = fw.tile("c4s", [128, NCH, 4], F32, dma=True)
    fw.op("dve", lambda: V.tensor_copy(c4s[:, :, :], g.C4all[:, 0:NCH, :]), R=[g.C4all], W=[c4s])
    for c in range(NCH):
        fw.idma(cx.c4D[:, :], bass.IndirectOffsetOnAxis(ap=posi[:, c:c + 1], axis=0), c4s[:, c, :], None,
                NS * 512 - 1, c4s, posi, store=True, deps=pfdep)
    fw.barrier()


def phase_F2(fw, cx, l, with_ctx):
    nc = fw.nc
    g = cx.g
    NS = NSLOT if with_ctx else NSLOT - 1
    NT = NTT if with_ctx else T
    NB = 512
    fw.phase("F%d" % l)
    idxs = fw.ring("idxs", 4, [128, 4], I32, dma=True)
    h2g = fw.ring("h2g", 2, [128, 4, 1024], BF16, dma=True)
    h2s = fw.ring("h2s", 2, [128, 8, NB], BF16, dma=True)
    c4 = fw.ring("c4", 2, [128, 4, 4], F32, dma=True)
    c4T = fw.tile("c4T", [4, NB], F32)
    cbs = fw.ring("cbs", 8, [128, NB], F32, dma=True)
    acc = fw.tile("acc", [128, 8, NB], F32, dma=True)
    om = fw.tile("om", [128, 4, 1024], F32, dma=True)
    wc = fw.ring("wc", 3, [128, 12288], BF16, dma=True)
    sg = fw.ring("sg", 2, [128, NB], F32)
    st = fw.ring("st", 2, [128, NB], F32)
    hid = fw.ring("hid", 2, [128, 4, NB], BF16)
    g_ps = fw.pring("g", 2, [128, 512])
    u_ps = fw.pring("u", 2, [128, 512])
    o_ps = fw.pring("o", 2, [128, 512])
    tp_ps = fw.ptile("tp", [128, 8, 128], BF16)
    m_ps = fw.ptile("m", [128, 512])
    for t in h2g:
        fw.op("dve", lambda t=t: nc.vector.memset(t[:, :, :], 0.0), W=[t])
    wcd = cx.b_wcat.rearrange("l e p n -> (l e p) n")
    permv = cx.permD.rearrange("(s p u) o -> s p (u o)", u=4, p=128)
    c4v = cx.c4D.rearrange("(s p u) j -> s p u j", u=4, p=128)
    cnt = {"g": 0, "o": 0, "h": 0, "w": 0, "cb": 0}
    items = [(sl, j) for sl in range(NS) for j in range(4)]
    slot_state = {}

    def slot_prep(sl):
        ix = idxs[sl % 4]
        fw.dma(ix[:, :], permv[sl], ix)
        hg = h2g[sl % 2]
        for u in range(4):
            fw.idma(hg[:, u, :], None, cx.h2tm[:, :], bass.IndirectOffsetOnAxis(ap=ix[:, u:u + 1], axis=0), NT - 1, hg, ix)
        cc = c4[sl % 2]
        fw.dma(cc[:, :, :], c4v[sl], cc)
        slot_state[sl] = (ix, hg, cc)

    def load(i):
        sl, j = items[i]
        if j == 0 and sl + 1 < NS:
            slot_prep(sl + 1)
        k = cnt["w"] % 3
        cnt["w"] += 1
        off = bass.IndirectOffsetOnAxis(ap=g.widx[:, sl, j:j + 1], axis=0)
        dep = cx.pdep(("f", l))
        fw.idma(wc[k][:, :], None, wcd, off, NE * 128 - 1, wc[k], g.widx, deps=dep)
        return wc[k]

    pend = []

    def w2part(j, a2_, hd):
        for oc in range(8):
            op_ = o_ps[cnt["o"] % 2]
            cnt["o"] += 1
            for ff in range(4):
                c0 = 8192 + ff * 1024 + oc * 128
                fw.op("pe", lambda: nc.tensor.matmul(
                    op_[:, 0:NB], a2_[:, c0:c0 + 128], hd[:, ff, :], start=(ff == 0), stop=(ff == 3)),
                    R=[a2_, hd], W=[op_], sig=(ff == 3))
            if j == 0:
                fw.op("act", lambda: nc.scalar.copy(acc[:, oc, :], op_[:, 0:NB]), R=[op_], W=[acc])
            else:
                fw.op("dve", lambda: nc.vector.tensor_tensor(acc[:, oc, :], acc[:, oc, :], op_[:, 0:NB], ALU.add),
                      R=[op_, acc], W=[acc])

    def slot_finish(sl):
        ix = slot_state[sl][0]
        if cx.debug and sl == 0 and l == 0:
            fw.dma(cx.d_acc, acc[:, :, :], acc, store=True)
        for u in range(4):
            for half in range(2):
                op_ = o_ps[cnt["o"] % 2]
                cnt["o"] += 1
                for q in range(4):
                    oc = half * 4 + q
                    fw.op("pe", lambda: nc.tensor.transpose(op_[:, q * 128:(q + 1) * 128], acc[:, oc, u * 128:(u + 1) * 128], g.identf[:, :]),
                          R=[acc, g.identf], W=[op_], sig=(q == 3))
                fw.op("act", lambda: nc.scalar.copy(om[:, u, half * 512:(half + 1) * 512], op_[:, :]), R=[op_], W=[om])
        if cx.debug and sl == 0 and l == 0:
            fw.dma(cx.d_om, om[:, :, :], om, store=True)
        for u in range(4):
            fw.idma(cx.moe_tm[:, :], bass.IndirectOffsetOnAxis(ap=ix[:, u:u + 1], axis=0), om[:, u, :], None, NT - 1, om, ix, store=True)

    def comp(i, ws):
        sl, j = items[i]
        a1_ = a3_ = a2_ = ws
        ix, hg, cc = slot_state[sl]
        hs = h2s[sl % 2]
        if j == 0:
            for u in range(4):
                for k in range(8):
                    fw.op("pe", lambda: nc.tensor.transpose(tp_ps[:, k, :], hg[:, u, k * 128:(k + 1) * 128], g.ident[:, :]),
                          R=[hg, g.ident], W=[tp_ps], sig=(k == 7))
                fw.op("act", lambda: nc.scalar.copy(hs[:, :, u * 128:(u + 1) * 128], tp_ps[:, :, :]), R=[tp_ps], W=[hs])
            for u in range(4):
                fw.op("pe", lambda: nc.tensor.matmul(m_ps[0:4, u * 128:(u + 1) * 128], cc[:, u, :], g.identf[:, :], start=True, stop=True),
                      R=[cc, g.identf], W=[m_ps], sig=(u == 3))
            fw.op("act", lambda: nc.scalar.copy(c4T[:, :], m_ps[0:4, :]), R=[m_ps], W=[c4T])
            if cx.debug and sl == 0 and l == 0:
                fw.dma(cx.d_hs, hs[:, :, :], hs, store=True)
                fw.dma(cx.d_hg, hg[:, :, :], hg, store=True)
        cbe = cbs[cnt["cb"] % 8]
        cnt["cb"] += 1
        fw.op("pe", lambda: nc.tensor.matmul(m_ps[:, :], g.sel4[:, j, :], c4T[:, :], start=True, stop=True), R=[g.sel4, c4T], W=[m_ps])
        fw.op("act", lambda: nc.scalar.copy(cbe[:, :], m_ps[:, :]), R=[m_ps], W=[cbe])
        if cx.debug and sl == 0 and l == 0:
            fw.dma(cx.d_cb[j], cbe[:, :], cbe, store=True)
            if j == 1:
                fw.dma(cx.d_w, a1_[:, :], a1_, store=True)
        hd = hid[cnt["h"] % 2]
        cnt["h"] += 1
        for ff in range(4):
            gp, up = g_ps[cnt["g"] % 2], u_ps[cnt["g"] % 2]
            sgt, stt = sg[cnt["g"] % 2], st[cnt["g"] % 2]
            cnt["g"] += 1
            for k in range(8):
                c0 = k * 512 + ff * 128
                fw.op("pe", lambda: nc.tensor.matmul(
                    gp[:, 0:NB], a1_[:, c0:c0 + 128], hs[:, k, :], start=(k == 0), stop=(k == 7)),
                    R=[a1_, hs], W=[gp], sig=(k == 7))
            for k in range(8):
                c0 = 4096 + k * 512 + ff * 128
                fw.op("pe", lambda: nc.tensor.matmul(
                    up[:, 0:NB], a3_[:, c0:c0 + 128], hs[:, k, :], start=(k == 0), stop=(k == 7)),
                    R=[a3_, hs], W=[up], sig=(k == 7))
            fw.op("act", lambda: nc.scalar.activation(out=sgt[:, :], in_=gp[:, 0:NB], func=AF.Silu), R=[gp], W=[sgt])
            fw.op("dve", lambda: nc.vector.tensor_tensor(stt[:, :], sgt[:, :], cbe[:, :], ALU.mult), R=[sgt, cbe], W=[stt])
            fw.op("dve", lambda: nc.vector.tensor_tensor(hd[:, ff, :], stt[:, :], up[:, 0:NB], ALU.mult), R=[stt, up], W=[hd])
            if ff == 1:
                while pend:
                    pj, pa2, phd, psl, plast = pend.pop(0)
                    w2part(pj, pa2, phd)
                    if plast:
                        slot_finish(psl)
        pend.append((j, a2_, hd, sl, j == 3))

    slot_prep(0)
    prefetch_loop(len(items), load, comp)
    while pend:
        pj, pa2, phd, psl, plast = pend.pop(0)
        w2part(pj, pa2, phd)
        if plast:
            slot_finish(psl)
    fw.barrier()


def phase_G(fw, cx, l, s, final):
    nc = fw.nc
    g = cx.g
    sc = cx.__dict__[s.name]
    NB, nsub, n = s.NB, s.nsub, s.n
    fw.phase("G%d%s" % (l, s.name))
    xts = fw.ring("xt", 2, [128, 8, NB], F32, dma=True)
    mts = fw.ring("mt", 2, [128, nsub, 1024], F32, dma=True)
    t_ps = fw.pring("t", 4, [128, 512])
    ss_ps = fw.ptile("ss", [128, 512])
    if final:
        scr = {"sq": fw.tile("sq", [128, 8, NB], BF16), "rt": fw.tile("rt", [128, NB], F32),
               "tmp": fw.tile("tmp", [128, 8, NB], F32, dma=True)}
    xmd = sc.xmid.rearrange("(c p) t -> p c t", p=128)
    r0 = 0 if s is LAT else T
    cnt = [0]

    def load(blk):
        xt, mt = xts[blk % 2], mts[blk % 2]
        fw.dma(xt[:, :, :], xmd[:, :, blk * NB:(blk + 1) * NB], xt)
        fw.dma(mt[:, :, :], cx.moe_tm[r0 + blk * NB:r0 + (blk + 1) * NB, :].rearrange("(u p) n -> p u n", p=128), mt)
        return (xt, mt)

    def comp(blk, xm):
        xt, mt = xm
        for oc in range(8):
            tp = t_ps[cnt[0] % 4]
            cnt[0] += 1
            for u in range(nsub):
                fw.op("pe", lambda: nc.tensor.transpose(tp[:, u * 128:(u + 1) * 128], mt[:, u, oc * 128:(oc + 1) * 128], g.identf[:, :]),
                      R=[mt, g.identf], W=[tp], sig=(u == nsub - 1))
            fw.op("dve", lambda: nc.vector.scalar_tensor_tensor(
                xt[:, oc, :], tp[:, 0:NB], g.mod[l][:, M_G2 + oc, n:n + 1], xt[:, oc, :], ALU.mult, ALU.add),
                R=[tp, g.mod[l], xt], W=[xt])
        c0, c1 = blk * NB, (blk + 1) * NB
        if not final:
            fw.dma(sc.x[l + 1].rearrange("(c p) t -> p c t", p=128)[:, :, c0:c1], xt[:, :, :], xt, store=True)
        else:
            y = scr["tmp"]
            final_norm(fw, cx, xt, y, NB, scr, g.vecs[l], ss_ps)
            fw.dma(cx.yT.rearrange("(c p) t -> p c t", p=128)[:, :, c0:c1], y[:, :, :], y, store=True)

    prefetch_loop(s.nblk, load, comp)
    fw.barrier()


def phase_F(fw, cx, l, s, final):
    nc = fw.nc
    g = cx.g
    sc = cx.__dict__[s.name]
    SB, NB = s.SB, s.NB
    nb = SB // NB
    n = s.n
    fw.phase("F%d%s" % (l, s.name))
    h2 = fw.tile("h2", [128, 8, SB], BF16, dma=True)
    xt = fw.tile("xt", [128, 8, SB], F32, dma=True)
    acc = fw.tile("acc", [128, 8, SB], F32)
    w1 = fw.ring("w1", 3, [128, 8, 512], BF16, dma=True)
    w3 = fw.ring("w3", 3, [128, 8, 512], BF16, dma=True)
    w2 = fw.ring("w2", 3, [128, 4, 1024], BF16, dma=True)
    cb = fw.ring("cb", 3, [128, SB], F32, dma=True)
    sg = fw.ring("sg", 2, [128, NB], F32)
    st = fw.ring("st", 2, [128, NB], F32)
    hid = fw.ring("hid", 2, [128, 4, NB], BF16)
    g_ps = fw.pring("g", 2, [128, 512])
    u_ps = fw.pring("u", 2, [128, 512])
    o_ps = fw.pring("o", 3, [128, 512])
    ss_ps = fw.ptile("ss", [128, 512])
    if final:
        scr = {"sq": fw.tile("sq", [128, 8, NB], BF16), "rt": fw.tile("rt", [128, NB], F32),
               "tmp": fw.tile("tmp", [128, 8, NB], F32, dma=True)}
    h2d = sc.h2T.rearrange("(c p) t -> p c t", p=128)
    xmd = sc.xmid.rearrange("(c p) t -> p c t", p=128)
    cnt = {"g": 0, "o": 0, "h": 0}
    for sb in range(s.nsb):
        s0, s1 = sb * SB, (sb + 1) * SB
        fw.dma(h2[:, :, :], h2d[:, :, s0:s1], h2)
        fw.dma(xt[:, :, :], xmd[:, :, s0:s1], xt)

        def load(e):
            i = (sb * NE + e) % 3
            fw.dma(w1[i][:, :, :], cx.b_w1[l, e], w1[i], deps=cx.pdep(("f", l)))
            fw.dma(w3[i][:, :, :], cx.b_w3[l, e], w3[i], deps=cx.pdep(("f", l)))
            fw.dma(w2[i][:, :, :], cx.b_w2[l, e], w2[i], deps=cx.pdep(("f", l)))
            fw.dma(cb[i][:, :], sc.combT[e:e + 1, s0:s1].partition_broadcast(128), cb[i])
            return (w1[i], w3[i], w2[i], cb[i])

        pend = []

        def w2part(e, a2_, hd, b0, b1):
            for oc in range(8):
                op_ = o_ps[cnt["o"] % 3]
                cnt["o"] += 1
                for ff in range(4):
                    fw.op("pe", lambda: nc.tensor.matmul(
                        op_[:, 0:NB], a2_[:, ff, oc * 128:(oc + 1) * 128], hd[:, ff, :], start=(ff == 0), stop=(ff == 3)),
                        R=[a2_, hd], W=[op_], sig=(ff == 3))
                if e == 0:
                    fw.op("act", lambda: nc.scalar.copy(acc[:, oc, b0:b1], op_[:, 0:NB]), R=[op_], W=[acc])
                else:
                    fw.op("dve", lambda: nc.vector.tensor_tensor(acc[:, oc, b0:b1], acc[:, oc, b0:b1], op_[:, 0:NB], ALU.add),
                          R=[op_, acc], W=[acc])

        def comp(e, ws):
            a1_, a3_, a2_, cbe = ws
            for blk in range(nb):
                b0, b1 = blk * NB, (blk + 1) * NB
                hd = hid[cnt["h"] % 2]
                cnt["h"] += 1
                for ff in range(4):
                    gp, up = g_ps[cnt["g"] % 2], u_ps[cnt["g"] % 2]
                    sgt, stt = sg[cnt["g"] % 2], st[cnt["g"] % 2]
                    cnt["g"] += 1
                    for k in range(8):
                        fw.op("pe", lambda: nc.tensor.matmul(
                            gp[:, 0:NB], a1_[:, k, ff * 128:(ff + 1) * 128], h2[:, k, b0:b1], start=(k == 0), stop=(k == 7)),
                            R=[a1_, h2], W=[gp], sig=(k == 7))
                    for k in range(8):
                        fw.op("pe", lambda: nc.tensor.matmul(
                            up[:, 0:NB], a3_[:, k, ff * 128:(ff + 1) * 128], h2[:, k, b0:b1], start=(k == 0), stop=(k == 7)),
                            R=[a3_, h2], W=[up], sig=(k == 7))
                    fw.op("act", lambda: nc.scalar.activation(out=sgt[:, :], in_=gp[:, 0:NB], func=AF.Silu),
                          R=[gp], W=[sgt])
                    fw.op("dve", lambda: nc.vector.tensor_tensor(stt[:, :], sgt[:, :], cbe[:, b0:b1], ALU.mult),
                          R=[sgt, cbe], W=[stt])
                    fw.op("dve", lambda: nc.vector.tensor_tensor(hd[:, ff, :], stt[:, :], up[:, 0:NB], ALU.mult),
                          R=[stt, up], W=[hd])
                    if ff == 1:
                        while pend:
                            w2part(*pend.pop(0))
                pend.append((e, a2_, hd, b0, b1))
        prefetch_loop(NE, load, comp)
        while pend:
            w2part(*pend.pop(0))
        for oc in range(8):
            fw.op("dve", lambda oc=oc: nc.vector.scalar_tensor_tensor(
                xt[:, oc, :], acc[:, oc, :], g.mod[l][:, M_G2 + oc, n:n + 1], xt[:, oc, :], ALU.mult, ALU.add),
                R=[acc, g.mod[l], xt], W=[xt])
        if not final:
            fw.dma(sc.x[l + 1].rearrange("(c p) t -> p c t", p=128)[:, :, s0:s1], xt[:, :, :], xt, store=True)
        else:
            v = g.vecs[l]
            for blk in range(nb):
                b0, b1 = blk * NB, (blk + 1) * NB
                xv = _Sub(xt, b0, b1)
                y = scr["tmp"]
                final_norm(fw, cx, xv, y, NB, scr, v, ss_ps)
                fw.dma(cx.yT.rearrange("(c p) t -> p c t", p=128)[:, :, s0 + b0:s0 + b1], y[:, :, :], y, store=True)
    fw.barrier()


def final_norm(fw, cx, xt, y, W, scr, v, ss_ps):
    nc = fw.nc
    g = cx.g
    sq, rt, tmp = scr["sq"], scr["rt"], scr["tmp"]
    fw.op("act", lambda: nc.scalar.activation(out=sq[:, :, 0:W], in_=xt[:, :, 0:W], func=AF.Square), R=[xt], W=[sq])
    for ch in range(8):
        fw.op("pe", lambda ch=ch: nc.tensor.matmul(ss_ps[:, 0:W], g.ones[:, :], sq[:, ch, 0:W], start=(ch == 0), stop=(ch == 7)),
              R=[g.ones, sq], W=[ss_ps], sig=(ch == 7))
    fw.op("act", lambda: nc.scalar.activation(out=rt[:, 0:W], in_=ss_ps[:, 0:W], func=AF.Sqrt, bias=EPS, scale=1.0 / D),
          R=[ss_ps], W=[rt])
    fw.op("dve", lambda: nc.vector.reciprocal(rt[:, 0:W], rt[:, 0:W]), R=[rt], W=[rt])
    fw.op("dve", lambda: nc.vector.tensor_tensor(tmp[:, :, 0:W], xt[:, :, 0:W],
                                                  rt[:, 0:W].unsqueeze(1).to_broadcast([128, 8, W]), ALU.mult),
          R=[xt, rt], W=[tmp])
    fw.op("dve", lambda: nc.vector.tensor_tensor(y[:, :, 0:W], tmp[:, :, 0:W],
                                                  v[:, V_NF:V_NF + 8].unsqueeze(2).to_broadcast([128, 8, W]), ALU.mult),
          R=[tmp, v], W=[y])


def _fm(vv):
    return np.ascontiguousarray(np.asarray(vv, np.float32).reshape(-1, 128).T)


def _kmaj(w):
    K, N = w.shape
    return np.ascontiguousarray(w.reshape(K // 128, 128, N).transpose(1, 0, 2))


_CONST = {}


def constants():
    if _CONST:
        return _CONST
    bf = ml_dtypes.bfloat16
    t = np.arange(T)
    row = (t // 64).astype(np.float64)
    col = (t % 64).astype(np.float64)
    inv = 10000.0 ** (-np.arange(0, 32, 2, dtype=np.float64) / 32)
    ang = np.stack([row[:, None] * inv, col[:, None] * inv], axis=1).astype(np.float32)
    cs = np.concatenate([np.cos(ang).reshape(T, 32), np.sin(ang).reshape(T, 32)], axis=1).astype(np.float32)
    _CONST["rope_cs"] = np.ascontiguousarray(cs.reshape(32, 128, 64).transpose(1, 0, 2))

    def dft(L):
        i = np.arange(L, dtype=np.int64)
        ph = (np.outer(i, i) % L).astype(np.float64) * (2 * np.pi / L)
        sc = 1.0 / np.sqrt(L * 128.0)
        return (np.cos(ph) * sc).astype(bf), (-np.sin(ph) * sc).astype(bf)
    _CONST["dftC"], _CONST["dftS"] = dft(T)
    _CONST["dftCc"], _CONST["dftSc"] = dft(CL)
    i = np.arange(128, dtype=np.int64)
    ph = (np.outer(i, i) % 128).astype(np.float64) * (2 * np.pi / 128)
    _CONST["csm"] = np.concatenate([np.cos(ph), np.sin(ph)], axis=1).astype(bf)
    _CONST["ident"] = np.eye(128, dtype=np.float32).astype(bf)
    _CONST["identf"] = np.eye(128, dtype=np.float32)
    kk = np.arange(128)[:, None]
    qq = np.arange(512)[None, :]
    wm = np.zeros((128, 6, 512), np.float32)
    for r in range(-1, 5):
        wm[:, r + 1, :] = (np.abs(qq - (128 * r + kk)) <= 128)
    _CONST["wmask"] = wm.astype(bf)

    def invc(L):
        pos = np.arange(L)
        out = np.zeros((4, 128, L), np.float32)
        for gi, w in enumerate((2, 4, 8, 16)):
            lo = np.clip(pos - w // 2, 0, L)
            hi = np.clip(pos + w // 2, 0, L)
            out[gi] = (1.0 / (hi - lo).astype(np.float32))[None, :]
        return out
    pp = np.arange(128)
    _CONST["ustrict"] = (pp[:, None] < pp[None, :]).astype(np.float32).astype(bf)
    _CONST["th9"] = np.ascontiguousarray(np.broadcast_to((512.0 * np.arange(9, dtype=np.float32))[None, None, :], (128, 4, 9)))
    _CONST["siota"] = np.ascontiguousarray(np.broadcast_to(np.arange(NSLOT, dtype=np.float32)[None, :, None], (128, NSLOT, 3)))
    _CONST["jp"] = (np.arange(4, dtype=np.float32)[None, :] * 128 + pp[:, None]).astype(np.float32)
    _CONST["tokidx"] = (np.arange(NCHT, dtype=np.int32)[None, :] * 128 + pp[:, None]).astype(np.int32)
    sel = np.zeros((4, 4, 128), np.float32)
    for jj in range(4):
        sel[jj, jj, :] = 1.0
    _CONST["sel4"] = sel
    _CONST["oobfill"] = np.full((NSLOT * 512, 1), NTT, np.int32)
    _CONST["zrow"] = np.zeros((128, D), np.float32).astype(bf)
    _CONST["zero4"] = np.zeros((NSLOT * 512, 4), np.float32)
    _CONST["invcnt"] = invc(T)
    _CONST["invcntc"] = invc(CL)
    return _CONST


def prep_shared(inp):
    f = lambda a: np.asarray(a, np.float32)
    sh = {}
    w_ada = f(inp["w_ada"])
    sh["w_ada"] = np.ascontiguousarray(w_ada.reshape(DEPTH, 8, 128, 12, 512).transpose(0, 3, 2, 1, 4))
    w_in = f(inp["w_in"])
    sh["w_in"] = np.ascontiguousarray(w_in.reshape(DEPTH, 8, 128, 2560).transpose(0, 2, 1, 3))
    wg = f(inp["w_gate"])
    sh["w_gate"] = np.ascontiguousarray(wg.reshape(DEPTH, 4, 8, 128, 8, 128).transpose(0, 4, 3, 1, 2, 5))
    wb = f(inp["w_branch"])
    sh["w_branch"] = np.ascontiguousarray(wb.reshape(DEPTH, 4, 4, 128, 8, 128).transpose(0, 4, 3, 1, 2, 5))
    wo = f(inp["w_out"])
    sh["w_out"] = np.ascontiguousarray(wo.reshape(DEPTH, 8, 128, 8, 128).transpose(0, 3, 2, 1, 4))
    pw = f(inp["pool_w"])
    sh["pool_w"] = np.ascontiguousarray(pw.transpose(0, 2, 1, 3))
    sh["router_w"] = _kmaj(f(inp["router_w"]))
    sh["w1"] = np.ascontiguousarray(f(inp["w1"]).reshape(DEPTH, NE, 8, 128, 512).transpose(0, 1, 3, 2, 4))
    sh["w3"] = np.ascontiguousarray(f(inp["w3"]).reshape(DEPTH, NE, 8, 128, 512).transpose(0, 1, 3, 2, 4))
    sh["w2"] = np.ascontiguousarray(f(inp["w2"]).reshape(DEPTH, NE, 4, 128, 1024).transpose(0, 1, 3, 2, 4))
    vecs = np.zeros((DEPTH, 128, NV), np.float32)
    rep = np.zeros((DEPTH, 128, NR), np.float32)
    for l in range(DEPTH):
        vecs[l, :, V_N1:V_N1 + 8] = _fm(inp["norm1"][l])
        vecs[l, :, V_N2:V_N2 + 8] = _fm(inp["norm2"][l])
        vecs[l, :, V_BG:V_BG + 32] = _fm(f(inp["b_gate"][l]).reshape(-1))
        vecs[l, :, V_PS:V_PS + 4] = _fm(inp["pool_scale"][l])
        vecs[l, :, V_BA:V_BA + 48] = _fm(inp["b_ada"][l])
        vecs[l, :, V_NF:V_NF + 8] = _fm(inp["norm_f"])
        gain = np.concatenate([np.tile(f(inp["q_gain"][l]), 8), np.tile(f(inp["k_gain"][l]), 2)])
        rep[l, :, R_GAIN:R_GAIN + 640] = gain[None, :]
        rep[l, :, R_SINK:R_SINK + 8] = f(inp["sink"][l])[None, :]
        rep[l, :, R_RB:R_RB + 16] = f(inp["router_bias"])[None, :]
    sh["vecs"] = vecs
    sh["rep"] = rep
    sh.update(constants())
    return sh


_NC = {}


def kernel(**inputs):
    inp = {k: np.asarray(v) for k, v in inputs.items()}
    if "nc" not in _NC:
        _NC["nc"] = build()
    nc = _NC["nc"]
    sh = prep_shared(inp)
    x = np.asarray(inp["x"], np.float32)
    ctx = np.asarray(inp["ctx"], np.float32)
    c = np.asarray(inp["c"], np.float32)
    c_ctx = np.asarray(inp["c_ctx"], np.float32)
    in_maps = []
    for b in range(8):
        m = dict(sh)
        m["xT"] = np.ascontiguousarray(x[b].T)
        m["ctxT"] = np.ascontiguousarray(ctx[b].T)
        c2 = np.stack([c[b], c_ctx], axis=1)
        m["c2"] = np.ascontiguousarray(c2.reshape(8, 128, 2).transpose(1, 0, 2))
        in_maps.append(m)
    res = run_bass_kernel_spmd(nc, in_maps, core_ids=list(range(8)))
    out = np.stack([np.ascontiguousarray(res.results[b]["yT"].T) for b in range(8)], axis=0)
    return out.astype(np.float32)
```
